# Optimizing a Trainium2 kernel written in Bass

```python
import math
import jax, jax.numpy as jnp
from jax import lax
import numpy as np

D_MODEL = 2048
BATCH = 2
SEQ = 4096
DEPTH = 1

HEAD_DIM = 128
NSA_HEADS = 8
NSA_KV_HEADS = 2
NSA_GROUP = NSA_HEADS // NSA_KV_HEADS
CMP_BLOCK = 32
CMP_STRIDE = 16
CMP_HIDDEN = 256
SEL_BLOCK = 64
SEL_TOPN = 16
WINDOW = 512
FORCED_SCORE = 1e4
MOBA_HEADS = 8
MOBA_BLOCK = 256
MOBA_TOPK = 3
N_HEADS_TOTAL = NSA_HEADS + MOBA_HEADS
REL_BUCKETS = 32
REL_MAX_DIST = 128
N_GROUPS = 8
EXPERTS_PER_GROUP = 8
N_EXPERTS = N_GROUPS * EXPERTS_PER_GROUP
EXPERT_FF = D_MODEL // 4
EXPERT_TOPK = 2
MOE_ROW_BLOCK = 128
Q_CHUNK = 64
BAND_BLOCK = 128
RMS_EPS = 1e-6
NSA_Q_COLS = NSA_HEADS * HEAD_DIM
NSA_KV_COLS = NSA_KV_HEADS * HEAD_DIM
NSA_GATE_COLS = 3 * NSA_HEADS
MOBA_COLS = MOBA_HEADS * HEAD_DIM
IN_SPLITS = (NSA_Q_COLS,) + (NSA_KV_COLS,) * 6 + (NSA_GATE_COLS, MOBA_COLS, MOBA_COLS, MOBA_COLS, D_MODEL, D_MODEL)
IN_COLS = sum(IN_SPLITS)

kernel_name = 'hybrid_nsa_moba_hier_moe_block'


def rmsnorm(x, g):
    xf = x.astype(jnp.float32)
    y = xf * lax.rsqrt(jnp.mean(xf * xf, axis=-1, keepdims=True) + RMS_EPS)
    return (y * g.astype(jnp.float32)).astype(x.dtype)


def rel_bucket(dist):
    n = jnp.maximum(jnp.asarray(dist, jnp.int32), 0)
    max_exact = REL_BUCKETS // 2
    nf = jnp.maximum(n, 1).astype(jnp.float32)
    large = max_exact + (jnp.log(nf / max_exact) / math.log(REL_MAX_DIST / max_exact) * (REL_BUCKETS - max_exact)).astype(jnp.int32)
    return jnp.where(n < max_exact, n, jnp.minimum(large, REL_BUCKETS - 1))


def masked_softmax(logits, mask):
    logits = jnp.where(mask, logits, -jnp.inf)
    m = jnp.max(logits, axis=-1, keepdims=True)
    m = jnp.where(jnp.isfinite(m), m, 0.0)
    p = jnp.exp(logits - m)
    d = jnp.sum(p, axis=-1, keepdims=True)
    return p / jnp.where(d > 0, d, 1.0)


def compress_blocks(kv, pe, w1, w2):
    B, S, G, dh = kv.shape
    n_c = (S - CMP_BLOCK) // CMP_STRIDE + 1
    idx = np.arange(n_c)[:, None] * CMP_STRIDE + np.arange(CMP_BLOCK)[None, :]
    blocks = kv[:, idx] + pe[None, None, :, None, :]
    blocks = blocks.transpose(0, 3, 1, 2, 4).reshape(B, G, n_c, CMP_BLOCK * dh)
    return jax.nn.gelu(blocks @ w1) @ w2


def nsa_mixer(q, k_cmp, v_cmp, k_slc, v_slc, k_win, v_win, gate_logits,
              pe_k, w1_k, w2_k, pe_v, w1_v, w2_v, rel_tab):
    B, S, H, dh = q.shape
    G = k_cmp.shape[2]
    hg = H // G
    scale = dh ** -0.5
    t = np.arange(S)
    qg = q.reshape(B, S, G, hg, dh).transpose(0, 2, 3, 1, 4)

    kc = compress_blocks(k_cmp, pe_k, w1_k, w2_k)
    vc = compress_blocks(v_cmp, pe_v, w1_v, w2_v)
    n_c = kc.shape[2]
    c_start = np.arange(n_c) * CMP_STRIDE
    dist_c = t[:, None] - (c_start + CMP_BLOCK - 1)[None, :]
    bias_c = rel_tab[rel_bucket(dist_c)].transpose(2, 0, 1).reshape(G, hg, S, n_c)
    logit_c = jnp.einsum('bgjsd,bgcd->bgjsc', qg, kc).astype(jnp.float32) * scale + bias_c
    p_c = masked_softmax(logit_c, dist_c >= 0)
    o_c = jnp.einsum('bgjsc,bgcd->bgjsd', p_c.astype(vc.dtype), vc)

    n_sel = S // SEL_BLOCK
    sb_start = np.arange(n_sel) * SEL_BLOCK
    overlap = (c_start[None, :] < sb_start[:, None] + SEL_BLOCK) & (c_start[None, :] + CMP_BLOCK > sb_start[:, None])
    p_sel = jnp.einsum('bgjsc,nc->bgsn', p_c, jnp.asarray(overlap, jnp.float32))
    j = np.arange(n_sel)[None, :]
    cur = (t // SEL_BLOCK)[:, None]
    valid = sb_start[None, :] <= t[:, None]
    forced = (j == 0) | (j == cur) | (j == cur - 1)
    score = jnp.where(forced, FORCED_SCORE, jnp.where(valid, p_sel, -jnp.inf))
    n_top = min(SEL_TOPN, n_sel)
    _, sel_idx = lax.top_k(score, n_top)

    ks_b = k_slc.transpose(0, 2, 1, 3).reshape(B, G, n_sel, SEL_BLOCK, dh)
    vs_b = v_slc.transpose(0, 2, 1, 3).reshape(B, G, n_sel, SEL_BLOCK, dh)
    tab_g = rel_tab.reshape(REL_BUCKETS, G, hg).transpose(1, 0, 2)
    b_ix = jnp.arange(B)[:, None, None, None]
    g_ix = jnp.arange(G)[None, :, None, None]
    n_keys = n_top * SEL_BLOCK

    def sel_chunk(ci):
        t0 = ci * Q_CHUNK
        qc = lax.dynamic_slice_in_dim(qg, t0, Q_CHUNK, axis=3)
        ic = lax.dynamic_slice_in_dim(sel_idx, t0, Q_CHUNK, axis=2)
        kk = ks_b[b_ix, g_ix, ic].reshape(B, G, Q_CHUNK, n_keys, dh)
        vv = vs_b[b_ix, g_ix, ic].reshape(B, G, Q_CHUNK, n_keys, dh)
        pos = (ic[..., None] * SEL_BLOCK + jnp.arange(SEL_BLOCK)).reshape(B, G, Q_CHUNK, n_keys)
        dist = (t0 + jnp.arange(Q_CHUNK))[:, None] - pos
        bias = jnp.moveaxis(tab_g[g_ix, rel_bucket(dist)], -1, 2)
        logit = jnp.einsum('bgjqd,bgqld->bgjql', qc, kk).astype(jnp.float32) * scale + bias
        p = masked_softmax(logit, (dist >= 0)[:, :, None])
        return jnp.einsum('bgjql,bgqld->bgjqd', p.astype(vv.dtype), vv)

    o_s = lax.map(sel_chunk, jnp.arange(S // Q_CHUNK))
    o_s = jnp.moveaxis(o_s, 0, 3).reshape(B, G, hg, S, dh)

    n_band = S // BAND_BLOCK
    span = BAND_BLOCK + WINDOW
    band_idx = np.arange(n_band)[:, None] * BAND_BLOCK + np.arange(span)[None, :]
    pad = ((0, 0), (0, 0), (WINDOW, 0), (0, 0))
    kw_b = jnp.pad(k_win.transpose(0, 2, 1, 3), pad)[:, :, band_idx]
    vw_b = jnp.pad(v_win.transpose(0, 2, 1, 3), pad)[:, :, band_idx]
    qb = qg.reshape(B, G, hg, n_band, BAND_BLOCK, dh)
    r = np.arange(BAND_BLOCK)[:, None]
    c = np.arange(span)[None, :]
    dist_w = r + WINDOW - c
    key_pos = np.arange(n_band)[:, None, None] * BAND_BLOCK + c[None] - WINDOW
    mask_w = (dist_w >= 0) & (dist_w < WINDOW) & (key_pos >= 0)
    bias_w = rel_tab[rel_bucket(dist_w)].transpose(2, 0, 1).reshape(G, hg, 1, BAND_BLOCK, span)
    logit_w = jnp.einsum('bgjnqd,bgnkd->bgjnqk', qb, kw_b).astype(jnp.float32) * scale + bias_w
    p_w = masked_softmax(logit_w, mask_w)
    o_w = jnp.einsum('bgjnqk,bgnkd->bgjnqd', p_w.astype(vw_b.dtype), vw_b).reshape(B, G, hg, S, dh)

    gates = jax.nn.sigmoid(gate_logits.astype(jnp.float32)).reshape(B, S, H, 3)

    def to_bshd(o):
        return o.transpose(0, 3, 1, 2, 4).reshape(B, S, H, dh)

    out = gates[..., 0:1] * to_bshd(o_c) + gates[..., 1:2] * to_bshd(o_s) + gates[..., 2:3] * to_bshd(o_w)
    return out.reshape(B, S, H * dh).astype(q.dtype)


def moba_mixer(q, k, v, rel_tab):
    B, S, H, dh = q.shape
    scale = dh ** -0.5
    q, k, v = (a.transpose(0, 2, 1, 3) for a in (q, k, v))
    n_blk = -(-S // MOBA_BLOCK)
    pad = ((0, 0), (0, 0), (0, n_blk * MOBA_BLOCK - S), (0, 0))
    kb = jnp.pad(k, pad).reshape(B, H, n_blk, MOBA_BLOCK, dh)
    vb = jnp.pad(v, pad).reshape(B, H, n_blk, MOBA_BLOCK, dh)
    k_mean = jnp.mean(kb, axis=3)
    t = np.arange(S)
    own = t // MOBA_BLOCK
    past = np.arange(n_blk)[None, :] < own[:, None]
    gate = jnp.einsum('bhsd,bhnd->bhsn', q, k_mean).astype(jnp.float32)
    gate = jnp.where(past, gate, -jnp.inf)
    n_top = max(1, min(MOBA_TOPK, n_blk - 1))
    _, top_idx = lax.top_k(gate, n_top)
    top_ok = top_idx < own[:, None]
    tab_h = rel_tab.T
    b_ix = jnp.arange(B)[:, None, None, None]
    h_ix = jnp.arange(H)[None, :, None, None]
    n_keys = n_top * MOBA_BLOCK

    def chunk(ci):
        t0 = ci * Q_CHUNK
        blk = t0 // MOBA_BLOCK
        tq = t0 + jnp.arange(Q_CHUNK)
        qc = lax.dynamic_slice_in_dim(q, t0, Q_CHUNK, axis=2)
        ic = lax.dynamic_slice_in_dim(top_idx, t0, Q_CHUNK, axis=2)
        okc = lax.dynamic_slice_in_dim(top_ok, t0, Q_CHUNK, axis=2)
        kk = kb[b_ix, h_ix, ic].reshape(B, H, Q_CHUNK, n_keys, dh)
        vv = vb[b_ix, h_ix, ic].reshape(B, H, Q_CHUNK, n_keys, dh)
        pos = (ic[..., None] * MOBA_BLOCK + jnp.arange(MOBA_BLOCK)).reshape(B, H, Q_CHUNK, n_keys)
        mask_sel = jnp.repeat(okc, MOBA_BLOCK, axis=-1)
        bias_sel = tab_h[h_ix, rel_bucket(tq[:, None] - pos)]
        k_own = lax.dynamic_index_in_dim(kb, blk, axis=2, keepdims=False)
        v_own = lax.dynamic_index_in_dim(vb, blk, axis=2, keepdims=False)
        dist_own = tq[:, None] - (blk * MOBA_BLOCK + jnp.arange(MOBA_BLOCK))[None, :]
        bias_own = rel_tab[rel_bucket(dist_own)].transpose(2, 0, 1)
        l_sel = jnp.einsum('bhqd,bhqld->bhql', qc, kk).astype(jnp.float32) * scale + bias_sel
        l_own = jnp.einsum('bhqd,bhkd->bhqk', qc, k_own).astype(jnp.float32) * scale + bias_own
        logit = jnp.concatenate([l_sel, l_own], axis=-1)
        mask = jnp.concatenate([mask_sel, jnp.broadcast_to(dist_own >= 0, (B, H, Q_CHUNK, MOBA_BLOCK))], axis=-1)
        p = masked_softmax(logit, mask).astype(v.dtype)
        return (jnp.einsum('bhql,bhqld->bhqd', p[..., :n_keys], vv)
                + jnp.einsum('bhqk,bhkd->bhqd', p[..., n_keys:], v_own))

    o = lax.map(chunk, jnp.arange(S // Q_CHUNK))
    o = jnp.moveaxis(o, 0, 2).reshape(B, H, S, dh)
    return o.transpose(0, 2, 1, 3).reshape(B, S, H * dh)


def hier_moe(h, w_group, b_group, w_router, b_router, w_gate, w_up, w_down):
    B, S, D = h.shape
    T = B * S
    ht = h.reshape(T, D)
    g_prob = jax.nn.softmax((ht @ w_group).astype(jnp.float32) + b_group, axis=-1)
    g_val, g_idx = lax.top_k(g_prob, 1)
    e_logit = jnp.einsum('td,gde->tge', ht, w_router).astype(jnp.float32) + b_router
    e_logit = jnp.take_along_axis(e_logit, g_idx[:, :, None], axis=1)[:, 0]
    e_val, e_idx = lax.top_k(jax.nn.softmax(e_logit, axis=-1), EXPERT_TOPK)
    weights = g_val * e_val / jnp.sum(e_val, axis=-1, keepdims=True)
    expert = g_idx * EXPERTS_PER_GROUP + e_idx

    n_assign = T * EXPERT_TOPK
    flat_e = expert.reshape(-1)
    flat_tok = jnp.repeat(jnp.arange(T), EXPERT_TOPK)
    flat_w = weights.reshape(-1)
    order = jnp.argsort(flat_e)
    s_e, s_tok, s_w = flat_e[order], flat_tok[order], flat_w[order]
    counts = jnp.bincount(flat_e, length=N_EXPERTS)
    padded = (counts + MOE_ROW_BLOCK - 1) // MOE_ROW_BLOCK * MOE_ROW_BLOCK
    pad_end = jnp.cumsum(padded)
    pad_start = pad_end - padded
    start = jnp.cumsum(counts) - counts
    dest = pad_start[s_e] + jnp.arange(n_assign) - start[s_e]
    n_blocks = -(-(n_assign + N_EXPERTS * (MOE_ROW_BLOCK - 1)) // MOE_ROW_BLOCK)
    n_rows = n_blocks * MOE_ROW_BLOCK
    row_tok = jnp.zeros((n_rows,), jnp.int32).at[dest].set(s_tok)
    row_w = jnp.zeros((n_rows,), jnp.float32).at[dest].set(s_w)
    block_e = jnp.minimum(jnp.searchsorted(pad_end, jnp.arange(n_blocks) * MOE_ROW_BLOCK, side='right'), N_EXPERTS - 1)

    def run_block(args):
        tok, e = args
        xr = ht[tok]
        return (jax.nn.silu(xr @ w_gate[e]) * (xr @ w_up[e])) @ w_down[e]

    y = lax.map(run_block, (row_tok.reshape(n_blocks, MOE_ROW_BLOCK), block_e)).reshape(n_rows, D)
    y = (y.astype(jnp.float32) * row_w[:, None]).astype(h.dtype)
    out = jnp.zeros((T, D), h.dtype).at[row_tok].add(y)
    return out.reshape(B, S, D)


def setup_inputs(seed: int = 0) -> dict:
    key = jax.random.key(seed)
    ks = jax.random.split(key, 24)

    def nrm(k, shape, scale):
        return jax.random.normal(k, shape, jnp.float32) * scale

    dh = HEAD_DIM
    return {
        'x': nrm(ks[0], (BATCH, SEQ, D_MODEL), 1.0),
        'rel_bias': nrm(ks[1], (REL_BUCKETS, N_HEADS_TOTAL), 0.2),
        'norm_mix': 1.0 + nrm(ks[2], (DEPTH, D_MODEL), 0.01),
        'w_in': nrm(ks[3], (DEPTH, D_MODEL, IN_COLS), D_MODEL ** -0.5),
        'cmp_pe_k': nrm(ks[4], (DEPTH, CMP_BLOCK, dh), 0.1),
        'cmp_w1_k': nrm(ks[5], (DEPTH, CMP_BLOCK * dh, CMP_HIDDEN), (CMP_BLOCK * dh) ** -0.5),
        'cmp_w2_k': nrm(ks[6], (DEPTH, CMP_HIDDEN, dh), CMP_HIDDEN ** -0.5),
        'cmp_pe_v': nrm(ks[7], (DEPTH, CMP_BLOCK, dh), 0.1),
        'cmp_w1_v': nrm(ks[8], (DEPTH, CMP_BLOCK * dh, CMP_HIDDEN), (CMP_BLOCK * dh) ** -0.5),
        'cmp_w2_v': nrm(ks[9], (DEPTH, CMP_HIDDEN, dh), CMP_HIDDEN ** -0.5),
        'w_up_nsa': nrm(ks[10], (DEPTH, NSA_Q_COLS, D_MODEL), NSA_Q_COLS ** -0.5),
        'w_up_moba': nrm(ks[11], (DEPTH, MOBA_COLS, D_MODEL), MOBA_COLS ** -0.5),
        'w_out': nrm(ks[12], (DEPTH, D_MODEL, D_MODEL), D_MODEL ** -0.5),
        'norm_ffn': 1.0 + nrm(ks[13], (DEPTH, D_MODEL), 0.01),
        'w_group': nrm(ks[14], (DEPTH, D_MODEL, N_GROUPS), D_MODEL ** -0.5),
        'b_group': nrm(ks[15], (DEPTH, N_GROUPS), 0.01),
        'w_router': nrm(ks[16], (DEPTH, N_GROUPS, D_MODEL, EXPERTS_PER_GROUP), D_MODEL ** -0.5),
        'b_router': nrm(ks[17], (DEPTH, N_GROUPS, EXPERTS_PER_GROUP), 0.01),
        'w_exp_gate': nrm(ks[18], (DEPTH, N_EXPERTS, D_MODEL, EXPERT_FF), D_MODEL ** -0.5),
        'w_exp_up': nrm(ks[19], (DEPTH, N_EXPERTS, D_MODEL, EXPERT_FF), D_MODEL ** -0.5),
        'w_exp_down': nrm(ks[20], (DEPTH, N_EXPERTS, EXPERT_FF, D_MODEL), EXPERT_FF ** -0.5),
        'final_norm': 1.0 + nrm(ks[21], (D_MODEL,), 0.01),
    }


def reference(x, rel_bias, norm_mix, w_in, cmp_pe_k, cmp_w1_k, cmp_w2_k, cmp_pe_v, cmp_w1_v, cmp_w2_v,
              w_up_nsa, w_up_moba, w_out, norm_ffn, w_group, b_group, w_router, b_router,
              w_exp_gate, w_exp_up, w_exp_down, final_norm):
    B, S, _ = x.shape
    split_at = [int(v) for v in np.cumsum(IN_SPLITS)[:-1]]
    tab_a = rel_bias[:, :NSA_HEADS]
    tab_b = rel_bias[:, NSA_HEADS:]

    def heads(a, n):
        return a.reshape(B, S, n, HEAD_DIM)

    for l in range(DEPTH):
        h = rmsnorm(x, norm_mix[l])
        (q_a, kc, vc, ksl, vsl, kw, vw, gate_a,
         q_b, k_b, v_b, gm_a, gm_b) = jnp.split(h @ w_in[l], split_at, axis=-1)
        o_a = nsa_mixer(heads(q_a, NSA_HEADS), heads(kc, NSA_KV_HEADS), heads(vc, NSA_KV_HEADS),
                        heads(ksl, NSA_KV_HEADS), heads(vsl, NSA_KV_HEADS),
                        heads(kw, NSA_KV_HEADS), heads(vw, NSA_KV_HEADS), gate_a,
                        cmp_pe_k[l], cmp_w1_k[l], cmp_w2_k[l], cmp_pe_v[l], cmp_w1_v[l], cmp_w2_v[l], tab_a)
        o_b = moba_mixer(heads(q_b, MOBA_HEADS), heads(k_b, MOBA_HEADS), heads(v_b, MOBA_HEADS), tab_b)
        merged = jax.nn.sigmoid(gm_a) * (o_a @ w_up_nsa[l]) + jax.nn.sigmoid(gm_b) * (o_b @ w_up_moba[l])
        x = x + merged @ w_out[l]
        x = x + hier_moe(rmsnorm(x, norm_ffn[l]), w_group[l], b_group[l], w_router[l], b_router[l],
                         w_exp_gate[l], w_exp_up[l], w_exp_down[l])
    return rmsnorm(x, final_norm)
```

```python
import contextlib
import math
import numpy as np
import ml_dtypes
import concourse.bass as bass
import concourse.mybir as mybir
from concourse.bass_utils import run_bass_kernel_spmd

F32 = mybir.dt.float32
BF16 = mybir.dt.bfloat16
U8 = mybir.dt.uint8
AF = mybir.ActivationFunctionType
ALU = mybir.AluOpType
AX = mybir.AxisListType
NEG = -30000.0


class Buf:
    __slots__ = ("name", "w", "r")

    def __init__(self, name=""):
        self.name = name
        self.w = None
        self.r = []


class Op:
    __slots__ = ("eng", "fns", "deps", "signal", "sval", "dsem", "dtgt")


class Sched:
    ENGS = ("sp", "act", "dve", "pool", "pe")

    def __init__(self, nc):
        self.nc = nc
        self.ops = {e: [] for e in self.ENGS}
        self.dkeys = {}
        self.dlast = {}
        self.pending = {e: [] for e in self.ENGS}
        self.final = []

    def _dep(self, op, w):
        if w is None or w is op:
            return
        if w.eng == "pe" and op.eng == "pe":
            return
        op.deps.append(w)
        if w.dsem is None:
            w.signal = True

    def add(self, eng, fns, reads=(), writes=(), dkey=None, extra=()):
        op = Op()
        op.eng = eng
        op.fns = fns if isinstance(fns, (list, tuple)) else [fns]
        op.deps = []
        op.signal = False
        op.sval = None
        op.dsem = None
        op.dtgt = None
        if dkey is not None:
            ent = self.dkeys.setdefault(dkey, [0])
            ent[0] += 16 * len(op.fns)
            op.dsem = dkey
            op.dtgt = ent[0]
            self.dlast[dkey] = op
        for b in reads:
            self._dep(op, b.w)
        for b in writes:
            self._dep(op, b.w)
            for r in b.r:
                self._dep(op, r)
        for w in self.pending[eng]:
            self._dep(op, w)
        self.pending[eng] = []
        for w in extra:
            self._dep(op, w)
        for b in writes:
            b.w = op
            b.r = []
        for b in reads:
            if b.w is not op:
                b.r.append(op)
        self.ops[eng].append(op)
        return op

    def barrier(self):
        lasts = [self.ops[e][-1] for e in self.ENGS if self.ops[e] and self.ops[e][-1].dsem != "cv"]
        lasts += [op for k, op in self.dlast.items() if k != "cv"]
        for e in self.ENGS:
            self.pending[e] = self.pending[e] + lasts

    def emit(self, stack):
        nc = self.nc
        sems = {e: stack.enter_context(nc.semaphore("s_" + e)) for e in self.ENGS}
        dsems = {k: stack.enter_context(nc.semaphore("d_" + str(k))) for k in self.dkeys}
        for e in self.ENGS:
            c = 0
            for op in self.ops[e]:
                if op.dsem is None and op.signal:
                    c += 1
                    op.sval = c
        block = stack.enter_context(nc.Block())
        engmap = {"sp": block.sync, "act": block.scalar, "dve": block.vector, "pool": block.gpsimd,
                  "pe": block.tensor}

        def make(e):
            def body(eng):
                known = {}

                def waits(deps):
                    need = {}
                    for w in deps:
                        if w.dsem is not None:
                            key, val = ("d", w.dsem), w.dtgt
                        else:
                            key, val = ("e", w.eng), w.sval
                        if need.get(key, 0) < val:
                            need[key] = val
                    for key, val in need.items():
                        if known.get(key, 0) >= val:
                            continue
                        known[key] = val
                        eng.wait_ge(dsems[key[1]] if key[0] == "d" else sems[key[1]], val)

                for op in self.ops[e]:
                    waits(op.deps)
                    last = None
                    for fn in op.fns:
                        last = fn(eng)
                        if op.dsem is not None:
                            last.then_inc(dsems[op.dsem], 16)
                    if op.dsem is None and op.signal:
                        last.then_inc(sems[e], 1)
                if e == "sp":
                    waits(self.final)
            return body

        for e in self.ENGS:
            if self.ops[e] or e == "sp":
                engmap[e](make(e))


class Pool:
    def __init__(self, nc, nbytes):
        self.t = nc.alloc_sbuf_tensor("pool", [128, nbytes], U8)
        self.nbytes = nbytes

    def at(self, off, cols, dt, parts=128):
        sz = cols * (4 if dt == F32 else 2)
        assert off % 32 == 0 and off + sz <= self.nbytes, (off, sz, self.nbytes)
        return self.t[0:parts, off:off + sz].bitcast(dt)


def rel_bucket_np(dist):
    n = np.maximum(dist.astype(np.int64), 0)
    nf = np.maximum(n, 1).astype(np.float32)
    large = 16 + (np.log(nf / np.float32(16)) / np.float32(math.log(8.0)) * np.float32(16)).astype(np.int32)
    return np.where(n < 16, n, np.minimum(large, 31)).astype(np.int64)


_IN_OFF = dict(qa=0, kc=1024, vc=1280, ksl=1536, vsl=1792, kw=2048, vw=2304, gate=2560, qb=2584, kb=3608,
               vb=4632, gma=5656, gmb=7704)
KB = 1024
POOLB = 206 * KB


def build(stop=None):
    nc = bass.Bass("TRN2", target_bir_lowering=False)

    declared = set()

    def din(name, shape, dt=F32):
        declared.add(name)
        return nc.dram_tensor(name, list(shape), dt, kind="ExternalInput").ap()

    def dscr(name, shape, dt=BF16):
        return nc.dram_tensor(name, list(shape), dt).ap()

    xb = din("xb", [4096, 2048])
    xo = din("xo", [1024, 2048])
    w_in = din("w_in", [2048, 9752])
    norm_mix = din("norm_mix", [1, 2048])
    norm_ffn = din("norm_ffn", [1, 2048])
    final_norm = din("final_norm", [1, 2048])
    peT_k = din("peT_k", [128, 32])
    peT_v = din("peT_v", [128, 32])
    w1_k = din("w1_k", [4096, 256])
    w1_v = din("w1_v", [4096, 256])
    w2_k = din("w2_k", [256, 128])
    w2_v = din("w2_v", [256, 128])
    w_up_a = din("w_up_a", [1024, 2048])
    w_up_b = din("w_up_b", [1024, 2048])
    w_out = din("w_out", [2048, 2048])
    w_rt = din("w_rt", [2048, 72])
    b_rt = din("b_rt", [1, 72])
    if stop is None:
        w_eg = din("w_eg", [64, 2048, 512])
        w_eu = din("w_eu", [64, 2048, 512])
        w_ed = din("w_ed", [64, 512, 2048])
    biasC = din("biasC", [8, 256, 1024])
    bdiag = din("bdiag", [16, 128, 5 * 128])
    bwin = din("bwin", [8, 128, 8 * 128])
    crep = din("crep", [128, 16])
    fsel = din("fsel", [128, 8 * 64])
    pmneg = din("pmneg", [128, 8 * 16])
    past01 = din("past01", [128, 8 * 16])
    own01 = din("own01", [128, 8 * 16])
    c_ident = din("c_ident", [128, 128], BF16)
    c_ones = din("c_ones", [128, 128], BF16)
    c_ovl = din("c_ovl", [128, 2 * 65], BF16)
    c_e64 = din("c_e64", [64, 32 * 128], BF16)
    c_e16 = din("c_e16", [16, 32 * 128], BF16)
    c_sel24 = din("c_sel24", [24, 24 * 128], BF16)
    c_ustr = din("c_ustr", [128, 128], BF16)
    c_iota = din("c_iota", [128, 128])
    out = nc.dram_tensor("out", [1024, 2048], F32, kind="ExternalOutput").ap()

    FT = dscr("FT", [16, 128, 4096])
    TM = dscr("TM", [12, 128, 4096])
    QT = dscr("QT", [16, 128, 1024])
    GM = dscr("GM", [32, 128, 1024])
    dbg = {}
    dbg['_declared'] = declared

    def dout(name, shape):
        dbg[name] = nc.dram_tensor(name, list(shape), F32, kind="ExternalOutput").ap()
        return dbg[name]

    P = Pool(nc, POOLB)
    S = Sched(nc)
    banks = [nc.alloc_psum_tensor("pb%d" % i, [128, 512], F32) for i in range(8)]
    bbuf = [Buf("pb%d" % i) for i in range(8)]

    def bank(i):
        return banks[i][:, :]

    def bank16(i):
        return banks[i][:, :].bitcast(BF16)

    PB = 200 * KB
    ident = P.at(PB, 128, BF16)
    ones = P.at(PB + 256, 128, BF16)
    gT = P.at(PB + 512, 1024, BF16, parts=24)
    sstat = P.at(PB + 2560, 64, F32)
    b_const = Buf("const")
    b_gT = Buf("gT")
    S.add("sp", [lambda e: e.dma_start(out=ident, in_=c_ident), lambda e: e.dma_start(out=ones, in_=c_ones)],
          writes=[b_const], dkey="const")

    evac_rr = [0]

    def evac(out_ap, in_ap, reads, writes, scale=None):
        evac_rr[0] ^= 1
        if evac_rr[0]:
            if scale is None:
                return S.add("act", lambda e: e.activation(out=out_ap, in_=in_ap, func=AF.Copy), reads=reads, writes=writes)
            return S.add("act", lambda e: e.activation(out=out_ap, in_=in_ap, func=AF.Copy, scale=scale), reads=reads, writes=writes)
        if scale is None:
            return S.add("dve", lambda e: e.tensor_copy(out=out_ap, in_=in_ap), reads=reads, writes=writes)
        return S.add("dve", lambda e: e.tensor_scalar(out=out_ap, in0=in_ap, scalar1=scale, scalar2=None, op0=ALU.mult),
                     reads=reads, writes=writes)

    hTb = P.at(0, 16 * 4096, BF16).rearrange("p (k t) -> p k t", k=16)
    hTo = P.at(128 * KB, 16 * 1024, BF16).rearrange("p (k t) -> p k t", k=16)
    b_hTb = [Buf() for _ in range(32)]
    b_hTo = [Buf() for _ in range(8)]
    R0 = 160 * KB

    def norm_phase(srcs, gvec_dram, region, emit_tile):
        xs = [P.at(region + i * 8 * KB, 2048, F32) for i in range(2)]
        bxs = [Buf() for _ in range(2)]
        gt = P.at(region + 16 * KB, 2048, F32)
        b_gt = Buf()
        hn = [P.at(region + 24 * KB + i * 4 * KB, 2048, BF16) for i in range(2)]
        b_hn = [Buf() for _ in range(2)]
        junk = P.at(region + 32 * KB, 2048, BF16)
        b_junk = Buf()
        S.add("sp", lambda e: e.dma_start(out=gt, in_=gvec_dram.partition_broadcast(128)), writes=[b_gt], dkey="ld_g")
        for i, src in enumerate(srcs):
            s = i % 2
            st = sstat[:, 4 * s:4 * s + 4]
            S.add("sp", lambda e, src=src, s=s: e.dma_start(out=xs[s], in_=src), writes=[bxs[s]], dkey="ld_x%d" % s)
            S.add("act", lambda e, s=s, st=st: e.activation(out=junk, in_=xs[s], func=AF.Square, accum_out=st[:, 0:1]),
                  reads=[bxs[s]], writes=[b_junk, b_st[s]])
            S.add("dve", lambda e, st=st: e.tensor_scalar(out=st[:, 1:2], in0=st[:, 0:1], scalar1=1.0 / 2048, scalar2=1e-6,
                                                          op0=ALU.mult, op1=ALU.add), reads=[b_st[s]], writes=[b_st[s]])
            S.add("act", lambda e, st=st: e.activation(out=st[:, 2:3], in_=st[:, 1:2], func=AF.Sqrt), reads=[b_st[s]], writes=[b_st[s]])
            S.add("dve", lambda e, st=st: e.reciprocal(out=st[:, 3:4], in_=st[:, 2:3]), reads=[b_st[s]], writes=[b_st[s]])
            S.add("dve", lambda e, s=s, st=st: e.scalar_tensor_tensor(out=hn[s], in0=xs[s], scalar=st[:, 3:4], in1=gt,
                                                                     op0=ALU.mult, op1=ALU.mult),
                  reads=[bxs[s], b_st[s], b_gt], writes=[b_hn[s]])
            emit_tile(i, hn[s], b_hn[s], xs[s], bxs[s], st[:, 3:4], b_st[s])

    b_st = [Buf(), Buf()]

    def transpose_tile(hn_ap, b_hn, dst, b_dst, tcol):
        for half in range(2):
            bk = half
            for q in range(8):
                kc = half * 8 + q
                S.add("pe", lambda e, kc=kc, q=q, bk=bk: e.transpose(out=bank16(bk)[:, q * 128:(q + 1) * 128],
                                                                    in_=hn_ap[:, kc * 128:(kc + 1) * 128], identity=ident),
                      reads=[b_hn, b_const], writes=[bbuf[bk]])
            o = dst[:, half * 8:(half + 1) * 8, tcol:tcol + 128]
            i_ = bank16(bk).rearrange("p (k t) -> p k t", k=8)
            evac(o, i_, [bbuf[bk]], [b_dst])

    srcsA = [xb[t * 128:(t + 1) * 128, :] for t in range(32)] + [xo[t * 128:(t + 1) * 128, :] for t in range(8)]

    def emitA(i, hn_ap, b_hn, x_ap, b_x, rstd, bst):
        if i < 32:
            transpose_tile(hn_ap, b_hn, hTb, b_hTb[i], i * 128)
        else:
            transpose_tile(hn_ap, b_hn, hTo, b_hTo[i - 32], (i - 32) * 128)

    norm_phase(srcsA, norm_mix, R0, emitA)
    S.barrier()

    def dump_bf16(name, ap, n, parts=128):
        d = dout(name, [parts, n])
        tmp = P.at(160 * KB, n, F32, parts=parts)
        bt = Buf()
        S.add("dve", lambda e: e.tensor_copy(out=tmp, in_=ap), writes=[bt])
        S.final.append(S.add("sp", lambda e: e.dma_start(out=d, in_=tmp), reads=[bt], dkey="st_dbg"))

    if stop == "A":
        dump_bf16("d_hTb", hTb[:, 3, 0:2048], 2048)
        S.barrier()
        dump_bf16("d_hTo", hTo[:, 5, 0:1024], 1024)
        return nc, S, dbg

    wts = [P.at(R0 + i * 8 * KB, 16 * 256, BF16).rearrange("p (k c) -> p k c", k=16) for i in range(2)]
    b_wt = [Buf(), Buf()]
    stg = [P.at(R0 + 16 * KB + i * 8 * KB, 4096, BF16) for i in range(3)]
    b_stg = [Buf() for _ in range(3)]
    wslot = [0]
    sslot = [0]
    pbank = [0]

    def load_w(col0, ncols):
        s = wslot[0] % 2
        wslot[0] += 1
        src = w_in[:, col0:col0 + ncols].rearrange("(k p) c -> p k c", p=128)
        S.add("pool", lambda e: e.dma_start(out=wts[s][:, :, 0:ncols], in_=src), writes=[b_wt[s]], dkey="ld_w%d" % s)
        return wts[s], b_wt[s]

    def next_bank(lo=2, n=6):
        b = lo + pbank[0] % n
        pbank[0] += 1
        return b

    def next_stg():
        s = sslot[0] % 3
        sslot[0] += 1
        return s

    def proj_fm(wt, bw, c0, hT, b_hT, ntok, dst_dram, func=None, scale=None, tm=False):
        s = next_stg()
        for ch in range(ntok // 512):
            bk = next_bank()
            rb = [bw] + b_hT[ch * 4:(ch + 1) * 4]
            for kc in range(16):
                S.add("pe", lambda e, kc=kc, bk=bk, ch=ch: e.matmul(bank(bk), lhsT=wt[:, kc, c0:c0 + 128],
                                                                   rhs=hT[:, kc, ch * 512:(ch + 1) * 512],
                                                                   start=(kc == 0), stop=(kc == 15)),
                      reads=rb, writes=[bbuf[bk]])
            o = stg[s][:, ch * 512:(ch + 1) * 512]
            if func is None:
                evac(o, bank(bk), [bbuf[bk]], [b_stg[s]], scale=scale)
            else:
                S.add("act", lambda e, o=o, bk=bk: e.activation(out=o, in_=bank(bk), func=func), reads=[bbuf[bk]], writes=[b_stg[s]])
        if not tm:
            S.add("sp", lambda e: e.dma_start(out=dst_dram, in_=stg[s][:, 0:ntok]), reads=[b_stg[s]], dkey="st_stg%d" % s)
            return
        s2 = next_stg()
        for g8 in range(ntok // 1024):
            bk = next_bank()
            for q in range(8):
                t = g8 * 8 + q
                S.add("pe", lambda e, q=q, t=t, bk=bk: e.transpose(out=bank16(bk)[:, q * 128:(q + 1) * 128],
                                                                  in_=stg[s][:, t * 128:(t + 1) * 128], identity=ident),
                      reads=[b_stg[s], b_const], writes=[bbuf[bk]])
            evac(stg[s2][:, g8 * 1024:(g8 + 1) * 1024], bank16(bk), [bbuf[bk]], [b_stg[s2]])
        S.add("sp", lambda e: e.dma_start(out=dst_dram, in_=stg[s2][:, 0:ntok]), reads=[b_stg[s2]], dkey="st_stg%d" % s2)

    def pairs(base, n):
        return [(base + 256 * i) for i in range(n // 2)]

    ft_cols = [_IN_OFF["kc"], _IN_OFF["vc"], _IN_OFF["ksl"], _IN_OFF["kw"]] + pairs(_IN_OFF["kb"], 8)
    for pi, col0 in enumerate(ft_cols):
        if stop == "B3s":
            break
        wt, bw = load_w(col0, 256)
        for hh in range(2):
            proj_fm(wt, bw, hh * 128, hTb, b_hTb, 4096, FT[2 * pi + hh])
        if stop == "B1":
            break
    if stop == "B1":
        S.barrier()
        for i in range(2):
            a = P.at(176 * KB, 4096, BF16)
            bt = Buf()
            S.add("sp", lambda e, i=i, a=a: e.dma_start(out=a, in_=FT[i]), writes=[bt], dkey="ld_dbg")
            S.barrier()
            dump_bf16("d_FT%d" % i, a[:, 0:2048], 2048)
            S.barrier()
        return nc, S, dbg

    def dbg_exit(items):
        S.barrier()
        bt = Buf()
        for name, scr, i, w in items:
            a = P.at(32 * KB, w, BF16)
            S.add("sp", lambda e, a=a, scr=scr, i=i: e.dma_start(out=a, in_=scr[i]), writes=[bt], dkey="ld_dbg")
            S.barrier()
            dump_bf16("%s%d" % (name, i), a[:, 0:1024], 1024)
            S.barrier()
        return nc, S, dbg
    if stop == "B2":
        return dbg_exit([("d_FT", FT, 3, 4096), ("d_FT", FT, 15, 4096)])
    tm_cols = [_IN_OFF["vsl"], _IN_OFF["vw"]] + pairs(_IN_OFF["vb"], 8)
    def proj_tm(pi, wt, bw):
        s0, s1 = next_stg(), next_stg()
        for t2 in range(16):
            bk = next_bank()
            for tt in range(2):
                t = 2 * t2 + tt
                for kc in range(16):
                    S.add("pe", lambda e, kc=kc, bk=bk, t=t, tt=tt: e.matmul(bank(bk)[:, tt * 256:(tt + 1) * 256],
                                                                            lhsT=hTb[:, kc, t * 128:(t + 1) * 128],
                                                                            rhs=wt[:, kc, 0:256], start=(kc == 0), stop=(kc == 15)),
                          reads=[bw, b_hTb[t]], writes=[bbuf[bk]])
            for hh, s in ((0, s0), (1, s1)):
                o = stg[s][:, t2 * 256:(t2 + 1) * 256].rearrange("p (a d) -> p a d", a=2)
                i_ = bank(bk).rearrange("p (a h d) -> p a h d", a=2, h=2)[:, :, hh, :]
                evac(o, i_, [bbuf[bk]], [b_stg[s]])
        for hh, s in ((0, s0), (1, s1)):
            S.add("sp", lambda e, s=s, hh=hh, pi=pi: e.dma_start(out=TM[2 * pi + hh], in_=stg[s]), reads=[b_stg[s]],
                  dkey="st_stg%d" % s)

    for pi, col0 in enumerate(tm_cols):
        wt_, bw_ = load_w(col0, 256)
        for hh in range(2):
            proj_fm(wt_, bw_, hh * 128, hTb, b_hTb, 4096, TM[2 * pi + hh], tm=True)
        if stop in ("B3a", "B3s"):
            return dbg_exit([("d_TM", TM, 0, 4096), ("d_TM", TM, 1, 4096)])
    if stop == "B3":
        return dbg_exit([("d_TM", TM, 0, 4096), ("d_TM", TM, 11, 4096)])
    qs = 1.0 / math.sqrt(128.0)
    for pi, col0 in enumerate(pairs(_IN_OFF["qa"], 8) + pairs(_IN_OFF["qb"], 8)):
        wt, bw = load_w(col0, 256)
        for hh in range(2):
            proj_fm(wt, bw, hh * 128, hTo, b_hTo, 1024, QT[2 * pi + hh], scale=qs)
    if stop == "C1":
        return dbg_exit([("d_QT", QT, 0, 1024), ("d_QT", QT, 15, 1024)])
    for pi, col0 in enumerate(pairs(_IN_OFF["gma"], 16) + pairs(_IN_OFF["gmb"], 16)):
        wt, bw = load_w(col0, 256)
        for hh in range(2):
            proj_fm(wt, bw, hh * 128, hTo, b_hTo, 1024, GM[2 * pi + hh], func=AF.Sigmoid)
    def proj_gate(wt, bw):
      for ch in range(2):
        bk = next_bank()
        for kc in range(16):
            S.add("pe", lambda e, kc=kc, bk=bk, ch=ch: e.matmul(bank(bk)[0:24, :], lhsT=wt[:, kc, 0:24],
                                                               rhs=hTo[:, kc, ch * 512:(ch + 1) * 512],
                                                               start=(kc == 0), stop=(kc == 15)),
                  reads=[bw] + b_hTo[ch * 4:(ch + 1) * 4], writes=[bbuf[bk]])
        S.add("act", lambda e, bk=bk, ch=ch: e.activation(out=gT[:, ch * 512:(ch + 1) * 512], in_=bank(bk)[0:24, :],
                                                         func=AF.Sigmoid), reads=[bbuf[bk]], writes=[b_gT])

    if stop == "C2":
        return dbg_exit([("d_GM", GM, 0, 1024), ("d_GM", GM, 31, 1024)])
    wt_, bw_ = load_w(_IN_OFF["gate"], 24)
    proj_gate(wt_, bw_)
    S.barrier()

    if stop == "C":
        d1 = dout("d_gT", [24, 1024])
        tmp = P.at(0, 1024, F32, parts=24)
        bt = Buf()
        S.add("dve", lambda e: e.tensor_copy(out=tmp, in_=gT), reads=[b_gT], writes=[bt])
        S.final.append(S.add("sp", lambda e: e.dma_start(out=d1, in_=tmp), reads=[bt], dkey="st_dbg"))
        for name, scr, idxs, w in (("d_TM", TM, (0, 5, 11), 4096), ("d_QT", QT, (0, 9, 15), 1024), ("d_GM", GM, (0, 17, 31), 1024)):
            for i in idxs:
                a = P.at(32 * KB, w, BF16)
                S.add("sp", lambda e, a=a, scr=scr, i=i: e.dma_start(out=a, in_=scr[i]), writes=[bt], dkey="ld_dbg")
                S.barrier()
                dump_bf16("%s%d" % (name, i), a[:, 0:1024], 1024)
                S.barrier()
        S.final.append(S.add("sp", lambda e: e.dma_start(out=out[0:128, :], in_=P.at(0, 2048, F32)), reads=[bt], dkey="st_dbg"))
        return nc, S, dbg

    NCV = 28 if stop is None else 0
    CV0 = 64 - NCV
    cv_chunks = []
    cv_last = [None]
    if NCV:
        WBg = dscr("WBg", [NCV, 512, 2048])
        WBu = dscr("WBu", [NCV, 512, 2048])
        WBd = dscr("WBd", [NCV, 512, 2048])
        for ex in range(CV0, 64):
            for src_t, dst_t in ((w_eg, WBg), (w_eu, WBu), (w_ed, WBd)):
                srcv = src_t[ex].rearrange("a b -> (a b)").rearrange("(r c) -> r c", c=2048)
                for c4 in range(4):
                    cv_chunks.append((srcv[c4 * 128:(c4 + 1) * 128, :], dst_t[ex - CV0][c4 * 128:(c4 + 1) * 128, :]))
    cv_pos = [0]

    def issue_cv(n, paced=True):
        extra = [S.ops["pe"][-1]] if paced else []
        for _ in range(n):
            if cv_pos[0] >= len(cv_chunks):
                return
            src, dst = cv_chunks[cv_pos[0]]
            cv_pos[0] += 1
            cv_last[0] = S.add("pool", lambda e, src=src, dst=dst: e.dma_start(out=dst, in_=src), dkey="cv", extra=extra)

    def cast_load(dst, src, writes, dkey):
        return S.add("pool", lambda e: e.dma_start(out=dst, in_=src), writes=writes, dkey=dkey)

    def sp_load(dst, src, writes, dkey):
        return S.add("sp", lambda e: e.dma_start(out=dst, in_=src), writes=writes, dkey=dkey)

    oaf = P.at(0, 8 * 1024, F32).rearrange("p (h t) -> p h t", h=8)
    obT = P.at(32 * KB, 8 * 1024, BF16).rearrange("p (h t) -> p h t", h=8)
    oaT = P.at(48 * KB, 8 * 1024, BF16).rearrange("p (h t) -> p h t", h=8)
    b_oaf = [Buf() for _ in range(8)]
    b_obT = [Buf() for _ in range(8)]
    b_oaT = Buf()
    selbT = P.at(64 * KB, 2 * 1024, BF16, parts=64).rearrange("p (g t) -> p g t", g=2)
    b_selbT = [Buf(), Buf()]
    selbTm = P.at(68 * KB, 1024, BF16, parts=16)
    b_selbTm = Buf()
    kcmpT = P.at(70 * KB, 2 * 256, BF16).rearrange("p (g c) -> p g c", g=2)
    vcmp = P.at(71 * KB, 2 * 256, BF16).rearrange("p (g c d) -> p g c d", g=2, c=2)
    b_kcmp = [Buf(), Buf()]
    b_vcmp = [Buf(), Buf()]
    psel = P.at(72 * KB, 512, F32).rearrange("p (t n) -> p t n", t=8)
    b_psel = Buf()
    e64 = P.at(74 * KB, 4096, BF16, parts=64).rearrange("p (k s) -> p k s", k=32)
    e16 = P.at(82 * KB, 4096, BF16, parts=16).rearrange("p (k s) -> p k s", k=32)
    sel24 = P.at(90 * KB, 3072, BF16, parts=24).rearrange("p (i s) -> p i s", i=24)
    ovl = P.at(96 * KB, 130, BF16).rearrange("p (c n) -> p c n", c=2)
    fselT = P.at(97 * KB, 512, F32).rearrange("p (t n) -> p t n", t=8)
    pmn = P.at(99 * KB, 128, F32).rearrange("p (t n) -> p t n", t=8)
    p01 = P.at(99 * KB + 512, 128, F32).rearrange("p (t n) -> p t n", t=8)
    o01 = P.at(100 * KB, 128, F32).rearrange("p (t n) -> p t n", t=8)
    crp = P.at(100 * KB + 512, 16, F32)
    b_c2 = Buf()
    S.add("sp", [lambda e: e.dma_start(out=P.at(74 * KB, 4096, BF16, parts=64), in_=c_e64),
                 lambda e: e.dma_start(out=P.at(82 * KB, 4096, BF16, parts=16), in_=c_e16),
                 lambda e: e.dma_start(out=P.at(90 * KB, 3072, BF16, parts=24), in_=c_sel24),
                 lambda e: e.dma_start(out=P.at(96 * KB, 130, BF16), in_=c_ovl),
                 lambda e: e.dma_start(out=P.at(97 * KB, 512, F32), in_=fsel),
                 lambda e: e.dma_start(out=P.at(99 * KB, 128, F32), in_=pmneg),
                 lambda e: e.dma_start(out=P.at(99 * KB + 512, 128, F32), in_=past01),
                 lambda e: e.dma_start(out=P.at(100 * KB, 128, F32), in_=own01),
                 lambda e: e.dma_start(out=crp, in_=crep)], writes=[b_c2], dkey="const2")

    KTs = [P.at(104 * KB + i * 8 * KB, 4096, BF16) for i in range(2)]
    Vs = [P.at(120 * KB + i * 8 * KB, 4096, BF16).rearrange("p (t d) -> p t d", t=32) for i in range(2)]
    Vs_flat = [P.at(120 * KB + i * 8 * KB, 4096, BF16) for i in range(2)]
    QTs = [P.at(136 * KB + i * 2 * KB, 1024, BF16) for i in range(2)]
    b_KT = [Buf(), Buf()]
    b_V = [Buf(), Buf()]
    b_QT = [Buf(), Buf()]
    biasCb = P.at(140 * KB, 2048, BF16).rearrange("p (c q) -> p c q", c=2)
    b_biasC = Buf()
    bdraw = P.at(144 * KB, 640, F32)
    bdb = P.at(147 * KB, 640, BF16).rearrange("p (v q) -> p v q", v=5)
    bdb_flat = P.at(147 * KB, 640, BF16)
    b_bdraw, b_bdb = Buf(), Buf()
    bwb = P.at(149 * KB, 1024, BF16).rearrange("p (v q) -> p v q", v=8)
    bwb_flat = P.at(149 * KB, 1024, BF16)
    b_bwb = Buf()
    PTs = [P.at(152 * KB + i * KB, 512, BF16) for i in range(3)]
    b_PT = [Buf() for _ in range(3)]
    rden = P.at(155 * KB, 512, F32)
    tmpf = P.at(157 * KB, 512, F32)
    b_rden, b_tmpf = Buf(), Buf()
    w1b = P.at(160 * KB, 32 * 256, BF16).rearrange("p (l h) -> p l h", l=32)
    w1b_flat = P.at(160 * KB, 32 * 256, BF16)
    w2b = P.at(176 * KB, 256, BF16).rearrange("p (c d) -> p c d", c=2)
    w2b_flat = P.at(176 * KB, 256, BF16)
    peb = P.at(176 * KB + 512, 32, BF16)
    hid = P.at(177 * KB, 512, BF16).rearrange("p (c n) -> p c n", c=2)
    zf = P.at(178 * KB, 256, F32)
    uf = P.at(179 * KB, 256, F32)
    sgf = P.at(180 * KB, 256, F32)
    bh = P.at(181 * KB, 8, F32)
    b_w1, b_w2, b_pe, b_hid, b_z, b_u, b_sg, b_bh = [Buf() for _ in range(8)]
    sc = P.at(182 * KB, 64, F32)
    sc2 = P.at(182 * KB + 256, 64, F32)
    m8a = P.at(182 * KB + 512, 8, F32)
    m8b = P.at(182 * KB + 576, 8, F32)
    selm = P.at(183 * KB, 64, F32)
    selb = P.at(183 * KB + 256, 64, BF16)
    km = P.at(184 * KB, 16, F32)
    kmT = P.at(184 * KB + 64, 16, BF16)
    rd1 = P.at(184 * KB + 128, 8, F32)
    b_sc, b_km, b_rd1 = Buf(), Buf(), Buf()
    lrot = [0]
    ptrot = [0]

    def Lbank():
        b = lrot[0] % 4
        lrot[0] += 1
        return b

    def PTslot():
        s = ptrot[0] % 3
        ptrot[0] += 1
        return s

    def mm(outap, lhsT, rhs, start, stop, reads, bk):
        S.add("pe", lambda e: e.matmul(outap, lhsT=lhsT, rhs=rhs, start=start, stop=stop), reads=reads, writes=[bbuf[bk]])

    def load_head(slot, ft_idx, tm_idx, q_idx):
        if ft_idx is not None:
            sp_load(KTs[slot], FT[ft_idx], [b_KT[slot]], "ld_KT%d" % slot)
        if tm_idx is not None:
            sp_load(Vs_flat[slot], TM[tm_idx], [b_V[slot]], "ld_V%d" % slot)
        if q_idx is not None:
            sp_load(QTs[slot], QT[q_idx], [b_QT[slot]], "ld_Q%d" % slot)

    def finalize_nsa(h, cq, gi, first, bo=4, bd_=5):
        ch = slice(cq * 512, (cq + 1) * 512)
        gb = Lbank()
        S.add("dve", lambda e: e.tensor_scalar(out=rden, in0=bank(bd_), scalar1=1e-30, scalar2=None, op0=ALU.max),
              reads=[bbuf[bd_]], writes=[b_rden])
        S.add("dve", lambda e: e.reciprocal(out=rden, in_=rden), reads=[b_rden], writes=[b_rden])
        mm(bank(gb), sel24[:, gi, :], gT[:, ch], True, True, [b_c2, b_gT], gb)
        S.add("dve", lambda e: e.tensor_tensor(out=tmpf, in0=bank(bo), in1=rden, op=ALU.mult), reads=[bbuf[bo], b_rden], writes=[b_tmpf])
        if first:
            S.add("dve", lambda e: e.tensor_tensor(out=oaf[:, h, ch], in0=bank(gb), in1=tmpf, op=ALU.mult),
                  reads=[bbuf[gb], b_tmpf], writes=[b_oaf[h]])
        else:
            S.add("dve", lambda e: e.tensor_tensor(out=tmpf, in0=bank(gb), in1=tmpf, op=ALU.mult), reads=[bbuf[gb], b_tmpf], writes=[b_tmpf])
            S.add("dve", lambda e: e.tensor_tensor(out=oaf[:, h, ch], in0=oaf[:, h, ch], in1=tmpf, op=ALU.add),
                  reads=[b_tmpf, b_oaf[h]], writes=[b_oaf[h]])

    def finalize_moba(h, cq, bo=4, bd_=5):
        ch = slice(cq * 512, (cq + 1) * 512)
        S.add("dve", lambda e: e.tensor_scalar(out=rden, in0=bank(bd_), scalar1=1e-30, scalar2=None, op0=ALU.max),
              reads=[bbuf[bd_]], writes=[b_rden])
        S.add("dve", lambda e: e.reciprocal(out=rden, in_=rden), reads=[b_rden], writes=[b_rden])
        S.add("dve", lambda e: e.tensor_tensor(out=obT[:, h, ch], in0=bank(bo), in1=rden, op=ALU.mult),
              reads=[bbuf[bo], b_rden], writes=[b_obT[h]])

    odrot = [0]

    def run_pipe(stages, lag=2):
        n = len(stages)
        for i in range(min(lag, n)):
            stages[i][0]()
        for i in range(n):
            if i + lag < n:
                stages[i + lag][0]()
            stages[i][1]()

    def attn_causal(ks, vs, qs_, selT, b_sel, E, fin):
        KTa, Va, QTa = KTs[ks], Vs[vs], QTs[qs_]
        stages = []
        for cq in range(2):
            nkt = 16 * cq + 16
            odrot[0] ^= 1
            bo, bd_ = (4, 5) if odrot[0] else (6, 7)
            for kt in range(nkt):
                st = {}

                def s1(cq=cq, kt=kt, st=st):
                    s0 = max(0, -((3 - kt) // 4) - 4 * cq)
                    c0 = 128 * s0
                    q0 = cq * 512 + c0
                    q1 = cq * 512 + 512
                    bl = Lbank()
                    diag = []
                    for s_ in range(s0, 4):
                        v = kt - (4 * (4 * cq + s_) - 1)
                        if 0 <= v <= 4:
                            diag.append((s_, v))
                    mm(bank(bl)[:, c0:512], KTa[:, kt * 128:(kt + 1) * 128], QTa[:, q0:q1], True, False, [b_KT[ks], b_QT[qs_]], bl)
                    mm(bank(bl)[:, c0:512], E[:, kt, :], selT[:, q0:q1], False, len(diag) == 0, [b_c2, b_sel], bl)
                    for di, (s_, v) in enumerate(diag):
                        mm(bank(bl)[:, s_ * 128:(s_ + 1) * 128], ident, bdb[:, v, :], False, di == len(diag) - 1, [b_bdb, b_const], bl)
                    ps_ = PTslot()
                    S.add("act", lambda e: e.activation(out=PTs[ps_][:, c0:512], in_=bank(bl)[:, c0:512], func=AF.Exp),
                          reads=[bbuf[bl]], writes=[b_PT[ps_]])
                    st["ps"] = ps_
                    st["c0"] = c0

                def s2(cq=cq, kt=kt, st=st, nkt=nkt, bo=bo, bd_=bd_):
                    ps_, c0 = st["ps"], st["c0"]
                    mm(bank(bo)[:, c0:512], Va[:, kt, :], PTs[ps_][:, c0:512], kt == 0, kt == nkt - 1, [b_V[vs], b_PT[ps_]], bo)
                    mm(bank(bd_)[:, c0:512], ones, PTs[ps_][:, c0:512], kt == 0, kt == nkt - 1, [b_const, b_PT[ps_]], bd_)
                    if kt == nkt - 1:
                        fin(cq, bo, bd_)

                stages.append((s1, s2))
        run_pipe(stages)

    def prep_bd(hidx):
        sp_load(bdraw, bdiag[hidx], [b_bdraw], "ld_bd")
        S.add("dve", lambda e: e.tensor_scalar(out=bdb_flat, in0=bdraw, scalar1=crp[:, hidx:hidx + 1], scalar2=None, op0=ALU.subtract),
              reads=[b_bdraw, b_c2], writes=[b_bdb])

    def compress(g, w1d, w2d, ped, src_ft, is_v):
        cast_load(w1b, w1d.rearrange("(l p) h -> p l h", p=128), [b_w1], "ld_w1")
        cast_load(w2b, w2d.rearrange("(c p) d -> p c d", p=128), [b_w2], "ld_w2")
        cast_load(peb, ped, [b_pe], "ld_pe")
        sp_load(KTs[0], FT[src_ft], [b_KT[0]], "ld_KT0")
        for hc in range(2):
            for l in range(32):
                mm(bank(7)[:, hc:hc + 1], w1b[:, l, hc * 128:(hc + 1) * 128], peb[:, l:l + 1], l == 0, l == 31, [b_w1, b_pe], 7)
        S.add("dve", lambda e: e.tensor_copy(out=bh[:, 0:2], in_=bank(7)[:, 0:2]), reads=[bbuf[7]], writes=[b_bh])
        for hc in range(2):
            bl = Lbank()
            for l in range(32):
                mm(bank(bl)[:, 0:255], w1b[:, l, hc * 128:(hc + 1) * 128], KTs[0][:, l:l + 16 * 254 + 1:16], l == 0, l == 31,
                   [b_w1, b_KT[0]], bl)
            S.add("dve", lambda e, bl=bl, hc=hc: e.tensor_scalar(out=zf[:, 0:255], in0=bank(bl)[:, 0:255], scalar1=bh[:, hc:hc + 1],
                                                                scalar2=None, op0=ALU.add), reads=[bbuf[bl], b_bh], writes=[b_z])
            S.add("dve", lambda e: e.tensor_tensor(out=uf[:, 0:255], in0=zf[:, 0:255], in1=zf[:, 0:255], op=ALU.mult), reads=[b_z], writes=[b_u])
            S.add("dve", lambda e: e.tensor_scalar(out=uf[:, 0:255], in0=uf[:, 0:255], scalar1=0.044715, scalar2=1.0, op0=ALU.mult, op1=ALU.add),
                  reads=[b_u], writes=[b_u])
            S.add("dve", lambda e: e.tensor_tensor(out=uf[:, 0:255], in0=uf[:, 0:255], in1=zf[:, 0:255], op=ALU.mult), reads=[b_u, b_z], writes=[b_u])
            S.add("act", lambda e: e.activation(out=sgf[:, 0:255], in_=uf[:, 0:255], func=AF.Sigmoid, scale=1.5957691216057308),
                  reads=[b_u], writes=[b_sg])
            S.add("dve", lambda e, hc=hc: e.tensor_tensor(out=hid[:, hc, 0:255], in0=zf[:, 0:255], in1=sgf[:, 0:255], op=ALU.mult),
                  reads=[b_z, b_sg], writes=[b_hid])
        if not is_v:
            bl = Lbank()
            for hc in range(2):
                mm(bank(bl)[:, 0:255], w2b[:, hc, :], hid[:, hc, 0:255], hc == 0, hc == 1, [b_w2, b_hid], bl)
            S.add("dve", lambda e: e.memset(kcmpT[:, g, :], 0.0), writes=[b_kcmp[g]])
            S.add("dve", lambda e, bl=bl: e.tensor_copy(out=kcmpT[:, g, 0:255], in_=bank(bl)[:, 0:255]), reads=[bbuf[bl]], writes=[b_kcmp[g]])
        else:
            S.add("dve", lambda e: e.memset(vcmp[:, g, :, :], 0.0), writes=[b_vcmp[g]])
            for ct in range(2):
                n = 128 if ct == 0 else 127
                bl = Lbank()
                for hc in range(2):
                    mm(bank(bl)[0:n, 0:128], hid[:, hc, ct * 128:ct * 128 + n], w2b[:, hc, :], hc == 0, hc == 1, [b_w2, b_hid], bl)
                S.add("dve", lambda e, bl=bl, ct=ct, n=n: e.tensor_copy(out=vcmp[0:n, g, ct, :], in_=bank(bl)[0:n, 0:128]),
                      reads=[bbuf[bl]], writes=[b_vcmp[g]])

    for g in range(2):
        compress(g, w1_k, w2_k, peT_k, 0 + g, False)
        compress(g, w1_v, w2_v, peT_v, 2 + g, True)

    for g in range(2):
        for jj in range(4):
            h = 4 * g + jj
            qs_ = h % 2
            load_head(qs_, None, None, h)
            cast_load(biasCb, biasC[h].rearrange("(c p) q -> p c q", p=128), [b_biasC], "ld_bc")
            issue_cv(21, paced=False)
            for cq in range(2):
                ch = slice(cq * 512, (cq + 1) * 512)
                pts = []
                for ct in range(2):
                    bl = Lbank()
                    mm(bank(bl), kcmpT[:, g, ct * 128:(ct + 1) * 128], QTs[qs_][:, ch], True, False, [b_kcmp[g], b_QT[qs_]], bl)
                    mm(bank(bl), ident, biasCb[:, ct, ch], False, True, [b_biasC, b_const], bl)
                    ps_ = PTslot()
                    pts.append(ps_)
                    S.add("act", lambda e, bl=bl, ps_=ps_: e.activation(out=PTs[ps_], in_=bank(bl), func=AF.Exp),
                          reads=[bbuf[bl]], writes=[b_PT[ps_]])
                for ct in range(2):
                    mm(bank(4), vcmp[:, g, ct, :], PTs[pts[ct]], ct == 0, ct == 1, [b_vcmp[g], b_PT[pts[ct]]], 4)
                for ct in range(2):
                    mm(bank(5), ones, PTs[pts[ct]], ct == 0, ct == 1, [b_const, b_PT[pts[ct]]], 5)
                for s in range(4):
                    for ct in range(2):
                        mm(bank(7)[:, s * 65:(s + 1) * 65], PTs[pts[ct]][:, s * 128:(s + 1) * 128], ovl[:, ct, :], ct == 0, ct == 1,
                           [b_c2, b_PT[pts[ct]]], 7)
                for s in range(4):
                    t = 4 * cq + s
                    S.add("dve", lambda e, s=s: e.tensor_scalar(out=rd1[:, 0:1], in0=bank(7)[:, s * 65 + 64:s * 65 + 65], scalar1=1e-30,
                                                               scalar2=None, op0=ALU.max), reads=[bbuf[7]], writes=[b_rd1])
                    S.add("dve", lambda e: e.reciprocal(out=rd1[:, 0:1], in_=rd1[:, 0:1]), reads=[b_rd1], writes=[b_rd1])
                    if jj == 0:
                        S.add("dve", lambda e, s=s, t=t: e.tensor_scalar(out=psel[:, t, :], in0=bank(7)[:, s * 65:s * 65 + 64],
                                                                        scalar1=rd1[:, 0:1], scalar2=None, op0=ALU.mult),
                              reads=[bbuf[7], b_rd1], writes=[b_psel])
                    else:
                        S.add("dve", lambda e, s=s, t=t: e.scalar_tensor_tensor(out=psel[:, t, :], in0=bank(7)[:, s * 65:s * 65 + 64],
                                                                               scalar=rd1[:, 0:1], in1=psel[:, t, :], op0=ALU.mult, op1=ALU.add),
                              reads=[bbuf[7], b_rd1, b_psel], writes=[b_psel])
                finalize_nsa(h, cq, 3 * h + 0, True)
        for t in range(8):
            S.add("dve", lambda e, t=t: e.tensor_tensor(out=sc, in0=psel[:, t, :], in1=fselT[:, t, :], op=ALU.add), reads=[b_psel, b_c2], writes=[b_sc])
            S.add("dve", lambda e: e.max(out=m8a, in_=sc), reads=[b_sc], writes=[b_sc])
            S.add("dve", lambda e: e.match_replace(out=sc2, in_to_replace=m8a, in_values=sc, imm_value=-1e30), reads=[b_sc], writes=[b_sc])
            S.add("dve", lambda e: e.max(out=m8b, in_=sc2), reads=[b_sc], writes=[b_sc])
            S.add("dve", lambda e: e.tensor_scalar(out=selm, in0=sc, scalar1=m8b[:, 7:8], scalar2=None, op0=ALU.is_ge), reads=[b_sc], writes=[b_sc])
            S.add("dve", lambda e: e.tensor_scalar(out=selb, in0=selm, scalar1=-NEG, scalar2=NEG, op0=ALU.mult, op1=ALU.add), reads=[b_sc], writes=[b_sc])
            S.add("pe", lambda e: e.transpose(out=bank16(7)[0:64, 0:128], in_=selb, identity=ident), reads=[b_sc, b_const], writes=[bbuf[7]])
            S.add("dve", lambda e, t=t, g=g: e.tensor_copy(out=selbT[:, g, t * 128:(t + 1) * 128], in_=bank16(7)[0:64, 0:128]),
                  reads=[bbuf[7]], writes=[b_selbT[g]])
        load_head(0, 4 + g, 0 + g, None)
        for jj in range(4):
            h = 4 * g + jj
            qs_ = h % 2
            load_head(qs_, None, None, h)
            prep_bd(h)
            attn_causal(0, 0, qs_, selbT[:, g, :], b_selbT[g], e64, lambda cq, bo, bd_, h=h: finalize_nsa(h, cq, 3 * h + 1, False, bo, bd_))
        load_head(1, 6 + g, 2 + g, None)
        for jj in range(4):
            h = 4 * g + jj
            qs_ = h % 2
            load_head(qs_, None, None, h)
            cast_load(bwb_flat, bwin[h], [b_bwb], "ld_bw")
            issue_cv(21, paced=False)
            stages = []
            for cq in range(2):
                odrot[0] ^= 1
                bo, bd_ = (4, 5) if odrot[0] else (6, 7)
                for s in range(4):
                    m = 4 * cq + s
                    kts = list(range(max(0, 4 * m - 4), 4 * m + 4))
                    for ki, kt in enumerate(kts):
                        st = {}

                        def s1(m=m, kt=kt, st=st, qs_=qs_):
                            v = kt - (4 * m - 4)
                            bl = Lbank()
                            mm(bank(bl)[:, 0:128], KTs[1][:, kt * 128:(kt + 1) * 128], QTs[qs_][:, m * 128:(m + 1) * 128], True, False,
                               [b_KT[1], b_QT[qs_]], bl)
                            mm(bank(bl)[:, 0:128], ident, bwb[:, v, :], False, True, [b_bwb, b_const], bl)
                            ps_ = PTslot()
                            S.add("act", lambda e: e.activation(out=PTs[ps_][:, 0:128], in_=bank(bl)[:, 0:128], func=AF.Exp),
                                  reads=[bbuf[bl]], writes=[b_PT[ps_]])
                            st["ps"] = ps_

                        def s2(s=s, kt=kt, ki=ki, nk=len(kts), st=st, bo=bo, bd_=bd_, cq=cq, h=h):
                            ps_ = st["ps"]
                            mm(bank(bo)[:, s * 128:(s + 1) * 128], Vs[1][:, kt, :], PTs[ps_][:, 0:128], ki == 0, ki == nk - 1,
                               [b_V[1], b_PT[ps_]], bo)
                            mm(bank(bd_)[:, s * 128:(s + 1) * 128], ones, PTs[ps_][:, 0:128], ki == 0, ki == nk - 1,
                               [b_const, b_PT[ps_]], bd_)
                            if s == 3 and ki == nk - 1:
                                finalize_nsa(h, cq, 3 * h + 2, False, bo, bd_)

                        stages.append((s1, s2))
            run_pipe(stages)

    for h in range(8):
        sl = h % 2
        load_head(sl, 8 + h, 4 + h, 8 + h)
        prep_bd(8 + h)
        S.add("dve", lambda e, sl=sl: e.reduce_sum(out=km, in_=KTs[sl].rearrange("p (n s) -> p n s", n=16), axis=AX.X),
              reads=[b_KT[sl]], writes=[b_km])
        S.add("dve", lambda e: e.tensor_scalar(out=kmT, in0=km, scalar1=1.0 / 256, scalar2=None, op0=ALU.mult), reads=[b_km], writes=[b_km])
        for t in range(8):
            mm(bank(7)[:, 0:16], QTs[sl][:, t * 128:(t + 1) * 128], kmT, True, True, [b_QT[sl], b_km], 7)
            S.add("dve", lambda e, t=t: e.tensor_tensor(out=sc[:, 0:16], in0=bank(7)[:, 0:16], in1=pmn[:, t, :], op=ALU.add),
                  reads=[bbuf[7], b_c2], writes=[b_sc])
            S.add("dve", lambda e: e.max(out=m8a, in_=sc[:, 0:16]), reads=[b_sc], writes=[b_sc])
            S.add("dve", lambda e: e.tensor_scalar(out=selm[:, 0:16], in0=sc[:, 0:16], scalar1=m8a[:, 2:3], scalar2=None, op0=ALU.is_ge),
                  reads=[b_sc], writes=[b_sc])
            S.add("dve", lambda e, t=t: e.tensor_tensor(out=selm[:, 0:16], in0=selm[:, 0:16], in1=p01[:, t, :], op=ALU.mult),
                  reads=[b_sc, b_c2], writes=[b_sc])
            S.add("dve", lambda e, t=t: e.tensor_tensor(out=selm[:, 0:16], in0=selm[:, 0:16], in1=o01[:, t, :], op=ALU.add),
                  reads=[b_sc, b_c2], writes=[b_sc])
            S.add("dve", lambda e: e.tensor_scalar(out=selb[:, 0:16], in0=selm[:, 0:16], scalar1=-NEG, scalar2=NEG, op0=ALU.mult, op1=ALU.add),
                  reads=[b_sc], writes=[b_sc])
            S.add("pe", lambda e: e.transpose(out=bank16(7)[0:16, 0:128], in_=selb[:, 0:16], identity=ident), reads=[b_sc, b_const], writes=[bbuf[7]])
            S.add("dve", lambda e, t=t: e.tensor_copy(out=selbTm[:, t * 128:(t + 1) * 128], in_=bank16(7)[0:16, 0:128]),
                  reads=[bbuf[7]], writes=[b_selbTm])
        attn_causal(sl, sl, sl, selbTm, b_selbTm, e16, lambda cq, bo, bd_, h=h: finalize_moba(h, cq, bo, bd_))
    for h in range(8):
        S.add("dve", lambda e, h=h: e.tensor_copy(out=oaT[:, h, :], in_=oaf[:, h, :]), reads=[b_oaf[h]], writes=[b_oaT])
    S.barrier()

    def dump_many(items):
        for name, ap, n, parts in items:
            S.barrier()
            dump_bf16(name, ap, n, parts)
        S.barrier()

    if stop == "D":
        items = [("d_oa%d" % h, oaT[:, h, :], 1024, 128) for h in range(8)] + [("d_ob%d" % h, obT[:, h, :], 1024, 128) for h in range(8)]
        items += [("d_kcmp%d" % g, kcmpT[:, g, :], 256, 128) for g in range(2)]
        items += [("d_vcmp%d" % g, vcmp[:, g, :, :].rearrange("p c d -> p (c d)"), 256, 128) for g in range(2)]
        items += [("d_selbT%d" % g, selbT[:, g, :], 1024, 64) for g in range(2)]
        dump_many(items)
        return nc, S, dbg
    wupA = P.at(64 * KB, 8 * 2048, BF16).rearrange("p (h c) -> p h c", h=8)
    wupB = P.at(96 * KB, 8 * 2048, BF16).rearrange("p (h c) -> p h c", h=8)
    b_wup = [Buf(), Buf()]
    for hh in range(8):
        cast_load(wupA[:, hh, :], w_up_a[hh * 128:(hh + 1) * 128, :], [b_wup[0]], "ld_wupa")
        cast_load(wupB[:, hh, :], w_up_b[hh * 128:(hh + 1) * 128, :], [b_wup[1]], "ld_wupb")
    mT = P.at(128 * KB, 16 * 1024, BF16).rearrange("p (k t) -> p k t", k=16)
    b_mT = [Buf() for _ in range(8)]
    gms = [P.at(160 * KB + i * 2 * KB, 1024, BF16) for i in range(4)]
    b_gm = [Buf() for _ in range(4)]
    t1 = P.at(168 * KB, 512, F32)
    t2 = P.at(170 * KB, 512, F32)
    b_t1, b_t2 = Buf(), Buf()
    for ct in range(16):
        sa, sb_ = (ct % 2) * 2, (ct % 2) * 2 + 1
        sp_load(gms[sa], GM[ct], [b_gm[sa]], "ld_gm%d" % sa)
        sp_load(gms[sb_], GM[16 + ct], [b_gm[sb_]], "ld_gm%d" % sb_)
        for cq in range(2):
            ch = slice(cq * 512, (cq + 1) * 512)
            ba, bb2 = Lbank(), Lbank()
            for hh in range(8):
                mm(bank(ba), wupA[:, hh, ct * 128:(ct + 1) * 128], oaT[:, hh, ch], hh == 0, hh == 7, [b_wup[0], b_oaT], ba)
            for hh in range(8):
                mm(bank(bb2), wupB[:, hh, ct * 128:(ct + 1) * 128], obT[:, hh, ch], hh == 0, hh == 7, [b_wup[1]] + b_obT, bb2)
            S.add("dve", lambda e, ba=ba, sa=sa, ch=ch: e.tensor_tensor(out=t1, in0=bank(ba), in1=gms[sa][:, ch], op=ALU.mult),
                  reads=[bbuf[ba], b_gm[sa]], writes=[b_t1])
            S.add("dve", lambda e, bb2=bb2, sb_=sb_, ch=ch: e.tensor_tensor(out=t2, in0=bank(bb2), in1=gms[sb_][:, ch], op=ALU.mult),
                  reads=[bbuf[bb2], b_gm[sb_]], writes=[b_t2])
            S.add("dve", lambda e, ct=ct, ch=ch: e.tensor_tensor(out=mT[:, ct, ch], in0=t1, in1=t2, op=ALU.add),
                  reads=[b_t1, b_t2], writes=b_mT[cq * 4:(cq + 1) * 4])
    S.barrier()
    x2 = P.at(64 * KB, 8 * 2048, F32).rearrange("p (t c) -> p t c", t=8)
    b_x2 = [Buf() for _ in range(8)]
    for t in range(8):
        sp_load(x2[:, t, :], xo[t * 128:(t + 1) * 128, :], [b_x2[t]], "ld_x2")
    wos = [P.at(i * 16 * KB, 16 * 512, BF16).rearrange("p (k c) -> p k c", k=16) for i in range(2)]
    b_wo = [Buf(), Buf()]
    for dg in range(4):
        s = dg % 2
        cast_load(wos[s], w_out[:, dg * 512:(dg + 1) * 512].rearrange("(k p) c -> p k c", p=128), [b_wo[s]], "ld_wo%d" % s)
        for t in range(8):
            bl = Lbank()
            for kc in range(16):
                mm(bank(bl), mT[:, kc, t * 128:(t + 1) * 128], wos[s][:, kc, :], kc == 0, kc == 15, [b_mT[t], b_wo[s]], bl)
            S.add("dve", lambda e, bl=bl, t=t, dg=dg: e.tensor_tensor(out=x2[:, t, dg * 512:(dg + 1) * 512], in0=bank(bl),
                                                                     in1=x2[:, t, dg * 512:(dg + 1) * 512], op=ALU.add),
                  reads=[bbuf[bl], b_x2[t]], writes=[b_x2[t]])
    S.barrier()

    if stop == "F":
        d = dout("d_x2", [128, 8 * 2048])
        S.final.append(S.add("sp", lambda e: e.dma_start(out=d, in_=P.at(64 * KB, 8 * 2048, F32)), reads=b_x2, dkey="st_dbg"))
        return nc, S, dbg
    hn2 = P.at(128 * KB, 8 * 2048, BF16).rearrange("p (t c) -> p t c", t=8)
    hn2T = P.at(160 * KB, 16 * 1024, BF16).rearrange("p (k t) -> p k t", k=16)
    b_hn2 = [Buf() for _ in range(8)]
    b_hn2T = [Buf() for _ in range(8)]
    gff = P.at(0, 2048, F32)
    b_gff = Buf()
    junk2 = P.at(8 * KB, 2048, BF16)
    b_junk2 = Buf()
    S.add("sp", lambda e: e.dma_start(out=gff, in_=norm_ffn.partition_broadcast(128)), writes=[b_gff], dkey="ld_g")
    for t in range(8):
        st = sstat[:, 8 + 4 * (t % 2):12 + 4 * (t % 2)]
        bs = b_st[t % 2]
        S.add("act", lambda e, t=t, st=st: e.activation(out=junk2, in_=x2[:, t, :], func=AF.Square, accum_out=st[:, 0:1]),
              reads=[b_x2[t]], writes=[b_junk2, bs])
        S.add("dve", lambda e, st=st: e.tensor_scalar(out=st[:, 1:2], in0=st[:, 0:1], scalar1=1.0 / 2048, scalar2=1e-6, op0=ALU.mult, op1=ALU.add),
              reads=[bs], writes=[bs])
        S.add("act", lambda e, st=st: e.activation(out=st[:, 2:3], in_=st[:, 1:2], func=AF.Sqrt), reads=[bs], writes=[bs])
        S.add("dve", lambda e, st=st: e.reciprocal(out=st[:, 3:4], in_=st[:, 2:3]), reads=[bs], writes=[bs])
        S.add("dve", lambda e, t=t, st=st: e.scalar_tensor_tensor(out=hn2[:, t, :], in0=x2[:, t, :], scalar=st[:, 3:4], in1=gff,
                                                                 op0=ALU.mult, op1=ALU.mult), reads=[b_x2[t], bs, b_gff], writes=[b_hn2[t]])
        transpose_tile(hn2[:, t, :], b_hn2[t], hn2T, b_hn2T[t], t * 128)
    wr = P.at(12 * KB, 16 * 72, BF16).rearrange("p (k c) -> p k c", k=16)
    brt = P.at(16 * KB, 72, F32)
    b_wr, b_brt = Buf(), Buf()
    cast_load(wr, w_rt.rearrange("(k p) c -> p k c", p=128), [b_wr], "ld_wr")
    S.add("sp", lambda e: e.dma_start(out=brt, in_=b_rt.partition_broadcast(128)), writes=[b_brt], dkey="ld_brt")
    ustr = P.at(17 * KB, 128, BF16)
    iot = P.at(63 * KB, 128, F32)
    b_c3 = Buf()
    S.add("sp", [lambda e: e.dma_start(out=ustr, in_=c_ustr), lambda e: e.dma_start(out=iot, in_=c_iota)], writes=[b_c3], dkey="const3")
    A1h = P.at(194 * KB, 512, BF16).rearrange("p (t n) -> p t n", t=8)
    A2h = P.at(195 * KB, 512, BF16).rearrange("p (t n) -> p t n", t=8)
    W12 = P.at(199 * KB, 16, F32)
    YY = dscr("YY", [8192, 2048])
    Ind = P.at(196 * KB, 512, BF16).rearrange("p (t n) -> p t n", t=8)
    Rk = P.at(197 * KB, 512, F32).rearrange("p (t n) -> p t n", t=8)
    b_Wm, b_Ind, b_Rk = Buf(), Buf(), Buf()
    rl = P.at(20 * KB, 72, F32)
    Lm = P.at(21 * KB, 64, F32)
    mg = P.at(22 * KB, 8, F32)
    pen = P.at(22 * KB + 64, 8, F32)
    m8r = P.at(22 * KB + 128, 8, F32)
    sm = P.at(22 * KB + 192, 16, F32)
    a1 = P.at(23 * KB, 64, F32)
    a2 = P.at(23 * KB + 256, 64, F32)
    b_r = Buf()
    for t in range(8):
        for kc in range(16):
            mm(bank(7)[:, 0:72], hn2T[:, kc, t * 128:(t + 1) * 128], wr[:, kc, :], kc == 0, kc == 15, [b_hn2T[t], b_wr], 7)
        R = [b_r]
        S.add("dve", lambda e: e.tensor_tensor(out=rl, in0=bank(7)[:, 0:72], in1=brt, op=ALU.add), reads=[bbuf[7], b_brt], writes=R)
        S.add("dve", lambda e: e.reduce_max(out=sm[:, 0:1], in_=rl[:, 0:8], axis=AX.X), reads=R, writes=R)
        S.add("dve", lambda e: e.tensor_scalar(out=mg, in0=rl[:, 0:8], scalar1=sm[:, 0:1], scalar2=None, op0=ALU.is_ge), reads=R, writes=R)
        S.add("dve", lambda e: e.tensor_scalar(out=sm[:, 1:2], in0=sm[:, 0:1], scalar1=-1.0, scalar2=None, op0=ALU.mult), reads=R, writes=R)
        S.add("act", lambda e: e.activation(out=a1[:, 0:8], in_=rl[:, 0:8], func=AF.Exp, bias=sm[:, 1:2], accum_out=sm[:, 2:3]), reads=R, writes=R)
        S.add("dve", lambda e: e.reciprocal(out=sm[:, 3:4], in_=sm[:, 2:3]), reads=R, writes=R)
        S.add("dve", lambda e: e.tensor_scalar(out=pen, in0=mg, scalar1=1e30, scalar2=-1e30, op0=ALU.mult, op1=ALU.add), reads=R, writes=R)
        S.add("dve", lambda e: e.tensor_tensor(out=Lm.rearrange("p (g x) -> p g x", g=8), in0=rl[:, 8:72].rearrange("p (g x) -> p g x", g=8),
                                               in1=mg.unsqueeze(2).to_broadcast([128, 8, 8]), op=ALU.mult), reads=R, writes=R)
        S.add("dve", lambda e: e.tensor_tensor(out=Lm.rearrange("p (g x) -> p g x", g=8), in0=Lm.rearrange("p (g x) -> p g x", g=8),
                                               in1=pen.unsqueeze(2).to_broadcast([128, 8, 8]), op=ALU.add), reads=R, writes=R)
        S.add("dve", lambda e: e.max(out=m8r, in_=Lm), reads=R, writes=R)
        S.add("dve", lambda e: e.tensor_tensor(out=sm[:, 4:5], in0=m8r[:, 0:1], in1=m8r[:, 1:2], op=ALU.subtract), reads=R, writes=R)
        S.add("act", lambda e: e.activation(out=sm[:, 5:6], in_=sm[:, 4:5], func=AF.Sigmoid), reads=R, writes=R)
        S.add("dve", lambda e: e.tensor_tensor(out=sm[:, 6:7], in0=sm[:, 5:6], in1=sm[:, 3:4], op=ALU.mult), reads=R, writes=R)
        S.add("dve", lambda e: e.tensor_tensor(out=sm[:, 7:8], in0=sm[:, 3:4], in1=sm[:, 6:7], op=ALU.subtract), reads=R, writes=R)
        S.add("dve", lambda e: e.tensor_scalar(out=a1, in0=Lm, scalar1=m8r[:, 0:1], scalar2=sm[:, 6:7], op0=ALU.is_equal, op1=ALU.mult), reads=R, writes=R)
        S.add("dve", lambda e: e.tensor_scalar(out=a2, in0=Lm, scalar1=m8r[:, 1:2], scalar2=sm[:, 7:8], op0=ALU.is_equal, op1=ALU.mult), reads=R, writes=R)
        S.add("dve", lambda e, t=t: e.tensor_scalar(out=A1h[:, t, :], in0=Lm, scalar1=m8r[:, 0:1], scalar2=None, op0=ALU.is_equal), reads=R, writes=[b_Wm])
        S.add("dve", lambda e, t=t: e.tensor_scalar(out=A2h[:, t, :], in0=Lm, scalar1=m8r[:, 1:2], scalar2=None, op0=ALU.is_equal), reads=R, writes=[b_Wm])
        S.add("dve", lambda e, t=t: e.tensor_copy(out=W12[:, 2 * t:2 * t + 2], in_=sm[:, 6:8]), reads=R, writes=[b_Wm])
        S.add("dve", lambda e, t=t: e.tensor_scalar(out=Ind[:, t, :], in0=Lm, scalar1=m8r[:, 1:2], scalar2=None, op0=ALU.is_ge), reads=R, writes=[b_Ind])
    for t in range(8):
        for tp in range(t + 1):
            mm(bank(7)[:, 0:64], ustr if tp == t else ones, Ind[:, tp, :], tp == 0, tp == t, [b_Ind, b_c3, b_const], 7)
        S.add("dve", lambda e, t=t: e.tensor_scalar(out=Rk[:, t, :], in0=bank(7)[:, 0:64], scalar1=1.0, scalar2=None, op0=ALU.add),
              reads=[bbuf[7]], writes=[b_Rk])
        S.add("dve", lambda e, t=t: e.tensor_tensor(out=Rk[:, t, :], in0=Rk[:, t, :], in1=Ind[:, t, :], op=ALU.mult), reads=[b_Rk, b_Ind], writes=[b_Rk])
        S.add("dve", lambda e, t=t: e.tensor_scalar(out=Rk[:, t, :], in0=Rk[:, t, :], scalar1=-1.0, scalar2=None, op0=ALU.add),
              reads=[b_Rk], writes=[b_Rk])
    S.barrier()
    wgs = [P.at(0, 16 * 512, BF16).rearrange("p (k f) -> p k f", k=16), P.at(160 * KB, 16 * 512, BF16).rearrange("p (k f) -> p k f", k=16)]
    wus = [P.at(16 * KB, 16 * 512, BF16).rearrange("p (k f) -> p k f", k=16), P.at(176 * KB, 16 * 512, BF16).rearrange("p (k f) -> p k f", k=16)]
    wd = P.at(32 * KB, 4 * 2048, BF16).rearrange("p (f d) -> p f d", f=4)
    wgs_flat = [P.at(0, 16 * 512, BF16).rearrange("p (a b) -> p a b", b=2048), P.at(160 * KB, 16 * 512, BF16).rearrange("p (a b) -> p a b", b=2048)]
    wus_flat = [P.at(16 * KB, 16 * 512, BF16).rearrange("p (a b) -> p a b", b=2048), P.at(176 * KB, 16 * 512, BF16).rearrange("p (a b) -> p a b", b=2048)]
    b_wg, b_wu, b_wd = [Buf(), Buf()], [Buf(), Buf()], Buf()
    Sel = P.at(48 * KB, 1024, BF16).rearrange("p (t r) -> p t r", t=8)
    XeT = P.at(50 * KB, 2048, BF16).rearrange("p (k r) -> p k r", k=16)
    HT = P.at(54 * KB, 512, BF16).rearrange("p (f r) -> p f r", f=4)
    Ysbs = [P.at(55 * KB, 2048, BF16), P.at(59 * KB, 2048, BF16)]
    b_Ysbs = [Buf(), Buf()]
    sgt = P.at(192 * KB, 512, F32)
    b_Sel, b_XeT, b_HT, b_sgt = [Buf() for _ in range(4)]
    Sel2 = [Sel, P.at(203 * KB, 1024, BF16).rearrange("p (t r) -> p t r", t=8)]
    b_Sel2 = [b_Sel, Buf()]

    def gen_sel(ex):
        sl = ex % 2
        for t in range(8):
            S.add("dve", lambda e, t=t: e.tensor_scalar(out=Sel2[sl][:, t, :], in0=iot, scalar1=Rk[:, t, ex:ex + 1], scalar2=None,
                                                       op0=ALU.is_equal), reads=[b_c3, b_Rk], writes=[b_Sel2[sl]])

    def load_exp(ex):
        s = ex % 2
        if ex >= CV0:
            i = ex - CV0
            ex_dep = [cv_last[0]]
            S.add("sp", lambda e: e.dma_start(out=wgs_flat[s], in_=WBg[i].rearrange("a b -> (a b)").rearrange("(p a b) -> p a b", p=128, b=2048)),
                  writes=[b_wg[s]], dkey="ld_wg%d" % s, extra=ex_dep)
            S.add("sp", lambda e: e.dma_start(out=wus_flat[s], in_=WBu[i].rearrange("a b -> (a b)").rearrange("(p a b) -> p a b", p=128, b=2048)),
                  writes=[b_wu[s]], dkey="ld_wu%d" % s, extra=ex_dep)
            return
        S.add("pool", lambda e: e.dma_start(out=wgs_flat[s], in_=w_eg[ex].rearrange("(p k) f -> p (k f)", k=16).rearrange("p (a b) -> p a b", b=2048)),
              writes=[b_wg[s]], dkey="ld_wg%d" % s)
        S.add("pool", lambda e: e.dma_start(out=wus_flat[s], in_=w_eu[ex].rearrange("(p k) f -> p (k f)", k=16).rearrange("p (a b) -> p a b", b=2048)),
              writes=[b_wu[s]], dkey="ld_wu%d" % s)

    def load_wd(ex):
        if ex >= CV0:
            i = ex - CV0
            S.add("sp", lambda e: e.dma_start(out=wd, in_=WBd[i].rearrange("(f p) d -> p f d", p=128)), writes=[b_wd], dkey="ld_wd",
                  extra=[cv_last[0]])
            return
        S.add("pool", lambda e: e.dma_start(out=wd, in_=w_ed[ex].rearrange("(f p) d -> p f d", p=128)), writes=[b_wd], dkey="ld_wd")

    def gather(ex):
        sl = ex % 2
        for k4 in range(4):
            bl = k4 % 2
            for q in range(4):
                kc = 4 * k4 + q
                for t in range(8):
                    mm(bank(bl)[:, q * 128:(q + 1) * 128], hn2[:, t, kc:2048:16], Sel2[sl][:, t, :], t == 0, t == 7,
                       [b_hn2[t], b_Sel2[sl]], bl)
            evac(XeT[:, 4 * k4:4 * k4 + 4, :], bank(bl).rearrange("p (q r) -> p q r", q=4), [bbuf[bl]], [b_XeT])

    issue_cv(len(cv_chunks), paced=False)
    load_exp(0)
    load_wd(0)
    gen_sel(0)
    gather(0)
    for ex in range(64):
        s = ex % 2
        if ex + 1 < 64:
            load_exp(ex + 1)
        for fc in range(4):
            for kc in range(16):
                mm(bank(2)[:, fc * 128:(fc + 1) * 128], wgs[s][:, kc, fc * 128:(fc + 1) * 128], XeT[:, kc, :], kc == 0, kc == 15, [b_wg[s], b_XeT], 2)
        for fc in range(4):
            for kc in range(16):
                mm(bank(3)[:, fc * 128:(fc + 1) * 128], wus[s][:, kc, fc * 128:(fc + 1) * 128], XeT[:, kc, :], kc == 0, kc == 15, [b_wu[s], b_XeT], 3)
        S.add("act", lambda e: e.activation(out=sgt, in_=bank(2), func=AF.Silu), reads=[bbuf[2]], writes=[b_sgt])
        S.add("dve", lambda e: e.tensor_tensor(out=HT.rearrange("p f r -> p (f r)"), in0=bank(3), in1=sgt, op=ALU.mult), reads=[bbuf[3], b_sgt], writes=[b_HT])
        if ex + 1 < 64:
            gen_sel(ex + 1)
            gather(ex + 1)
        ys = ex % 2
        for dg in range(4):
            bl = 4 + dg % 4
            for fc in range(4):
                mm(bank(bl), HT[:, fc, :], wd[:, fc, dg * 512:(dg + 1) * 512], fc == 0, fc == 3, [b_HT, b_wd], bl)
            evac(Ysbs[ys][:, dg * 512:(dg + 1) * 512], bank(bl), [bbuf[bl]], [b_Ysbs[ys]])
        if ex + 1 < 64:
            load_wd(ex + 1)
        S.add("sp", lambda e, ys=ys, ex=ex: e.dma_start(out=YY[ex * 128:(ex + 1) * 128, :], in_=Ysbs[ys]), reads=[b_Ysbs[ys]],
              dkey="st_ys%d" % ys)
    S.barrier()
    eb = P.at(0, 64, F32)
    vv = P.at(512, 64, F32)
    idf = P.at(1024, 16, F32)
    idu = P.at(2048, 16, F32).bitcast(mybir.dt.uint32)
    b_cmb = Buf()
    gbuf = [P.at(8 * KB + i * 4 * KB, 2048, BF16) for i in range(4)]
    b_gbuf = [Buf() for _ in range(4)]
    S.add("dve", lambda e: e.tensor_scalar(out=eb, in0=iot[:, 0:64], scalar1=128.0, scalar2=None, op0=ALU.mult), reads=[b_c3], writes=[b_cmb])
    for t in range(8):
        for k, Ah in enumerate((A1h, A2h)):
            col = 2 * t + k
            S.add("dve", lambda e, t=t: e.tensor_tensor(out=vv, in0=Rk[:, t, :], in1=eb, op=ALU.add), reads=[b_Rk, b_cmb], writes=[b_cmb])
            S.add("dve", lambda e, t=t, Ah=Ah: e.tensor_tensor(out=vv, in0=vv, in1=Ah[:, t, :], op=ALU.mult), reads=[b_cmb, b_Wm], writes=[b_cmb])
            S.add("dve", lambda e, col=col: e.reduce_sum(out=idf[:, col:col + 1], in_=vv, axis=AX.X), reads=[b_cmb], writes=[b_cmb])
    S.add("dve", lambda e: e.tensor_copy(out=idu, in_=idf), reads=[b_cmb], writes=[b_cmb])
    for t in range(8):
        for k in range(2):
            col = 2 * t + k
            gi_ = (2 * t + k) % 4
            S.add("pool", lambda e, col=col, gi_=gi_: e.indirect_dma_start(out=gbuf[gi_], out_offset=None, in_=YY,
                                                                          in_offset=bass.IndirectOffsetOnAxis(ap=idu[:, col:col + 1], axis=0)),
                  reads=[b_cmb], writes=[b_gbuf[gi_]], dkey="ld_gb%d" % gi_)
            S.add("dve", lambda e, t=t, col=col, gi_=gi_: e.scalar_tensor_tensor(out=x2[:, t, :], in0=gbuf[gi_], scalar=W12[:, col:col + 1],
                                                                                in1=x2[:, t, :], op0=ALU.mult, op1=ALU.add),
                  reads=[b_gbuf[gi_], b_Wm, b_x2[t]], writes=[b_x2[t]])
    S.barrier()

    gfn = P.at(0, 2048, F32)
    b_gfn = Buf()
    S.add("sp", lambda e: e.dma_start(out=gfn, in_=final_norm.partition_broadcast(128)), writes=[b_gfn], dkey="ld_g")
    ost = [P.at(8 * KB + i * 8 * KB, 2048, F32) for i in range(2)]
    b_ost = [Buf(), Buf()]
    junk3 = P.at(24 * KB, 2048, BF16)
    b_junk3 = Buf()
    for t in range(8):
        st = sstat[:, 16 + 4 * (t % 2):20 + 4 * (t % 2)]
        bs = b_st[t % 2]
        o_ = ost[t % 2]
        S.add("act", lambda e, t=t, st=st: e.activation(out=junk3, in_=x2[:, t, :], func=AF.Square, accum_out=st[:, 0:1]),
              reads=[b_x2[t]], writes=[b_junk3, bs])
        S.add("dve", lambda e, st=st: e.tensor_scalar(out=st[:, 1:2], in0=st[:, 0:1], scalar1=1.0 / 2048, scalar2=1e-6, op0=ALU.mult, op1=ALU.add),
              reads=[bs], writes=[bs])
        S.add("act", lambda e, st=st: e.activation(out=st[:, 2:3], in_=st[:, 1:2], func=AF.Sqrt), reads=[bs], writes=[bs])
        S.add("dve", lambda e, st=st: e.reciprocal(out=st[:, 3:4], in_=st[:, 2:3]), reads=[bs], writes=[bs])
        S.add("dve", lambda e, t=t, st=st, o_=o_: e.scalar_tensor_tensor(out=o_, in0=x2[:, t, :], scalar=st[:, 3:4], in1=gfn,
                                                                        op0=ALU.mult, op1=ALU.mult), reads=[b_x2[t], bs, b_gfn], writes=[b_ost[t % 2]])
        S.final.append(S.add("sp", lambda e, t=t, o_=o_: e.dma_start(out=out[t * 128:(t + 1) * 128, :], in_=o_), reads=[b_ost[t % 2]],
                             dkey="st_out%d" % (t % 2)))
    return nc, S, dbg


def host_inputs(inputs):
    x = np.ascontiguousarray(inputs["x"], dtype=np.float32)
    rel = np.asarray(inputs["rel_bias"], np.float32)
    bf = ml_dtypes.bfloat16
    shared = dict(
        w_in=np.ascontiguousarray(inputs["w_in"][0]),
        norm_mix=np.ascontiguousarray(inputs["norm_mix"]).reshape(1, 2048),
        norm_ffn=np.ascontiguousarray(inputs["norm_ffn"]).reshape(1, 2048),
        final_norm=np.ascontiguousarray(inputs["final_norm"]).reshape(1, 2048),
        peT_k=np.ascontiguousarray(inputs["cmp_pe_k"][0].T),
        peT_v=np.ascontiguousarray(inputs["cmp_pe_v"][0].T),
        w1_k=np.ascontiguousarray(inputs["cmp_w1_k"][0]),
        w1_v=np.ascontiguousarray(inputs["cmp_w1_v"][0]),
        w2_k=np.ascontiguousarray(inputs["cmp_w2_k"][0]),
        w2_v=np.ascontiguousarray(inputs["cmp_w2_v"][0]),
        w_up_a=np.ascontiguousarray(inputs["w_up_nsa"][0]),
        w_up_b=np.ascontiguousarray(inputs["w_up_moba"][0]),
        w_out=np.ascontiguousarray(inputs["w_out"][0]),
        w_rt=np.ascontiguousarray(np.concatenate(
            [inputs["w_group"][0], inputs["w_router"][0].transpose(1, 0, 2).reshape(2048, 64)], axis=1)),
        b_rt=np.ascontiguousarray(np.concatenate([inputs["b_group"][0].reshape(8), inputs["b_router"][0].reshape(64)]).reshape(1, 72)),
        w_eg=np.ascontiguousarray(inputs["w_exp_gate"][0]),
        w_eu=np.ascontiguousarray(inputs["w_exp_up"][0]),
        w_ed=np.ascontiguousarray(inputs["w_exp_down"][0]),
        c_ident=np.eye(128, dtype=np.float32).astype(bf),
        c_ones=np.ones((128, 128), np.float32).astype(bf),
        c_ustr=np.triu(np.ones((128, 128), np.float32), 1).astype(bf),
        c_iota=np.tile(np.arange(128, dtype=np.float32)[None, :], (128, 1)),
        crep=np.ascontiguousarray(np.tile(rel[31:32, :], (128, 1))),
    )
    c_start = np.arange(255) * 16
    sb = np.arange(64) * 64
    ovl = ((c_start[:, None] < sb[None, :] + 64) & (c_start[:, None] + 32 > sb[None, :])).astype(np.float32)
    ovl65 = np.zeros((256, 65), np.float32)
    ovl65[:255, :64] = ovl
    ovl65[:255, 64] = 1.0
    shared["c_ovl"] = np.ascontiguousarray(ovl65.reshape(2, 128, 65).transpose(1, 0, 2).reshape(128, 130)).astype(bf)
    e64 = np.zeros((64, 32, 128), np.float32)
    e16 = np.zeros((16, 32, 128), np.float32)
    for kt in range(32):
        e64[2 * kt, kt, 0:64] = 1
        e64[2 * kt + 1, kt, 64:128] = 1
        e16[kt // 2, kt, :] = 1
    shared["c_e64"] = e64.reshape(64, 4096).astype(bf)
    shared["c_e16"] = e16.reshape(16, 4096).astype(bf)
    s24 = np.zeros((24, 24, 128), np.float32)
    for i in range(24):
        s24[i, i, :] = 1
    shared["c_sel24"] = s24.reshape(24, 24 * 128).astype(bf)

    maps = []
    for c in range(8):
        b, j = divmod(c, 4)
        tiles = [4 * m + j for m in range(8)]
        tpos = np.concatenate([np.arange(128) + 128 * a for a in tiles])
        m = dict(shared)
        m["xb"] = x[b]
        m["xo"] = np.ascontiguousarray(x[b].reshape(32, 128, 2048)[tiles].reshape(1024, 2048))
        cend = c_start + 31
        dist_c = tpos[None, :] - cend[:, None]
        bc = np.full((8, 256, 1024), NEG, np.float32)
        val = rel[rel_bucket_np(dist_c)][:, :, :8]
        bc[:, :255, :] = np.where((dist_c >= 0)[None], val.transpose(2, 0, 1), NEG)
        m["biasC"] = bc
        r = np.arange(128)
        bd = np.zeros((16, 128, 5, 128), np.float32)
        for v in range(5):
            dist = 128 * (j + 1 - v) + r[None, :] - r[:, None]
            tb = rel[rel_bucket_np(dist)]
            bd[:, :, v, :] = np.where((dist >= 0)[None], tb.transpose(2, 0, 1), NEG)
        m["bdiag"] = bd.reshape(16, 128, 640)
        bw = np.zeros((8, 128, 8, 128), np.float32)
        for v in range(8):
            dist = 128 * (j + 4 - v) + r[None, :] - r[:, None]
            tb = rel[rel_bucket_np(dist)][:, :, :8]
            bw[:, :, v, :] = np.where(((dist >= 0) & (dist < 512))[None], tb.transpose(2, 0, 1), NEG)
        m["bwin"] = bw.reshape(8, 128, 1024)
        cur = tpos // 64
        n64 = np.arange(64)[None, :]
        forced = (n64 == 0) | (n64 == cur[:, None]) | (n64 == cur[:, None] - 1)
        m["fsel"] = np.ascontiguousarray(np.where(forced, 1e4, 0.0).astype(np.float32).reshape(8, 128, 64).transpose(1, 0, 2).reshape(128, 512))
        own = tpos // 256
        n16 = np.arange(16)[None, :]
        past = n16 < own[:, None]

        def lay16(a):
            return np.ascontiguousarray(a.astype(np.float32).reshape(8, 128, 16).transpose(1, 0, 2).reshape(128, 128))
        m["pmneg"] = lay16(np.where(past, 0.0, -1e30))
        m["past01"] = lay16(past)
        m["own01"] = lay16(n16 == own[:, None])
        maps.append(m)
    return maps


def kernel(**inputs):
    nc, S, dbg = build()
    with contextlib.ExitStack() as st:
        S.emit(st)
    maps = host_inputs(inputs)
    res = run_bass_kernel_spmd(nc, maps, core_ids=list(range(8)))
    outp = np.zeros((2, 32, 128, 2048), np.float32)
    for c in range(8):
        b, j = divmod(c, 4)
        o = np.asarray(res.results[c]["out"]).reshape(8, 128, 2048)
        for m in range(8):
            outp[b, 4 * m + j] = o[m]
    return outp.reshape(2, 4096, 2048)
```

```python
import contextlib
import math
import numpy as np
import ml_dtypes
import concourse.bass as bass
import concourse.mybir as mybir
from concourse.bass_utils import run_bass_kernel_spmd

F32 = mybir.dt.float32
BF16 = mybir.dt.bfloat16
U8 = mybir.dt.uint8
AF = mybir.ActivationFunctionType
ALU = mybir.AluOpType
AX = mybir.AxisListType
NEG = -30000.0


class Buf:
    __slots__ = ("name", "w", "r")

    def __init__(self, name=""):
        self.name = name
        self.w = None
        self.r = []


class Op:
    __slots__ = ("eng", "fns", "deps", "signal", "sval", "dsem", "dtgt")


class Sched:
    ENGS = ("sp", "act", "dve", "pool", "pe")

    def __init__(self, nc):
        self.nc = nc
        self.ops = {e: [] for e in self.ENGS}
        self.dkeys = {}
        self.dlast = {}
        self.pending = {e: [] for e in self.ENGS}
        self.final = []

    def _dep(self, op, w):
        if w is None or w is op:
            return
        if w.eng == "pe" and op.eng == "pe":
            return
        op.deps.append(w)
        if w.dsem is None:
            w.signal = True

    def add(self, eng, fns, reads=(), writes=(), dkey=None, extra=()):
        op = Op()
        op.eng = eng
        op.fns = fns if isinstance(fns, (list, tuple)) else [fns]
        op.deps = []
        op.signal = False
        op.sval = None
        op.dsem = None
        op.dtgt = None
        if dkey is not None:
            ent = self.dkeys.setdefault(dkey, [0])
            ent[0] += 16 * len(op.fns)
            op.dsem = dkey
            op.dtgt = ent[0]
            self.dlast[dkey] = op
        for b in reads:
            self._dep(op, b.w)
        for b in writes:
            self._dep(op, b.w)
            for r in b.r:
                self._dep(op, r)
        for w in self.pending[eng]:
            self._dep(op, w)
        self.pending[eng] = []
        for w in extra:
            self._dep(op, w)
        for b in writes:
            b.w = op
            b.r = []
        for b in reads:
            if b.w is not op:
                b.r.append(op)
        self.ops[eng].append(op)
        return op

    def barrier(self):
        lasts = [self.ops[e][-1] for e in self.ENGS if self.ops[e] and self.ops[e][-1].dsem != "cv"]
        lasts += [op for k, op in self.dlast.items() if k != "cv"]
        for e in self.ENGS:
            self.pending[e] = self.pending[e] + lasts

    def emit(self, stack):
        nc = self.nc
        sems = {e: stack.enter_context(nc.semaphore("s_" + e)) for e in self.ENGS}
        dsems = {k: stack.enter_context(nc.semaphore("d_" + str(k))) for k in self.dkeys}
        for e in self.ENGS:
            c = 0
            for op in self.ops[e]:
                if op.dsem is None and op.signal:
                    c += 1
                    op.sval = c
        block = stack.enter_context(nc.Block())
        engmap = {"sp": block.sync, "act": block.scalar, "dve": block.vector, "pool": block.gpsimd,
                  "pe": block.tensor}

        def make(e):
            def body(eng):
                known = {}

                def waits(deps):
                    need = {}
                    for w in deps:
                        if w.dsem is not None:
                            key, val = ("d", w.dsem), w.dtgt
                        else:
                            key, val = ("e", w.eng), w.sval
                        if need.get(key, 0) < val:
                            need[key] = val
                    for key, val in need.items():
                        if known.get(key, 0) >= val:
                            continue
                        known[key] = val
                        eng.wait_ge(dsems[key[1]] if key[0] == "d" else sems[key[1]], val)

                for op in self.ops[e]:
                    waits(op.deps)
                    last = None
                    for fn in op.fns:
                        last = fn(eng)
                        if op.dsem is not None:
                            last.then_inc(dsems[op.dsem], 16)
                    if op.dsem is None and op.signal:
                        last.then_inc(sems[e], 1)
                if e == "sp":
                    waits(self.final)
            return body

        for e in self.ENGS:
            if self.ops[e] or e == "sp":
                engmap[e](make(e))


class Pool:
    def __init__(self, nc, nbytes):
        self.t = nc.alloc_sbuf_tensor("pool", [128, nbytes], U8)
        self.nbytes = nbytes

    def at(self, off, cols, dt, parts=128):
        sz = cols * (4 if dt == F32 else 2)
        assert off % 32 == 0 and off + sz <= self.nbytes, (off, sz, self.nbytes)
        return self.t[0:parts, off:off + sz].bitcast(dt)


def rel_bucket_np(dist):
    n = np.maximum(dist.astype(np.int64), 0)
    nf = np.maximum(n, 1).astype(np.float32)
    large = 16 + (np.log(nf / np.float32(16)) / np.float32(math.log(8.0)) * np.float32(16)).astype(np.int32)
    return np.where(n < 16, n, np.minimum(large, 31)).astype(np.int64)


_IN_OFF = dict(qa=0, kc=1024, vc=1280, ksl=1536, vsl=1792, kw=2048, vw=2304, gate=2560, qb=2584, kb=3608,
               vb=4632, gma=5656, gmb=7704)
KB = 1024
POOLB = 206 * KB


def build(stop=None):
    nc = bass.Bass("TRN2", target_bir_lowering=False)

    declared = set()

    def din(name, shape, dt=F32):
        declared.add(name)
        return nc.dram_tensor(name, list(shape), dt, kind="ExternalInput").ap()

    def dscr(name, shape, dt=BF16):
        return nc.dram_tensor(name, list(shape), dt).ap()

    xb = din("xb", [4096, 2048])
    xo = din("xo", [1024, 2048])
    w_in = din("w_in", [2048, 9752])
    norm_mix = din("norm_mix", [1, 2048])
    norm_ffn = din("norm_ffn", [1, 2048])
    final_norm = din("final_norm", [1, 2048])
    peT_k = din("peT_k", [128, 32])
    peT_v = din("peT_v", [128, 32])
    w1_k = din("w1_k", [4096, 256])
    w1_v = din("w1_v", [4096, 256])
    w2_k = din("w2_k", [256, 128])
    w2_v = din("w2_v", [256, 128])
    w_up_a = din("w_up_a", [1024, 2048])
    w_up_b = din("w_up_b", [1024, 2048])
    w_out = din("w_out", [2048, 2048])
    w_rt = din("w_rt", [2048, 72])
    b_rt = din("b_rt", [1, 72])
    if stop is None:
        w_eg = din("w_eg", [64, 2048, 512])
        w_eu = din("w_eu", [64, 2048, 512])
        w_ed = din("w_ed", [64, 512, 2048])
    biasC = din("biasC", [8, 256, 1024])
    bdiag = din("bdiag", [16, 128, 5 * 128])
    bwin = din("bwin", [8, 128, 8 * 128])
    crep = din("crep", [128, 16])
    fsel = din("fsel", [128, 8 * 64])
    pmneg = din("pmneg", [128, 8 * 16])
    past01 = din("past01", [128, 8 * 16])
    own01 = din("own01", [128, 8 * 16])
    c_ident = din("c_ident", [128, 128], BF16)
    c_ones = din("c_ones", [128, 128], BF16)
    c_ovl = din("c_ovl", [128, 2 * 65], BF16)
    c_e64 = din("c_e64", [64, 32 * 128], BF16)
    c_e16 = din("c_e16", [16, 32 * 128], BF16)
    c_sel24 = din("c_sel24", [24, 24 * 128], BF16)
    c_ustr = din("c_ustr", [128, 128], BF16)
    c_iota = din("c_iota", [128, 128])
    out = nc.dram_tensor("out", [1024, 2048], F32, kind="ExternalOutput").ap()

    FT = dscr("FT", [16, 128, 4096])
    TM = dscr("TM", [12, 128, 4096])
    QT = dscr("QT", [16, 128, 1024])
    GM = dscr("GM", [32, 128, 1024])
    dbg = {}
    dbg['_declared'] = declared

    def dout(name, shape):
        dbg[name] = nc.dram_tensor(name, list(shape), F32, kind="ExternalOutput").ap()
        return dbg[name]

    P = Pool(nc, POOLB)
    S = Sched(nc)
    banks = [nc.alloc_psum_tensor("pb%d" % i, [128, 512], F32) for i in range(8)]
    bbuf = [Buf("pb%d" % i) for i in range(8)]

    def bank(i):
        return banks[i][:, :]

    def bank16(i):
        return banks[i][:, :].bitcast(BF16)

    PB = 200 * KB
    ident = P.at(PB, 128, BF16)
    ones = P.at(PB + 256, 128, BF16)
    gT = P.at(PB + 512, 1024, BF16, parts=24)
    sstat = P.at(PB + 2560, 64, F32)
    b_const = Buf("const")
    b_gT = Buf("gT")
    S.add("sp", [lambda e: e.dma_start(out=ident, in_=c_ident), lambda e: e.dma_start(out=ones, in_=c_ones)],
          writes=[b_const], dkey="const")

    evac_rr = [0]

    def evac(out_ap, in_ap, reads, writes, scale=None):
        evac_rr[0] ^= 1
        if evac_rr[0]:
            if scale is None:
                return S.add("act", lambda e: e.activation(out=out_ap, in_=in_ap, func=AF.Copy), reads=reads, writes=writes)
            return S.add("act", lambda e: e.activation(out=out_ap, in_=in_ap, func=AF.Copy, scale=scale), reads=reads, writes=writes)
        if scale is None:
            return S.add("dve", lambda e: e.tensor_copy(out=out_ap, in_=in_ap), reads=reads, writes=writes)
        return S.add("dve", lambda e: e.tensor_scalar(out=out_ap, in0=in_ap, scalar1=scale, scalar2=None, op0=ALU.mult),
                     reads=reads, writes=writes)

    hTb = P.at(0, 16 * 4096, BF16).rearrange("p (k t) -> p k t", k=16)
    hTo = P.at(128 * KB, 16 * 1024, BF16).rearrange("p (k t) -> p k t", k=16)
    b_hTb = [Buf() for _ in range(32)]
    b_hTo = [Buf() for _ in range(8)]
    R0 = 160 * KB

    def norm_phase(srcs, gvec_dram, region, emit_tile):
        xs = [P.at(region + i * 8 * KB, 2048, F32) for i in range(2)]
        bxs = [Buf() for _ in range(2)]
        gt = P.at(region + 16 * KB, 2048, F32)
        b_gt = Buf()
        hn = [P.at(region + 24 * KB + i * 4 * KB, 2048, BF16) for i in range(2)]
        b_hn = [Buf() for _ in range(2)]
        junk = P.at(region + 32 * KB, 2048, BF16)
        b_junk = Buf()
        S.add("sp", lambda e: e.dma_start(out=gt, in_=gvec_dram.partition_broadcast(128)), writes=[b_gt], dkey="ld_g")
        for i, src in enumerate(srcs):
            s = i % 2
            st = sstat[:, 4 * s:4 * s + 4]
            S.add("sp", lambda e, src=src, s=s: e.dma_start(out=xs[s], in_=src), writes=[bxs[s]], dkey="ld_x%d" % s)
            S.add("act", lambda e, s=s, st=st: e.activation(out=junk, in_=xs[s], func=AF.Square, accum_out=st[:, 0:1]),
                  reads=[bxs[s]], writes=[b_junk, b_st[s]])
            S.add("dve", lambda e, st=st: e.tensor_scalar(out=st[:, 1:2], in0=st[:, 0:1], scalar1=1.0 / 2048, scalar2=1e-6,
                                                          op0=ALU.mult, op1=ALU.add), reads=[b_st[s]], writes=[b_st[s]])
            S.add("act", lambda e, st=st: e.activation(out=st[:, 2:3], in_=st[:, 1:2], func=AF.Sqrt), reads=[b_st[s]], writes=[b_st[s]])
            S.add("dve", lambda e, st=st: e.reciprocal(out=st[:, 3:4], in_=st[:, 2:3]), reads=[b_st[s]], writes=[b_st[s]])
            S.add("dve", lambda e, s=s, st=st: e.scalar_tensor_tensor(out=hn[s], in0=xs[s], scalar=st[:, 3:4], in1=gt,
                                                                     op0=ALU.mult, op1=ALU.mult),
                  reads=[bxs[s], b_st[s], b_gt], writes=[b_hn[s]])
            emit_tile(i, hn[s], b_hn[s], xs[s], bxs[s], st[:, 3:4], b_st[s])

    b_st = [Buf(), Buf()]

    def transpose_tile(hn_ap, b_hn, dst, b_dst, tcol):
        for half in range(2):
            bk = half
            for q in range(8):
                kc = half * 8 + q
                S.add("pe", lambda e, kc=kc, q=q, bk=bk: e.transpose(out=bank16(bk)[:, q * 128:(q + 1) * 128],
                                                                    in_=hn_ap[:, kc * 128:(kc + 1) * 128], identity=ident),
                      reads=[b_hn, b_const], writes=[bbuf[bk]])
            o = dst[:, half * 8:(half + 1) * 8, tcol:tcol + 128]
            i_ = bank16(bk).rearrange("p (k t) -> p k t", k=8)
            evac(o, i_, [bbuf[bk]], [b_dst])

    srcsA = [xb[t * 128:(t + 1) * 128, :] for t in range(32)] + [xo[t * 128:(t + 1) * 128, :] for t in range(8)]

    def emitA(i, hn_ap, b_hn, x_ap, b_x, rstd, bst):
        if i < 32:
            transpose_tile(hn_ap, b_hn, hTb, b_hTb[i], i * 128)
        else:
            transpose_tile(hn_ap, b_hn, hTo, b_hTo[i - 32], (i - 32) * 128)

    norm_phase(srcsA, norm_mix, R0, emitA)
    S.barrier()

    def dump_bf16(name, ap, n, parts=128):
        d = dout(name, [parts, n])
        tmp = P.at(160 * KB, n, F32, parts=parts)
        bt = Buf()
        S.add("dve", lambda e: e.tensor_copy(out=tmp, in_=ap), writes=[bt])
        S.final.append(S.add("sp", lambda e: e.dma_start(out=d, in_=tmp), reads=[bt], dkey="st_dbg"))

    if stop == "A":
        dump_bf16("d_hTb", hTb[:, 3, 0:2048], 2048)
        S.barrier()
        dump_bf16("d_hTo", hTo[:, 5, 0:1024], 1024)
        return nc, S, dbg

    wts = [P.at(R0 + i * 8 * KB, 16 * 256, BF16).rearrange("p (k c) -> p k c", k=16) for i in range(2)]
    b_wt = [Buf(), Buf()]
    stg = [P.at(R0 + 16 * KB + i * 8 * KB, 4096, BF16) for i in range(3)]
    b_stg = [Buf() for _ in range(3)]
    wslot = [0]
    sslot = [0]
    pbank = [0]

    def load_w(col0, ncols):
        s = wslot[0] % 2
        wslot[0] += 1
        src = w_in[:, col0:col0 + ncols].rearrange("(k p) c -> p k c", p=128)
        S.add("pool", lambda e: e.dma_start(out=wts[s][:, :, 0:ncols], in_=src), writes=[b_wt[s]], dkey="ld_w%d" % s)
        return wts[s], b_wt[s]

    def next_bank(lo=2, n=6):
        b = lo + pbank[0] % n
        pbank[0] += 1
        return b

    def next_stg():
        s = sslot[0] % 3
        sslot[0] += 1
        return s

    def proj_fm(wt, bw, c0, hT, b_hT, ntok, dst_dram, func=None, scale=None, tm=False):
        s = next_stg()
        for ch in range(ntok // 512):
            bk = next_bank()
            rb = [bw] + b_hT[ch * 4:(ch + 1) * 4]
            for kc in range(16):
                S.add("pe", lambda e, kc=kc, bk=bk, ch=ch: e.matmul(bank(bk), lhsT=wt[:, kc, c0:c0 + 128],
                                                                   rhs=hT[:, kc, ch * 512:(ch + 1) * 512],
                                                                   start=(kc == 0), stop=(kc == 15)),
                      reads=rb, writes=[bbuf[bk]])
            o = stg[s][:, ch * 512:(ch + 1) * 512]
            if func is None:
                evac(o, bank(bk), [bbuf[bk]], [b_stg[s]], scale=scale)
            else:
                S.add("act", lambda e, o=o, bk=bk: e.activation(out=o, in_=bank(bk), func=func), reads=[bbuf[bk]], writes=[b_stg[s]])
        if not tm:
            S.add("sp", lambda e: e.dma_start(out=dst_dram, in_=stg[s][:, 0:ntok]), reads=[b_stg[s]], dkey="st_stg%d" % s)
            return
        s2 = next_stg()
        for g8 in range(ntok // 1024):
            bk = next_bank()
            for q in range(8):
                t = g8 * 8 + q
                S.add("pe", lambda e, q=q, t=t, bk=bk: e.transpose(out=bank16(bk)[:, q * 128:(q + 1) * 128],
                                                                  in_=stg[s][:, t * 128:(t + 1) * 128], identity=ident),
                      reads=[b_stg[s], b_const], writes=[bbuf[bk]])
            evac(stg[s2][:, g8 * 1024:(g8 + 1) * 1024], bank16(bk), [bbuf[bk]], [b_stg[s2]])
        S.add("sp", lambda e: e.dma_start(out=dst_dram, in_=stg[s2][:, 0:ntok]), reads=[b_stg[s2]], dkey="st_stg%d" % s2)

    def pairs(base, n):
        return [(base + 256 * i) for i in range(n // 2)]

    ft_cols = [_IN_OFF["kc"], _IN_OFF["vc"], _IN_OFF["ksl"], _IN_OFF["kw"]] + pairs(_IN_OFF["kb"], 8)
    for pi, col0 in enumerate(ft_cols):
        if stop == "B3s":
            break
        wt, bw = load_w(col0, 256)
        for hh in range(2):
            proj_fm(wt, bw, hh * 128, hTb, b_hTb, 4096, FT[2 * pi + hh])
        if stop == "B1":
            break
    if stop == "B1":
        S.barrier()
        for i in range(2):
            a = P.at(176 * KB, 4096, BF16)
            bt = Buf()
            S.add("sp", lambda e, i=i, a=a: e.dma_start(out=a, in_=FT[i]), writes=[bt], dkey="ld_dbg")
            S.barrier()
            dump_bf16("d_FT%d" % i, a[:, 0:2048], 2048)
            S.barrier()
        return nc, S, dbg

    def dbg_exit(items):
        S.barrier()
        bt = Buf()
        for name, scr, i, w in items:
            a = P.at(32 * KB, w, BF16)
            S.add("sp", lambda e, a=a, scr=scr, i=i: e.dma_start(out=a, in_=scr[i]), writes=[bt], dkey="ld_dbg")
            S.barrier()
            dump_bf16("%s%d" % (name, i), a[:, 0:1024], 1024)
            S.barrier()
        return nc, S, dbg
    if stop == "B2":
        return dbg_exit([("d_FT", FT, 3, 4096), ("d_FT", FT, 15, 4096)])
    tm_cols = [_IN_OFF["vsl"], _IN_OFF["vw"]] + pairs(_IN_OFF["vb"], 8)
    def proj_tm(pi, wt, bw):
        s0, s1 = next_stg(), next_stg()
        for t2 in range(16):
            bk = next_bank()
            for tt in range(2):
                t = 2 * t2 + tt
                for kc in range(16):
                    S.add("pe", lambda e, kc=kc, bk=bk, t=t, tt=tt: e.matmul(bank(bk)[:, tt * 256:(tt + 1) * 256],
                                                                            lhsT=hTb[:, kc, t * 128:(t + 1) * 128],
                                                                            rhs=wt[:, kc, 0:256], start=(kc == 0), stop=(kc == 15)),
                          reads=[bw, b_hTb[t]], writes=[bbuf[bk]])
            for hh, s in ((0, s0), (1, s1)):
                o = stg[s][:, t2 * 256:(t2 + 1) * 256].rearrange("p (a d) -> p a d", a=2)
                i_ = bank(bk).rearrange("p (a h d) -> p a h d", a=2, h=2)[:, :, hh, :]
                evac(o, i_, [bbuf[bk]], [b_stg[s]])
        for hh, s in ((0, s0), (1, s1)):
            S.add("sp", lambda e, s=s, hh=hh, pi=pi: e.dma_start(out=TM[2 * pi + hh], in_=stg[s]), reads=[b_stg[s]],
                  dkey="st_stg%d" % s)

    for pi, col0 in enumerate(tm_cols):
        wt_, bw_ = load_w(col0, 256)
        for hh in range(2):
            proj_fm(wt_, bw_, hh * 128, hTb, b_hTb, 4096, TM[2 * pi + hh], tm=True)
        if stop in ("B3a", "B3s"):
            return dbg_exit([("d_TM", TM, 0, 4096), ("d_TM", TM, 1, 4096)])
    if stop == "B3":
        return dbg_exit([("d_TM", TM, 0, 4096), ("d_TM", TM, 11, 4096)])
    qs = 1.0 / math.sqrt(128.0)
    for pi, col0 in enumerate(pairs(_IN_OFF["qa"], 8) + pairs(_IN_OFF["qb"], 8)):
        wt, bw = load_w(col0, 256)
        for hh in range(2):
            proj_fm(wt, bw, hh * 128, hTo, b_hTo, 1024, QT[2 * pi + hh], scale=qs)
    if stop == "C1":
        return dbg_exit([("d_QT", QT, 0, 1024), ("d_QT", QT, 15, 1024)])
    for pi, col0 in enumerate(pairs(_IN_OFF["gma"], 16) + pairs(_IN_OFF["gmb"], 16)):
        wt, bw = load_w(col0, 256)
        for hh in range(2):
            proj_fm(wt, bw, hh * 128, hTo, b_hTo, 1024, GM[2 * pi + hh], func=AF.Sigmoid)
    def proj_gate(wt, bw):
      for ch in range(2):
        bk = next_bank()
        for kc in range(16):
            S.add("pe", lambda e, kc=kc, bk=bk, ch=ch: e.matmul(bank(bk)[0:24, :], lhsT=wt[:, kc, 0:24],
                                                               rhs=hTo[:, kc, ch * 512:(ch + 1) * 512],
                                                               start=(kc == 0), stop=(kc == 15)),
                  reads=[bw] + b_hTo[ch * 4:(ch + 1) * 4], writes=[bbuf[bk]])
        S.add("act", lambda e, bk=bk, ch=ch: e.activation(out=gT[:, ch * 512:(ch + 1) * 512], in_=bank(bk)[0:24, :],
                                                         func=AF.Sigmoid), reads=[bbuf[bk]], writes=[b_gT])

    if stop == "C2":
        return dbg_exit([("d_GM", GM, 0, 1024), ("d_GM", GM, 31, 1024)])
    wt_, bw_ = load_w(_IN_OFF["gate"], 24)
    proj_gate(wt_, bw_)
    S.barrier()

    if stop == "C":
        d1 = dout("d_gT", [24, 1024])
        tmp = P.at(0, 1024, F32, parts=24)
        bt = Buf()
        S.add("dve", lambda e: e.tensor_copy(out=tmp, in_=gT), reads=[b_gT], writes=[bt])
        S.final.append(S.add("sp", lambda e: e.dma_start(out=d1, in_=tmp), reads=[bt], dkey="st_dbg"))
        for name, scr, idxs, w in (("d_TM", TM, (0, 5, 11), 4096), ("d_QT", QT, (0, 9, 15), 1024), ("d_GM", GM, (0, 17, 31), 1024)):
            for i in idxs:
                a = P.at(32 * KB, w, BF16)
                S.add("sp", lambda e, a=a, scr=scr, i=i: e.dma_start(out=a, in_=scr[i]), writes=[bt], dkey="ld_dbg")
                S.barrier()
                dump_bf16("%s%d" % (name, i), a[:, 0:1024], 1024)
                S.barrier()
        S.final.append(S.add("sp", lambda e: e.dma_start(out=out[0:128, :], in_=P.at(0, 2048, F32)), reads=[bt], dkey="st_dbg"))
        return nc, S, dbg

    NCV = 16 if stop is None else 0
    CV0 = 64 - NCV
    cv_chunks = []
    cv_last = [None]
    if NCV:
        WBg = dscr("WBg", [NCV, 512, 2048])
        WBu = dscr("WBu", [NCV, 512, 2048])
        WBd = dscr("WBd", [NCV, 512, 2048])
        for ex in range(CV0, 64):
            for src_t, dst_t in ((w_eg, WBg), (w_eu, WBu), (w_ed, WBd)):
                srcv = src_t[ex].rearrange("a b -> (a b)").rearrange("(r c) -> r c", c=2048)
                for c4 in range(4):
                    cv_chunks.append((srcv[c4 * 128:(c4 + 1) * 128, :], dst_t[ex - CV0][c4 * 128:(c4 + 1) * 128, :]))
    cv_pos = [0]

    def issue_cv(n):
        for _ in range(n):
            if cv_pos[0] >= len(cv_chunks):
                return
            src, dst = cv_chunks[cv_pos[0]]
            cv_pos[0] += 1
            cv_last[0] = S.add("pool", lambda e, src=src, dst=dst: e.dma_start(out=dst, in_=src), dkey="cv")

    def cast_load(dst, src, writes, dkey, ncv=0):
        op = S.add("pool", lambda e: e.dma_start(out=dst, in_=src), writes=writes, dkey=dkey)
        issue_cv(ncv)
        return op

    def sp_load(dst, src, writes, dkey):
        return S.add("sp", lambda e: e.dma_start(out=dst, in_=src), writes=writes, dkey=dkey)

    oaf = P.at(0, 8 * 1024, F32).rearrange("p (h t) -> p h t", h=8)
    obT = P.at(32 * KB, 8 * 1024, BF16).rearrange("p (h t) -> p h t", h=8)
    oaT = P.at(48 * KB, 8 * 1024, BF16).rearrange("p (h t) -> p h t", h=8)
    b_oaf = [Buf() for _ in range(8)]
    b_obT = [Buf() for _ in range(8)]
    b_oaT = Buf()
    selbT = P.at(64 * KB, 2 * 1024, BF16, parts=64).rearrange("p (g t) -> p g t", g=2)
    b_selbT = [Buf(), Buf()]
    selbTm = P.at(68 * KB, 1024, BF16, parts=16)
    b_selbTm = Buf()
    kcmpT = P.at(70 * KB, 2 * 256, BF16).rearrange("p (g c) -> p g c", g=2)
    vcmp = P.at(71 * KB, 2 * 256, BF16).rearrange("p (g c d) -> p g c d", g=2, c=2)
    b_kcmp = [Buf(), Buf()]
    b_vcmp = [Buf(), Buf()]
    psel = P.at(72 * KB, 512, F32).rearrange("p (t n) -> p t n", t=8)
    b_psel = Buf()
    e64 = P.at(74 * KB, 4096, BF16, parts=64).rearrange("p (k s) -> p k s", k=32)
    e16 = P.at(82 * KB, 4096, BF16, parts=16).rearrange("p (k s) -> p k s", k=32)
    sel24 = P.at(90 * KB, 3072, BF16, parts=24).rearrange("p (i s) -> p i s", i=24)
    ovl = P.at(96 * KB, 130, BF16).rearrange("p (c n) -> p c n", c=2)
    fselT = P.at(97 * KB, 512, F32).rearrange("p (t n) -> p t n", t=8)
    pmn = P.at(99 * KB, 128, F32).rearrange("p (t n) -> p t n", t=8)
    p01 = P.at(99 * KB + 512, 128, F32).rearrange("p (t n) -> p t n", t=8)
    o01 = P.at(100 * KB, 128, F32).rearrange("p (t n) -> p t n", t=8)
    crp = P.at(100 * KB + 512, 16, F32)
    b_c2 = Buf()
    S.add("sp", [lambda e: e.dma_start(out=P.at(74 * KB, 4096, BF16, parts=64), in_=c_e64),
                 lambda e: e.dma_start(out=P.at(82 * KB, 4096, BF16, parts=16), in_=c_e16),
                 lambda e: e.dma_start(out=P.at(90 * KB, 3072, BF16, parts=24), in_=c_sel24),
                 lambda e: e.dma_start(out=P.at(96 * KB, 130, BF16), in_=c_ovl),
                 lambda e: e.dma_start(out=P.at(97 * KB, 512, F32), in_=fsel),
                 lambda e: e.dma_start(out=P.at(99 * KB, 128, F32), in_=pmneg),
                 lambda e: e.dma_start(out=P.at(99 * KB + 512, 128, F32), in_=past01),
                 lambda e: e.dma_start(out=P.at(100 * KB, 128, F32), in_=own01),
                 lambda e: e.dma_start(out=crp, in_=crep)], writes=[b_c2], dkey="const2")

    KTs = [P.at(104 * KB + i * 8 * KB, 4096, BF16) for i in range(2)]
    Vs = [P.at(120 * KB + i * 8 * KB, 4096, BF16).rearrange("p (t d) -> p t d", t=32) for i in range(2)]
    Vs_flat = [P.at(120 * KB + i * 8 * KB, 4096, BF16) for i in range(2)]
    QTs = [P.at(136 * KB + i * 2 * KB, 1024, BF16) for i in range(2)]
    b_KT = [Buf(), Buf()]
    b_V = [Buf(), Buf()]
    b_QT = [Buf(), Buf()]
    biasCb = P.at(140 * KB, 2048, BF16).rearrange("p (c q) -> p c q", c=2)
    b_biasC = Buf()
    bdraw = P.at(144 * KB, 640, F32)
    bdb = P.at(147 * KB, 640, BF16).rearrange("p (v q) -> p v q", v=5)
    bdb_flat = P.at(147 * KB, 640, BF16)
    b_bdraw, b_bdb = Buf(), Buf()
    bwb = P.at(149 * KB, 1024, BF16).rearrange("p (v q) -> p v q", v=8)
    bwb_flat = P.at(149 * KB, 1024, BF16)
    b_bwb = Buf()
    PTs = [P.at(152 * KB + i * KB, 512, BF16) for i in range(3)]
    b_PT = [Buf() for _ in range(3)]
    rden = P.at(155 * KB, 512, F32)
    tmpf = P.at(157 * KB, 512, F32)
    b_rden, b_tmpf = Buf(), Buf()
    w1b = P.at(160 * KB, 32 * 256, BF16).rearrange("p (l h) -> p l h", l=32)
    w1b_flat = P.at(160 * KB, 32 * 256, BF16)
    w2b = P.at(176 * KB, 256, BF16).rearrange("p (c d) -> p c d", c=2)
    w2b_flat = P.at(176 * KB, 256, BF16)
    peb = P.at(176 * KB + 512, 32, BF16)
    hid = P.at(177 * KB, 512, BF16).rearrange("p (c n) -> p c n", c=2)
    zf = P.at(178 * KB, 256, F32)
    uf = P.at(179 * KB, 256, F32)
    sgf = P.at(180 * KB, 256, F32)
    bh = P.at(181 * KB, 8, F32)
    b_w1, b_w2, b_pe, b_hid, b_z, b_u, b_sg, b_bh = [Buf() for _ in range(8)]
    sc = P.at(182 * KB, 64, F32)
    sc2 = P.at(182 * KB + 256, 64, F32)
    m8a = P.at(182 * KB + 512, 8, F32)
    m8b = P.at(182 * KB + 576, 8, F32)
    selm = P.at(183 * KB, 64, F32)
    selb = P.at(183 * KB + 256, 64, BF16)
    km = P.at(184 * KB, 16, F32)
    kmT = P.at(184 * KB + 64, 16, BF16)
    rd1 = P.at(184 * KB + 128, 8, F32)
    b_sc, b_km, b_rd1 = Buf(), Buf(), Buf()
    lrot = [0]
    ptrot = [0]

    def Lbank():
        b = lrot[0] % 4
        lrot[0] += 1
        return b

    def PTslot():
        s = ptrot[0] % 3
        ptrot[0] += 1
        return s

    def mm(outap, lhsT, rhs, start, stop, reads, bk):
        S.add("pe", lambda e: e.matmul(outap, lhsT=lhsT, rhs=rhs, start=start, stop=stop), reads=reads, writes=[bbuf[bk]])

    def load_head(slot, ft_idx, tm_idx, q_idx):
        if ft_idx is not None:
            sp_load(KTs[slot], FT[ft_idx], [b_KT[slot]], "ld_KT%d" % slot)
        if tm_idx is not None:
            sp_load(Vs_flat[slot], TM[tm_idx], [b_V[slot]], "ld_V%d" % slot)
        if q_idx is not None:
            sp_load(QTs[slot], QT[q_idx], [b_QT[slot]], "ld_Q%d" % slot)

    def finalize_nsa(h, cq, gi, first, bo=4, bd_=5):
        ch = slice(cq * 512, (cq + 1) * 512)
        gb = Lbank()
        S.add("dve", lambda e: e.tensor_scalar(out=rden, in0=bank(bd_), scalar1=1e-30, scalar2=None, op0=ALU.max),
              reads=[bbuf[bd_]], writes=[b_rden])
        S.add("dve", lambda e: e.reciprocal(out=rden, in_=rden), reads=[b_rden], writes=[b_rden])
        mm(bank(gb), sel24[:, gi, :], gT[:, ch], True, True, [b_c2, b_gT], gb)
        S.add("dve", lambda e: e.tensor_tensor(out=tmpf, in0=bank(bo), in1=rden, op=ALU.mult), reads=[bbuf[bo], b_rden], writes=[b_tmpf])
        if first:
            S.add("dve", lambda e: e.tensor_tensor(out=oaf[:, h, ch], in0=bank(gb), in1=tmpf, op=ALU.mult),
                  reads=[bbuf[gb], b_tmpf], writes=[b_oaf[h]])
        else:
            S.add("dve", lambda e: e.tensor_tensor(out=tmpf, in0=bank(gb), in1=tmpf, op=ALU.mult), reads=[bbuf[gb], b_tmpf], writes=[b_tmpf])
            S.add("dve", lambda e: e.tensor_tensor(out=oaf[:, h, ch], in0=oaf[:, h, ch], in1=tmpf, op=ALU.add),
                  reads=[b_tmpf, b_oaf[h]], writes=[b_oaf[h]])

    def finalize_moba(h, cq, bo=4, bd_=5):
        ch = slice(cq * 512, (cq + 1) * 512)
        S.add("dve", lambda e: e.tensor_scalar(out=rden, in0=bank(bd_), scalar1=1e-30, scalar2=None, op0=ALU.max),
              reads=[bbuf[bd_]], writes=[b_rden])
        S.add("dve", lambda e: e.reciprocal(out=rden, in_=rden), reads=[b_rden], writes=[b_rden])
        S.add("dve", lambda e: e.tensor_tensor(out=obT[:, h, ch], in0=bank(bo), in1=rden, op=ALU.mult),
              reads=[bbuf[bo], b_rden], writes=[b_obT[h]])

    odrot = [0]

    def run_pipe(stages, lag=2, extra=None):
        n = len(stages)
        extra = list(extra or [])
        for i in range(min(lag, n)):
            stages[i][0]()
        for i in range(n):
            if i + lag < n:
                stages[i + lag][0]()
            stages[i][1]()
            if extra and i % 2 == 1:
                extra.pop(0)()
        for f in extra:
            f()

    def attn_causal(ks, vs, qs_, selT, b_sel, E, fin, extra=None):
        KTa, Va, QTa = KTs[ks], Vs[vs], QTs[qs_]
        stages = []
        for cq in range(2):
            nkt = 16 * cq + 16
            odrot[0] ^= 1
            bo, bd_ = (4, 5) if odrot[0] else (6, 7)
            for kt in range(nkt):
                st = {}

                def s1(cq=cq, kt=kt, st=st):
                    s0 = max(0, -((3 - kt) // 4) - 4 * cq)
                    c0 = 128 * s0
                    q0 = cq * 512 + c0
                    q1 = cq * 512 + 512
                    bl = Lbank()
                    diag = []
                    for s_ in range(s0, 4):
                        v = kt - (4 * (4 * cq + s_) - 1)
                        if 0 <= v <= 4:
                            diag.append((s_, v))
                    mm(bank(bl)[:, c0:512], KTa[:, kt * 128:(kt + 1) * 128], QTa[:, q0:q1], True, False, [b_KT[ks], b_QT[qs_]], bl)
                    mm(bank(bl)[:, c0:512], E[:, kt, :], selT[:, q0:q1], False, len(diag) == 0, [b_c2, b_sel], bl)
                    for di, (s_, v) in enumerate(diag):
                        mm(bank(bl)[:, s_ * 128:(s_ + 1) * 128], ident, bdb[:, v, :], False, di == len(diag) - 1, [b_bdb, b_const], bl)
                    ps_ = PTslot()
                    S.add("act", lambda e: e.activation(out=PTs[ps_][:, c0:512], in_=bank(bl)[:, c0:512], func=AF.Exp),
                          reads=[bbuf[bl]], writes=[b_PT[ps_]])
                    st["ps"] = ps_
                    st["c0"] = c0

                def s2(cq=cq, kt=kt, st=st, nkt=nkt, bo=bo, bd_=bd_):
                    ps_, c0 = st["ps"], st["c0"]
                    mm(bank(bo)[:, c0:512], Va[:, kt, :], PTs[ps_][:, c0:512], kt == 0, kt == nkt - 1, [b_V[vs], b_PT[ps_]], bo)
                    mm(bank(bd_)[:, c0:512], ones, PTs[ps_][:, c0:512], kt == 0, kt == nkt - 1, [b_const, b_PT[ps_]], bd_)
                    if kt == nkt - 1:
                        fin(cq, bo, bd_)

                stages.append((s1, s2))
        run_pipe(stages, extra=extra)

    def prep_bd(hidx):
        sp_load(bdraw, bdiag[hidx], [b_bdraw], "ld_bd")
        S.add("dve", lambda e: e.tensor_scalar(out=bdb_flat, in0=bdraw, scalar1=crp[:, hidx:hidx + 1], scalar2=None, op0=ALU.subtract),
              reads=[b_bdraw, b_c2], writes=[b_bdb])

    def compress(g, w1d, w2d, ped, src_ft, is_v):
        cast_load(w1b, w1d.rearrange("(l p) h -> p l h", p=128), [b_w1], "ld_w1")
        cast_load(w2b, w2d.rearrange("(c p) d -> p c d", p=128), [b_w2], "ld_w2")
        cast_load(peb, ped, [b_pe], "ld_pe")
        sp_load(KTs[0], FT[src_ft], [b_KT[0]], "ld_KT0")
        for hc in range(2):
            for l in range(32):
                mm(bank(7)[:, hc:hc + 1], w1b[:, l, hc * 128:(hc + 1) * 128], peb[:, l:l + 1], l == 0, l == 31, [b_w1, b_pe], 7)
        S.add("dve", lambda e: e.tensor_copy(out=bh[:, 0:2], in_=bank(7)[:, 0:2]), reads=[bbuf[7]], writes=[b_bh])
        for hc in range(2):
            bl = Lbank()
            for l in range(32):
                mm(bank(bl)[:, 0:255], w1b[:, l, hc * 128:(hc + 1) * 128], KTs[0][:, l:l + 16 * 254 + 1:16], l == 0, l == 31,
                   [b_w1, b_KT[0]], bl)
            S.add("dve", lambda e, bl=bl, hc=hc: e.tensor_scalar(out=zf[:, 0:255], in0=bank(bl)[:, 0:255], scalar1=bh[:, hc:hc + 1],
                                                                scalar2=None, op0=ALU.add), reads=[bbuf[bl], b_bh], writes=[b_z])
            S.add("dve", lambda e: e.tensor_tensor(out=uf[:, 0:255], in0=zf[:, 0:255], in1=zf[:, 0:255], op=ALU.mult), reads=[b_z], writes=[b_u])
            S.add("dve", lambda e: e.tensor_scalar(out=uf[:, 0:255], in0=uf[:, 0:255], scalar1=0.044715, scalar2=1.0, op0=ALU.mult, op1=ALU.add),
                  reads=[b_u], writes=[b_u])
            S.add("dve", lambda e: e.tensor_tensor(out=uf[:, 0:255], in0=uf[:, 0:255], in1=zf[:, 0:255], op=ALU.mult), reads=[b_u, b_z], writes=[b_u])
            S.add("act", lambda e: e.activation(out=sgf[:, 0:255], in_=uf[:, 0:255], func=AF.Sigmoid, scale=1.5957691216057308),
                  reads=[b_u], writes=[b_sg])
            S.add("dve", lambda e, hc=hc: e.tensor_tensor(out=hid[:, hc, 0:255], in0=zf[:, 0:255], in1=sgf[:, 0:255], op=ALU.mult),
                  reads=[b_z, b_sg], writes=[b_hid])
        if not is_v:
            bl = Lbank()
            for hc in range(2):
                mm(bank(bl)[:, 0:255], w2b[:, hc, :], hid[:, hc, 0:255], hc == 0, hc == 1, [b_w2, b_hid], bl)
            S.add("dve", lambda e: e.memset(kcmpT[:, g, :], 0.0), writes=[b_kcmp[g]])
            S.add("dve", lambda e, bl=bl: e.tensor_copy(out=kcmpT[:, g, 0:255], in_=bank(bl)[:, 0:255]), reads=[bbuf[bl]], writes=[b_kcmp[g]])
        else:
            S.add("dve", lambda e: e.memset(vcmp[:, g, :, :], 0.0), writes=[b_vcmp[g]])
            for ct in range(2):
                n = 128 if ct == 0 else 127
                bl = Lbank()
                for hc in range(2):
                    mm(bank(bl)[0:n, 0:128], hid[:, hc, ct * 128:ct * 128 + n], w2b[:, hc, :], hc == 0, hc == 1, [b_w2, b_hid], bl)
                S.add("dve", lambda e, bl=bl, ct=ct, n=n: e.tensor_copy(out=vcmp[0:n, g, ct, :], in_=bank(bl)[0:n, 0:128]),
                      reads=[bbuf[bl]], writes=[b_vcmp[g]])

    for g in range(2):
        compress(g, w1_k, w2_k, peT_k, 0 + g, False)
        compress(g, w1_v, w2_v, peT_v, 2 + g, True)

    for g in range(2):
        for jj in range(4):
            h = 4 * g + jj
            qs_ = h % 2
            load_head(qs_, None, None, h)
            cast_load(biasCb, biasC[h].rearrange("(c p) q -> p c q", p=128), [b_biasC], "ld_bc", ncv=7)
            for cq in range(2):
                ch = slice(cq * 512, (cq + 1) * 512)
                pts = []
                for ct in range(2):
                    bl = Lbank()
                    mm(bank(bl), kcmpT[:, g, ct * 128:(ct + 1) * 128], QTs[qs_][:, ch], True, False, [b_kcmp[g], b_QT[qs_]], bl)
                    mm(bank(bl), ident, biasCb[:, ct, ch], False, True, [b_biasC, b_const], bl)
                    ps_ = PTslot()
                    pts.append(ps_)
                    S.add("act", lambda e, bl=bl, ps_=ps_: e.activation(out=PTs[ps_], in_=bank(bl), func=AF.Exp),
                          reads=[bbuf[bl]], writes=[b_PT[ps_]])
                for ct in range(2):
                    mm(bank(4), vcmp[:, g, ct, :], PTs[pts[ct]], ct == 0, ct == 1, [b_vcmp[g], b_PT[pts[ct]]], 4)
                for ct in range(2):
                    mm(bank(5), ones, PTs[pts[ct]], ct == 0, ct == 1, [b_const, b_PT[pts[ct]]], 5)
                for s in range(4):
                    for ct in range(2):
                        mm(bank(7)[:, s * 65:(s + 1) * 65], PTs[pts[ct]][:, s * 128:(s + 1) * 128], ovl[:, ct, :], ct == 0, ct == 1,
                           [b_c2, b_PT[pts[ct]]], 7)
                for s in range(4):
                    t = 4 * cq + s
                    S.add("dve", lambda e, s=s: e.tensor_scalar(out=rd1[:, 0:1], in0=bank(7)[:, s * 65 + 64:s * 65 + 65], scalar1=1e-30,
                                                               scalar2=None, op0=ALU.max), reads=[bbuf[7]], writes=[b_rd1])
                    S.add("dve", lambda e: e.reciprocal(out=rd1[:, 0:1], in_=rd1[:, 0:1]), reads=[b_rd1], writes=[b_rd1])
                    if jj == 0:
                        S.add("dve", lambda e, s=s, t=t: e.tensor_scalar(out=psel[:, t, :], in0=bank(7)[:, s * 65:s * 65 + 64],
                                                                        scalar1=rd1[:, 0:1], scalar2=None, op0=ALU.mult),
                              reads=[bbuf[7], b_rd1], writes=[b_psel])
                    else:
                        S.add("dve", lambda e, s=s, t=t: e.scalar_tensor_tensor(out=psel[:, t, :], in0=bank(7)[:, s * 65:s * 65 + 64],
                                                                               scalar=rd1[:, 0:1], in1=psel[:, t, :], op0=ALU.mult, op1=ALU.add),
                              reads=[bbuf[7], b_rd1, b_psel], writes=[b_psel])
                finalize_nsa(h, cq, 3 * h + 0, True)
        for t in range(8):
            S.add("dve", lambda e, t=t: e.tensor_tensor(out=sc, in0=psel[:, t, :], in1=fselT[:, t, :], op=ALU.add), reads=[b_psel, b_c2], writes=[b_sc])
            S.add("dve", lambda e: e.max(out=m8a, in_=sc), reads=[b_sc], writes=[b_sc])
            S.add("dve", lambda e: e.match_replace(out=sc2, in_to_replace=m8a, in_values=sc, imm_value=-1e30), reads=[b_sc], writes=[b_sc])
            S.add("dve", lambda e: e.max(out=m8b, in_=sc2), reads=[b_sc], writes=[b_sc])
            S.add("dve", lambda e: e.tensor_scalar(out=selm, in0=sc, scalar1=m8b[:, 7:8], scalar2=None, op0=ALU.is_ge), reads=[b_sc], writes=[b_sc])
            S.add("dve", lambda e: e.tensor_scalar(out=selb, in0=selm, scalar1=-NEG, scalar2=NEG, op0=ALU.mult, op1=ALU.add), reads=[b_sc], writes=[b_sc])
            S.add("pe", lambda e: e.transpose(out=bank16(7)[0:64, 0:128], in_=selb, identity=ident), reads=[b_sc, b_const], writes=[bbuf[7]])
            S.add("dve", lambda e, t=t, g=g: e.tensor_copy(out=selbT[:, g, t * 128:(t + 1) * 128], in_=bank16(7)[0:64, 0:128]),
                  reads=[bbuf[7]], writes=[b_selbT[g]])
        load_head(0, 4 + g, 0 + g, None)
        for jj in range(4):
            h = 4 * g + jj
            qs_ = h % 2
            load_head(qs_, None, None, h)
            prep_bd(h)
            attn_causal(0, 0, qs_, selbT[:, g, :], b_selbT[g], e64, lambda cq, bo, bd_, h=h: finalize_nsa(h, cq, 3 * h + 1, False, bo, bd_))
        load_head(1, 6 + g, 2 + g, None)
        for jj in range(4):
            h = 4 * g + jj
            qs_ = h % 2
            load_head(qs_, None, None, h)
            cast_load(bwb_flat, bwin[h], [b_bwb], "ld_bw", ncv=7)
            stages = []
            for cq in range(2):
                odrot[0] ^= 1
                bo, bd_ = (4, 5) if odrot[0] else (6, 7)
                for s in range(4):
                    m = 4 * cq + s
                    kts = list(range(max(0, 4 * m - 4), 4 * m + 4))
                    for ki, kt in enumerate(kts):
                        st = {}

                        def s1(m=m, kt=kt, st=st, qs_=qs_):
                            v = kt - (4 * m - 4)
                            bl = Lbank()
                            mm(bank(bl)[:, 0:128], KTs[1][:, kt * 128:(kt + 1) * 128], QTs[qs_][:, m * 128:(m + 1) * 128], True, False,
                               [b_KT[1], b_QT[qs_]], bl)
                            mm(bank(bl)[:, 0:128], ident, bwb[:, v, :], False, True, [b_bwb, b_const], bl)
                            ps_ = PTslot()
                            S.add("act", lambda e: e.activation(out=PTs[ps_][:, 0:128], in_=bank(bl)[:, 0:128], func=AF.Exp),
                                  reads=[bbuf[bl]], writes=[b_PT[ps_]])
                            st["ps"] = ps_

                        def s2(s=s, kt=kt, ki=ki, nk=len(kts), st=st, bo=bo, bd_=bd_, cq=cq, h=h):
                            ps_ = st["ps"]
                            mm(bank(bo)[:, s * 128:(s + 1) * 128], Vs[1][:, kt, :], PTs[ps_][:, 0:128], ki == 0, ki == nk - 1,
                               [b_V[1], b_PT[ps_]], bo)
                            mm(bank(bd_)[:, s * 128:(s + 1) * 128], ones, PTs[ps_][:, 0:128], ki == 0, ki == nk - 1,
                               [b_const, b_PT[ps_]], bd_)
                            if s == 3 and ki == nk - 1:
                                finalize_nsa(h, cq, 3 * h + 2, False, bo, bd_)

                        stages.append((s1, s2))
            run_pipe(stages)

    selbTm2 = [selbTm, P.at(186 * KB, 1024, BF16, parts=16)]
    b_selbTm2 = [b_selbTm, Buf()]
    kmT2 = [kmT, P.at(184 * KB + 256, 16, BF16)]
    b_km2 = [b_km, Buf()]

    def moba_sel_steps(h):
        sl = h % 2
        steps = []

        def kstep():
            S.add("dve", lambda e: e.reduce_sum(out=km, in_=KTs[sl].rearrange("p (n s) -> p n s", n=16), axis=AX.X),
                  reads=[b_KT[sl]], writes=[b_sc])
            S.add("dve", lambda e: e.tensor_scalar(out=kmT2[sl], in0=km, scalar1=1.0 / 256, scalar2=None, op0=ALU.mult), reads=[b_sc], writes=[b_km2[sl]])
        steps.append(kstep)
        for t in range(8):
            def a_step(t=t):
                bl = Lbank()
                mm(bank(bl)[:, 0:16], QTs[sl][:, t * 128:(t + 1) * 128], kmT2[sl], True, True, [b_QT[sl], b_km2[sl]], bl)
                S.add("dve", lambda e: e.tensor_tensor(out=sc[:, 0:16], in0=bank(bl)[:, 0:16], in1=pmn[:, t, :], op=ALU.add),
                      reads=[bbuf[bl], b_c2], writes=[b_sc])
                S.add("dve", lambda e: e.max(out=m8a, in_=sc[:, 0:16]), reads=[b_sc], writes=[b_sc])
                S.add("dve", lambda e: e.tensor_scalar(out=selm[:, 0:16], in0=sc[:, 0:16], scalar1=m8a[:, 2:3], scalar2=None, op0=ALU.is_ge),
                      reads=[b_sc], writes=[b_sc])
                S.add("dve", lambda e: e.tensor_tensor(out=selm[:, 0:16], in0=selm[:, 0:16], in1=p01[:, t, :], op=ALU.mult),
                      reads=[b_sc, b_c2], writes=[b_sc])
                S.add("dve", lambda e: e.tensor_tensor(out=selm[:, 0:16], in0=selm[:, 0:16], in1=o01[:, t, :], op=ALU.add),
                      reads=[b_sc, b_c2], writes=[b_sc])
                S.add("dve", lambda e: e.tensor_scalar(out=selb[:, 0:16], in0=selm[:, 0:16], scalar1=-NEG, scalar2=NEG, op0=ALU.mult, op1=ALU.add),
                      reads=[b_sc], writes=[b_sc])

            def b_step(t=t):
                bl = Lbank()
                S.add("pe", lambda e: e.transpose(out=bank16(bl)[0:16, 0:128], in_=selb[:, 0:16], identity=ident), reads=[b_sc, b_const], writes=[bbuf[bl]])
                S.add("dve", lambda e: e.tensor_copy(out=selbTm2[sl][:, t * 128:(t + 1) * 128], in_=bank16(bl)[0:16, 0:128]),
                      reads=[bbuf[bl]], writes=[b_selbTm2[sl], b_sc])
            steps.append(a_step)
            steps.append(b_step)
        return steps

    load_head(0, 8, 4, 8)
    for f in moba_sel_steps(0):
        f()
    for h in range(8):
        sl = h % 2
        extra = None
        if h + 1 < 8:
            load_head((h + 1) % 2, 8 + h + 1, 4 + h + 1, 8 + h + 1)
            extra = moba_sel_steps(h + 1)
        prep_bd(8 + h)
        attn_causal(sl, sl, sl, selbTm2[sl], b_selbTm2[sl], e16, lambda cq, bo, bd_, h=h: finalize_moba(h, cq, bo, bd_), extra=extra)
    for h in range(8):
        S.add("dve", lambda e, h=h: e.tensor_copy(out=oaT[:, h, :], in_=oaf[:, h, :]), reads=[b_oaf[h]], writes=[b_oaT])
    S.barrier()

    def dump_many(items):
        for name, ap, n, parts in items:
            S.barrier()
            dump_bf16(name, ap, n, parts)
        S.barrier()

    if stop == "D":
        items = [("d_oa%d" % h, oaT[:, h, :], 1024, 128) for h in range(8)] + [("d_ob%d" % h, obT[:, h, :], 1024, 128) for h in range(8)]
        items += [("d_kcmp%d" % g, kcmpT[:, g, :], 256, 128) for g in range(2)]
        items += [("d_vcmp%d" % g, vcmp[:, g, :, :].rearrange("p c d -> p (c d)"), 256, 128) for g in range(2)]
        items += [("d_selbT%d" % g, selbT[:, g, :], 1024, 64) for g in range(2)]
        dump_many(items)
        return nc, S, dbg
    wupA = P.at(64 * KB, 8 * 2048, BF16).rearrange("p (h c) -> p h c", h=8)
    wupB = P.at(96 * KB, 8 * 2048, BF16).rearrange("p (h c) -> p h c", h=8)
    b_wup = [Buf(), Buf()]
    for hh in range(8):
        cast_load(wupA[:, hh, :], w_up_a[hh * 128:(hh + 1) * 128, :], [b_wup[0]], "ld_wupa", ncv=5)
        cast_load(wupB[:, hh, :], w_up_b[hh * 128:(hh + 1) * 128, :], [b_wup[1]], "ld_wupb")
    mT = P.at(128 * KB, 16 * 1024, BF16).rearrange("p (k t) -> p k t", k=16)
    b_mT = [Buf() for _ in range(8)]
    gms = [P.at(160 * KB + i * 2 * KB, 1024, BF16) for i in range(4)]
    b_gm = [Buf() for _ in range(4)]
    t1 = P.at(168 * KB, 512, F32)
    t2 = P.at(170 * KB, 512, F32)
    b_t1, b_t2 = Buf(), Buf()
    for ct in range(16):
        sa, sb_ = (ct % 2) * 2, (ct % 2) * 2 + 1
        sp_load(gms[sa], GM[ct], [b_gm[sa]], "ld_gm%d" % sa)
        sp_load(gms[sb_], GM[16 + ct], [b_gm[sb_]], "ld_gm%d" % sb_)
        for cq in range(2):
            ch = slice(cq * 512, (cq + 1) * 512)
            ba, bb2 = Lbank(), Lbank()
            for hh in range(8):
                mm(bank(ba), wupA[:, hh, ct * 128:(ct + 1) * 128], oaT[:, hh, ch], hh == 0, hh == 7, [b_wup[0], b_oaT], ba)
            for hh in range(8):
                mm(bank(bb2), wupB[:, hh, ct * 128:(ct + 1) * 128], obT[:, hh, ch], hh == 0, hh == 7, [b_wup[1]] + b_obT, bb2)
            S.add("dve", lambda e, ba=ba, sa=sa, ch=ch: e.tensor_tensor(out=t1, in0=bank(ba), in1=gms[sa][:, ch], op=ALU.mult),
                  reads=[bbuf[ba], b_gm[sa]], writes=[b_t1])
            S.add("dve", lambda e, bb2=bb2, sb_=sb_, ch=ch: e.tensor_tensor(out=t2, in0=bank(bb2), in1=gms[sb_][:, ch], op=ALU.mult),
                  reads=[bbuf[bb2], b_gm[sb_]], writes=[b_t2])
            S.add("dve", lambda e, ct=ct, ch=ch: e.tensor_tensor(out=mT[:, ct, ch], in0=t1, in1=t2, op=ALU.add),
                  reads=[b_t1, b_t2], writes=b_mT[cq * 4:(cq + 1) * 4])
    S.barrier()
    x2 = P.at(64 * KB, 8 * 2048, F32).rearrange("p (t c) -> p t c", t=8)
    b_x2 = [Buf() for _ in range(8)]
    for t in range(8):
        sp_load(x2[:, t, :], xo[t * 128:(t + 1) * 128, :], [b_x2[t]], "ld_x2")
    wos = [P.at(i * 16 * KB, 16 * 512, BF16).rearrange("p (k c) -> p k c", k=16) for i in range(2)]
    b_wo = [Buf(), Buf()]
    for dg in range(4):
        s = dg % 2
        cast_load(wos[s], w_out[:, dg * 512:(dg + 1) * 512].rearrange("(k p) c -> p k c", p=128), [b_wo[s]], "ld_wo%d" % s)
        for t in range(8):
            bl = Lbank()
            for kc in range(16):
                mm(bank(bl), mT[:, kc, t * 128:(t + 1) * 128], wos[s][:, kc, :], kc == 0, kc == 15, [b_mT[t], b_wo[s]], bl)
            S.add("dve", lambda e, bl=bl, t=t, dg=dg: e.tensor_tensor(out=x2[:, t, dg * 512:(dg + 1) * 512], in0=bank(bl),
                                                                     in1=x2[:, t, dg * 512:(dg + 1) * 512], op=ALU.add),
                  reads=[bbuf[bl], b_x2[t]], writes=[b_x2[t]])
    S.barrier()

    if stop == "F":
        d = dout("d_x2", [128, 8 * 2048])
        S.final.append(S.add("sp", lambda e: e.dma_start(out=d, in_=P.at(64 * KB, 8 * 2048, F32)), reads=b_x2, dkey="st_dbg"))
        return nc, S, dbg
    hn2 = P.at(128 * KB, 8 * 2048, BF16).rearrange("p (t c) -> p t c", t=8)
    hn2T = P.at(160 * KB, 16 * 1024, BF16).rearrange("p (k t) -> p k t", k=16)
    b_hn2 = [Buf() for _ in range(8)]
    b_hn2T = [Buf() for _ in range(8)]
    gff = P.at(0, 2048, F32)
    b_gff = Buf()
    junk2 = P.at(8 * KB, 2048, BF16)
    b_junk2 = Buf()
    S.add("sp", lambda e: e.dma_start(out=gff, in_=norm_ffn.partition_broadcast(128)), writes=[b_gff], dkey="ld_g")
    for t in range(8):
        st = sstat[:, 8 + 4 * (t % 2):12 + 4 * (t % 2)]
        bs = b_st[t % 2]
        S.add("act", lambda e, t=t, st=st: e.activation(out=junk2, in_=x2[:, t, :], func=AF.Square, accum_out=st[:, 0:1]),
              reads=[b_x2[t]], writes=[b_junk2, bs])
        S.add("dve", lambda e, st=st: e.tensor_scalar(out=st[:, 1:2], in0=st[:, 0:1], scalar1=1.0 / 2048, scalar2=1e-6, op0=ALU.mult, op1=ALU.add),
              reads=[bs], writes=[bs])
        S.add("act", lambda e, st=st: e.activation(out=st[:, 2:3], in_=st[:, 1:2], func=AF.Sqrt), reads=[bs], writes=[bs])
        S.add("dve", lambda e, st=st: e.reciprocal(out=st[:, 3:4], in_=st[:, 2:3]), reads=[bs], writes=[bs])
        S.add("dve", lambda e, t=t, st=st: e.scalar_tensor_tensor(out=hn2[:, t, :], in0=x2[:, t, :], scalar=st[:, 3:4], in1=gff,
                                                                 op0=ALU.mult, op1=ALU.mult), reads=[b_x2[t], bs, b_gff], writes=[b_hn2[t]])
        transpose_tile(hn2[:, t, :], b_hn2[t], hn2T, b_hn2T[t], t * 128)
    wr = P.at(12 * KB, 16 * 72, BF16).rearrange("p (k c) -> p k c", k=16)
    brt = P.at(16 * KB, 72, F32)
    b_wr, b_brt = Buf(), Buf()
    cast_load(wr, w_rt.rearrange("(k p) c -> p k c", p=128), [b_wr], "ld_wr")
    S.add("sp", lambda e: e.dma_start(out=brt, in_=b_rt.partition_broadcast(128)), writes=[b_brt], dkey="ld_brt")
    ustr = P.at(17 * KB, 128, BF16)
    iot = P.at(63 * KB, 128, F32)
    b_c3 = Buf()
    S.add("sp", [lambda e: e.dma_start(out=ustr, in_=c_ustr), lambda e: e.dma_start(out=iot, in_=c_iota)], writes=[b_c3], dkey="const3")
    A1h = P.at(194 * KB, 512, BF16).rearrange("p (t n) -> p t n", t=8)
    A2h = P.at(195 * KB, 512, BF16).rearrange("p (t n) -> p t n", t=8)
    W12 = P.at(199 * KB, 16, F32)
    YY = dscr("YY", [8192, 2048])
    Ind = P.at(196 * KB, 512, BF16).rearrange("p (t n) -> p t n", t=8)
    Rk = P.at(197 * KB, 512, F32).rearrange("p (t n) -> p t n", t=8)
    b_Wm, b_Ind, b_Rk = Buf(), Buf(), Buf()
    rl = P.at(20 * KB, 72, F32)
    Lm = P.at(21 * KB, 64, F32)
    mg = P.at(22 * KB, 8, F32)
    pen = P.at(22 * KB + 64, 8, F32)
    m8r = P.at(22 * KB + 128, 8, F32)
    sm = P.at(22 * KB + 192, 16, F32)
    a1 = P.at(23 * KB, 64, F32)
    a2 = P.at(23 * KB + 256, 64, F32)
    b_r = Buf()
    for t in range(8):
        for kc in range(16):
            mm(bank(7)[:, 0:72], hn2T[:, kc, t * 128:(t + 1) * 128], wr[:, kc, :], kc == 0, kc == 15, [b_hn2T[t], b_wr], 7)
        R = [b_r]
        S.add("dve", lambda e: e.tensor_tensor(out=rl, in0=bank(7)[:, 0:72], in1=brt, op=ALU.add), reads=[bbuf[7], b_brt], writes=R)
        S.add("dve", lambda e: e.reduce_max(out=sm[:, 0:1], in_=rl[:, 0:8], axis=AX.X), reads=R, writes=R)
        S.add("dve", lambda e: e.tensor_scalar(out=mg, in0=rl[:, 0:8], scalar1=sm[:, 0:1], scalar2=None, op0=ALU.is_ge), reads=R, writes=R)
        S.add("dve", lambda e: e.tensor_scalar(out=sm[:, 1:2], in0=sm[:, 0:1], scalar1=-1.0, scalar2=None, op0=ALU.mult), reads=R, writes=R)
        S.add("act", lambda e: e.activation(out=a1[:, 0:8], in_=rl[:, 0:8], func=AF.Exp, bias=sm[:, 1:2], accum_out=sm[:, 2:3]), reads=R, writes=R)
        S.add("dve", lambda e: e.reciprocal(out=sm[:, 3:4], in_=sm[:, 2:3]), reads=R, writes=R)
        S.add("dve", lambda e: e.tensor_scalar(out=pen, in0=mg, scalar1=1e30, scalar2=-1e30, op0=ALU.mult, op1=ALU.add), reads=R, writes=R)
        S.add("dve", lambda e: e.tensor_tensor(out=Lm.rearrange("p (g x) -> p g x", g=8), in0=rl[:, 8:72].rearrange("p (g x) -> p g x", g=8),
                                               in1=mg.unsqueeze(2).to_broadcast([128, 8, 8]), op=ALU.mult), reads=R, writes=R)
        S.add("dve", lambda e: e.tensor_tensor(out=Lm.rearrange("p (g x) -> p g x", g=8), in0=Lm.rearrange("p (g x) -> p g x", g=8),
                                               in1=pen.unsqueeze(2).to_broadcast([128, 8, 8]), op=ALU.add), reads=R, writes=R)
        S.add("dve", lambda e: e.max(out=m8r, in_=Lm), reads=R, writes=R)
        S.add("dve", lambda e: e.tensor_tensor(out=sm[:, 4:5], in0=m8r[:, 0:1], in1=m8r[:, 1:2], op=ALU.subtract), reads=R, writes=R)
        S.add("act", lambda e: e.activation(out=sm[:, 5:6], in_=sm[:, 4:5], func=AF.Sigmoid), reads=R, writes=R)
        S.add("dve", lambda e: e.tensor_tensor(out=sm[:, 6:7], in0=sm[:, 5:6], in1=sm[:, 3:4], op=ALU.mult), reads=R, writes=R)
        S.add("dve", lambda e: e.tensor_tensor(out=sm[:, 7:8], in0=sm[:, 3:4], in1=sm[:, 6:7], op=ALU.subtract), reads=R, writes=R)
        S.add("dve", lambda e: e.tensor_scalar(out=a1, in0=Lm, scalar1=m8r[:, 0:1], scalar2=sm[:, 6:7], op0=ALU.is_equal, op1=ALU.mult), reads=R, writes=R)
        S.add("dve", lambda e: e.tensor_scalar(out=a2, in0=Lm, scalar1=m8r[:, 1:2], scalar2=sm[:, 7:8], op0=ALU.is_equal, op1=ALU.mult), reads=R, writes=R)
        S.add("dve", lambda e, t=t: e.tensor_scalar(out=A1h[:, t, :], in0=Lm, scalar1=m8r[:, 0:1], scalar2=None, op0=ALU.is_equal), reads=R, writes=[b_Wm])
        S.add("dve", lambda e, t=t: e.tensor_scalar(out=A2h[:, t, :], in0=Lm, scalar1=m8r[:, 1:2], scalar2=None, op0=ALU.is_equal), reads=R, writes=[b_Wm])
        S.add("dve", lambda e, t=t: e.tensor_copy(out=W12[:, 2 * t:2 * t + 2], in_=sm[:, 6:8]), reads=R, writes=[b_Wm])
        S.add("dve", lambda e, t=t: e.tensor_scalar(out=Ind[:, t, :], in0=Lm, scalar1=m8r[:, 1:2], scalar2=None, op0=ALU.is_ge), reads=R, writes=[b_Ind])
    for t in range(8):
        for tp in range(t + 1):
            mm(bank(7)[:, 0:64], ustr if tp == t else ones, Ind[:, tp, :], tp == 0, tp == t, [b_Ind, b_c3, b_const], 7)
        S.add("dve", lambda e, t=t: e.tensor_scalar(out=Rk[:, t, :], in0=bank(7)[:, 0:64], scalar1=1.0, scalar2=None, op0=ALU.add),
              reads=[bbuf[7]], writes=[b_Rk])
        S.add("dve", lambda e, t=t: e.tensor_tensor(out=Rk[:, t, :], in0=Rk[:, t, :], in1=Ind[:, t, :], op=ALU.mult), reads=[b_Rk, b_Ind], writes=[b_Rk])
        S.add("dve", lambda e, t=t: e.tensor_scalar(out=Rk[:, t, :], in0=Rk[:, t, :], scalar1=-1.0, scalar2=None, op0=ALU.add),
              reads=[b_Rk], writes=[b_Rk])
    S.barrier()
    wgs = [P.at(0, 16 * 512, BF16).rearrange("p (k f) -> p k f", k=16), P.at(160 * KB, 16 * 512, BF16).rearrange("p (k f) -> p k f", k=16)]
    wus = [P.at(16 * KB, 16 * 512, BF16).rearrange("p (k f) -> p k f", k=16), P.at(176 * KB, 16 * 512, BF16).rearrange("p (k f) -> p k f", k=16)]
    wd = P.at(32 * KB, 4 * 2048, BF16).rearrange("p (f d) -> p f d", f=4)
    wgs_flat = [P.at(0, 16 * 512, BF16).rearrange("p (a b) -> p a b", b=2048), P.at(160 * KB, 16 * 512, BF16).rearrange("p (a b) -> p a b", b=2048)]
    wus_flat = [P.at(16 * KB, 16 * 512, BF16).rearrange("p (a b) -> p a b", b=2048), P.at(176 * KB, 16 * 512, BF16).rearrange("p (a b) -> p a b", b=2048)]
    b_wg, b_wu, b_wd = [Buf(), Buf()], [Buf(), Buf()], Buf()
    Sel = P.at(48 * KB, 1024, BF16).rearrange("p (t r) -> p t r", t=8)
    XeT = P.at(50 * KB, 2048, BF16).rearrange("p (k r) -> p k r", k=16)
    HT = P.at(54 * KB, 512, BF16).rearrange("p (f r) -> p f r", f=4)
    Ysbs = [P.at(55 * KB, 2048, BF16), P.at(59 * KB, 2048, BF16)]
    b_Ysbs = [Buf(), Buf()]
    sgt = P.at(192 * KB, 512, F32)
    b_Sel, b_XeT, b_HT, b_sgt = [Buf() for _ in range(4)]
    Sel2 = [Sel, P.at(203 * KB, 1024, BF16).rearrange("p (t r) -> p t r", t=8)]
    b_Sel2 = [b_Sel, Buf()]

    def gen_sel(ex):
        sl = ex % 2
        for t in range(8):
            S.add("dve", lambda e, t=t: e.tensor_scalar(out=Sel2[sl][:, t, :], in0=iot, scalar1=Rk[:, t, ex:ex + 1], scalar2=None,
                                                       op0=ALU.is_equal), reads=[b_c3, b_Rk], writes=[b_Sel2[sl]])

    def load_exp(ex):
        s = ex % 2
        if ex >= CV0:
            i = ex - CV0
            ex_dep = [cv_last[0]]
            S.add("sp", lambda e: e.dma_start(out=wgs_flat[s], in_=WBg[i].rearrange("a b -> (a b)").rearrange("(p a b) -> p a b", p=128, b=2048)),
                  writes=[b_wg[s]], dkey="ld_wg%d" % s, extra=ex_dep)
            S.add("sp", lambda e: e.dma_start(out=wus_flat[s], in_=WBu[i].rearrange("a b -> (a b)").rearrange("(p a b) -> p a b", p=128, b=2048)),
                  writes=[b_wu[s]], dkey="ld_wu%d" % s, extra=ex_dep)
            return
        S.add("pool", lambda e: e.dma_start(out=wgs_flat[s], in_=w_eg[ex].rearrange("(p k) f -> p (k f)", k=16).rearrange("p (a b) -> p a b", b=2048)),
              writes=[b_wg[s]], dkey="ld_wg%d" % s)
        S.add("pool", lambda e: e.dma_start(out=wus_flat[s], in_=w_eu[ex].rearrange("(p k) f -> p (k f)", k=16).rearrange("p (a b) -> p a b", b=2048)),
              writes=[b_wu[s]], dkey="ld_wu%d" % s)

    def load_wd(ex):
        if ex >= CV0:
            i = ex - CV0
            S.add("sp", lambda e: e.dma_start(out=wd, in_=WBd[i].rearrange("(f p) d -> p f d", p=128)), writes=[b_wd], dkey="ld_wd",
                  extra=[cv_last[0]])
            return
        S.add("pool", lambda e: e.dma_start(out=wd, in_=w_ed[ex].rearrange("(f p) d -> p f d", p=128)), writes=[b_wd], dkey="ld_wd")

    def gather(ex):
        sl = ex % 2
        for k4 in range(4):
            bl = k4 % 2
            for q in range(4):
                kc = 4 * k4 + q
                for t in range(8):
                    mm(bank(bl)[:, q * 128:(q + 1) * 128], hn2[:, t, kc:2048:16], Sel2[sl][:, t, :], t == 0, t == 7,
                       [b_hn2[t], b_Sel2[sl]], bl)
            evac(XeT[:, 4 * k4:4 * k4 + 4, :], bank(bl).rearrange("p (q r) -> p q r", q=4), [bbuf[bl]], [b_XeT])

    issue_cv(len(cv_chunks))
    load_exp(0)
    load_wd(0)
    gen_sel(0)
    gather(0)
    for ex in range(64):
        s = ex % 2
        if ex + 1 < 64:
            load_exp(ex + 1)
        for fc in range(4):
            for kc in range(16):
                mm(bank(2)[:, fc * 128:(fc + 1) * 128], wgs[s][:, kc, fc * 128:(fc + 1) * 128], XeT[:, kc, :], kc == 0, kc == 15, [b_wg[s], b_XeT], 2)
        for fc in range(4):
            for kc in range(16):
                mm(bank(3)[:, fc * 128:(fc + 1) * 128], wus[s][:, kc, fc * 128:(fc + 1) * 128], XeT[:, kc, :], kc == 0, kc == 15, [b_wu[s], b_XeT], 3)
        S.add("act", lambda e: e.activation(out=sgt, in_=bank(2), func=AF.Silu), reads=[bbuf[2]], writes=[b_sgt])
        S.add("dve", lambda e: e.tensor_tensor(out=HT.rearrange("p f r -> p (f r)"), in0=bank(3), in1=sgt, op=ALU.mult), reads=[bbuf[3], b_sgt], writes=[b_HT])
        if ex + 1 < 64:
            gen_sel(ex + 1)
            gather(ex + 1)
        ys = ex % 2
        for dg in range(4):
            bl = 4 + dg % 4
            for fc in range(4):
                mm(bank(bl), HT[:, fc, :], wd[:, fc, dg * 512:(dg + 1) * 512], fc == 0, fc == 3, [b_HT, b_wd], bl)
            evac(Ysbs[ys][:, dg * 512:(dg + 1) * 512], bank(bl), [bbuf[bl]], [b_Ysbs[ys]])
        if ex + 1 < 64:
            load_wd(ex + 1)
        S.add("sp", lambda e, ys=ys, ex=ex: e.dma_start(out=YY[ex * 128:(ex + 1) * 128, :], in_=Ysbs[ys]), reads=[b_Ysbs[ys]],
              dkey="st_ys%d" % ys)
    S.barrier()
    eb = P.at(0, 64, F32)
    vv = P.at(512, 64, F32)
    idf = P.at(1024, 16, F32)
    idu = P.at(2048, 16, F32).bitcast(mybir.dt.uint32)
    b_cmb = Buf()
    gbuf = [P.at(8 * KB + i * 4 * KB, 2048, BF16) for i in range(4)]
    b_gbuf = [Buf() for _ in range(4)]
    S.add("dve", lambda e: e.tensor_scalar(out=eb, in0=iot[:, 0:64], scalar1=128.0, scalar2=None, op0=ALU.mult), reads=[b_c3], writes=[b_cmb])
    for t in range(8):
        for k, Ah in enumerate((A1h, A2h)):
            col = 2 * t + k
            S.add("dve", lambda e, t=t: e.tensor_tensor(out=vv, in0=Rk[:, t, :], in1=eb, op=ALU.add), reads=[b_Rk, b_cmb], writes=[b_cmb])
            S.add("dve", lambda e, t=t, Ah=Ah: e.tensor_tensor(out=vv, in0=vv, in1=Ah[:, t, :], op=ALU.mult), reads=[b_cmb, b_Wm], writes=[b_cmb])
            S.add("dve", lambda e, col=col: e.reduce_sum(out=idf[:, col:col + 1], in_=vv, axis=AX.X), reads=[b_cmb], writes=[b_cmb])
    S.add("dve", lambda e: e.tensor_copy(out=idu, in_=idf), reads=[b_cmb], writes=[b_cmb])
    for t in range(8):
        for k in range(2):
            col = 2 * t + k
            gi_ = (2 * t + k) % 4
            S.add("pool", lambda e, col=col, gi_=gi_: e.indirect_dma_start(out=gbuf[gi_], out_offset=None, in_=YY,
                                                                          in_offset=bass.IndirectOffsetOnAxis(ap=idu[:, col:col + 1], axis=0)),
                  reads=[b_cmb], writes=[b_gbuf[gi_]], dkey="ld_gb%d" % gi_)
            S.add("dve", lambda e, t=t, col=col, gi_=gi_: e.scalar_tensor_tensor(out=x2[:, t, :], in0=gbuf[gi_], scalar=W12[:, col:col + 1],
                                                                                in1=x2[:, t, :], op0=ALU.mult, op1=ALU.add),
                  reads=[b_gbuf[gi_], b_Wm, b_x2[t]], writes=[b_x2[t]])
    S.barrier()

    gfn = P.at(0, 2048, F32)
    b_gfn = Buf()
    S.add("sp", lambda e: e.dma_start(out=gfn, in_=final_norm.partition_broadcast(128)), writes=[b_gfn], dkey="ld_g")
    ost = [P.at(8 * KB + i * 8 * KB, 2048, F32) for i in range(2)]
    b_ost = [Buf(), Buf()]
    junk3 = P.at(24 * KB, 2048, BF16)
    b_junk3 = Buf()
    for t in range(8):
        st = sstat[:, 16 + 4 * (t % 2):20 + 4 * (t % 2)]
        bs = b_st[t % 2]
        o_ = ost[t % 2]
        S.add("act", lambda e, t=t, st=st: e.activation(out=junk3, in_=x2[:, t, :], func=AF.Square, accum_out=st[:, 0:1]),
              reads=[b_x2[t]], writes=[b_junk3, bs])
        S.add("dve", lambda e, st=st: e.tensor_scalar(out=st[:, 1:2], in0=st[:, 0:1], scalar1=1.0 / 2048, scalar2=1e-6, op0=ALU.mult, op1=ALU.add),
              reads=[bs], writes=[bs])
        S.add("act", lambda e, st=st: e.activation(out=st[:, 2:3], in_=st[:, 1:2], func=AF.Sqrt), reads=[bs], writes=[bs])
        S.add("dve", lambda e, st=st: e.reciprocal(out=st[:, 3:4], in_=st[:, 2:3]), reads=[bs], writes=[bs])
        S.add("dve", lambda e, t=t, st=st, o_=o_: e.scalar_tensor_tensor(out=o_, in0=x2[:, t, :], scalar=st[:, 3:4], in1=gfn,
                                                                        op0=ALU.mult, op1=ALU.mult), reads=[b_x2[t], bs, b_gfn], writes=[b_ost[t % 2]])
        S.final.append(S.add("sp", lambda e, t=t, o_=o_: e.dma_start(out=out[t * 128:(t + 1) * 128, :], in_=o_), reads=[b_ost[t % 2]],
                             dkey="st_out%d" % (t % 2)))
    return nc, S, dbg


def host_inputs(inputs):
    x = np.ascontiguousarray(inputs["x"], dtype=np.float32)
    rel = np.asarray(inputs["rel_bias"], np.float32)
    bf = ml_dtypes.bfloat16
    shared = dict(
        w_in=np.ascontiguousarray(inputs["w_in"][0]),
        norm_mix=np.ascontiguousarray(inputs["norm_mix"]).reshape(1, 2048),
        norm_ffn=np.ascontiguousarray(inputs["norm_ffn"]).reshape(1, 2048),
        final_norm=np.ascontiguousarray(inputs["final_norm"]).reshape(1, 2048),
        peT_k=np.ascontiguousarray(inputs["cmp_pe_k"][0].T),
        peT_v=np.ascontiguousarray(inputs["cmp_pe_v"][0].T),
        w1_k=np.ascontiguousarray(inputs["cmp_w1_k"][0]),
        w1_v=np.ascontiguousarray(inputs["cmp_w1_v"][0]),
        w2_k=np.ascontiguousarray(inputs["cmp_w2_k"][0]),
        w2_v=np.ascontiguousarray(inputs["cmp_w2_v"][0]),
        w_up_a=np.ascontiguousarray(inputs["w_up_nsa"][0]),
        w_up_b=np.ascontiguousarray(inputs["w_up_moba"][0]),
        w_out=np.ascontiguousarray(inputs["w_out"][0]),
        w_rt=np.ascontiguousarray(np.concatenate(
            [inputs["w_group"][0], inputs["w_router"][0].transpose(1, 0, 2).reshape(2048, 64)], axis=1)),
        b_rt=np.ascontiguousarray(np.concatenate([inputs["b_group"][0].reshape(8), inputs["b_router"][0].reshape(64)]).reshape(1, 72)),
        w_eg=np.ascontiguousarray(inputs["w_exp_gate"][0]),
        w_eu=np.ascontiguousarray(inputs["w_exp_up"][0]),
        w_ed=np.ascontiguousarray(inputs["w_exp_down"][0]),
        c_ident=np.eye(128, dtype=np.float32).astype(bf),
        c_ones=np.ones((128, 128), np.float32).astype(bf),
        c_ustr=np.triu(np.ones((128, 128), np.float32), 1).astype(bf),
        c_iota=np.tile(np.arange(128, dtype=np.float32)[None, :], (128, 1)),
        crep=np.ascontiguousarray(np.tile(rel[31:32, :], (128, 1))),
    )
    c_start = np.arange(255) * 16
    sb = np.arange(64) * 64
    ovl = ((c_start[:, None] < sb[None, :] + 64) & (c_start[:, None] + 32 > sb[None, :])).astype(np.float32)
    ovl65 = np.zeros((256, 65), np.float32)
    ovl65[:255, :64] = ovl
    ovl65[:255, 64] = 1.0
    shared["c_ovl"] = np.ascontiguousarray(ovl65.reshape(2, 128, 65).transpose(1, 0, 2).reshape(128, 130)).astype(bf)
    e64 = np.zeros((64, 32, 128), np.float32)
    e16 = np.zeros((16, 32, 128), np.float32)
    for kt in range(32):
        e64[2 * kt, kt, 0:64] = 1
        e64[2 * kt + 1, kt, 64:128] = 1
        e16[kt // 2, kt, :] = 1
    shared["c_e64"] = e64.reshape(64, 4096).astype(bf)
    shared["c_e16"] = e16.reshape(16, 4096).astype(bf)
    s24 = np.zeros((24, 24, 128), np.float32)
    for i in range(24):
        s24[i, i, :] = 1
    shared["c_sel24"] = s24.reshape(24, 24 * 128).astype(bf)

    maps = []
    for c in range(8):
        b, j = divmod(c, 4)
        tiles = [4 * m + j for m in range(8)]
        tpos = np.concatenate([np.arange(128) + 128 * a for a in tiles])
        m = dict(shared)
        m["xb"] = x[b]
        m["xo"] = np.ascontiguousarray(x[b].reshape(32, 128, 2048)[tiles].reshape(1024, 2048))
        cend = c_start + 31
        dist_c = tpos[None, :] - cend[:, None]
        bc = np.full((8, 256, 1024), NEG, np.float32)
        val = rel[rel_bucket_np(dist_c)][:, :, :8]
        bc[:, :255, :] = np.where((dist_c >= 0)[None], val.transpose(2, 0, 1), NEG)
        m["biasC"] = bc
        r = np.arange(128)
        bd = np.zeros((16, 128, 5, 128), np.float32)
        for v in range(5):
            dist = 128 * (j + 1 - v) + r[None, :] - r[:, None]
            tb = rel[rel_bucket_np(dist)]
            bd[:, :, v, :] = np.where((dist >= 0)[None], tb.transpose(2, 0, 1), NEG)
        m["bdiag"] = bd.reshape(16, 128, 640)
        bw = np.zeros((8, 128, 8, 128), np.float32)
        for v in range(8):
            dist = 128 * (j + 4 - v) + r[None, :] - r[:, None]
            tb = rel[rel_bucket_np(dist)][:, :, :8]
            bw[:, :, v, :] = np.where(((dist >= 0) & (dist < 512))[None], tb.transpose(2, 0, 1), NEG)
        m["bwin"] = bw.reshape(8, 128, 1024)
        cur = tpos // 64
        n64 = np.arange(64)[None, :]
        forced = (n64 == 0) | (n64 == cur[:, None]) | (n64 == cur[:, None] - 1)
        m["fsel"] = np.ascontiguousarray(np.where(forced, 1e4, 0.0).astype(np.float32).reshape(8, 128, 64).transpose(1, 0, 2).reshape(128, 512))
        own = tpos // 256
        n16 = np.arange(16)[None, :]
        past = n16 < own[:, None]

        def lay16(a):
            return np.ascontiguousarray(a.astype(np.float32).reshape(8, 128, 16).transpose(1, 0, 2).reshape(128, 128))
        m["pmneg"] = lay16(np.where(past, 0.0, -1e30))
        m["past01"] = lay16(past)
        m["own01"] = lay16(n16 == own[:, None])
        maps.append(m)
    return maps


def kernel(**inputs):
    nc, S, dbg = build()
    with contextlib.ExitStack() as st:
        S.emit(st)
    maps = host_inputs(inputs)
    res = run_bass_kernel_spmd(nc, maps, core_ids=list(range(8)))
    outp = np.zeros((2, 32, 128, 2048), np.float32)
    for c in range(8):
        b, j = divmod(c, 4)
        o = np.asarray(res.results[c]["out"]).reshape(8, 128, 2048)
        for m in range(8):
            outp[b, 4 * m + j] = o[m]
    return outp.reshape(2, 4096, 2048)
```

```python
import contextlib
import math
import numpy as np
import ml_dtypes
import concourse.bass as bass
import concourse.mybir as mybir
from concourse.bass_utils import run_bass_kernel_spmd

F32 = mybir.dt.float32
BF16 = mybir.dt.bfloat16
U8 = mybir.dt.uint8
AF = mybir.ActivationFunctionType
ALU = mybir.AluOpType
AX = mybir.AxisListType
NEG = -30000.0


class Buf:
    __slots__ = ("name", "w", "r")

    def __init__(self, name=""):
        self.name = name
        self.w = None
        self.r = []


class Op:
    __slots__ = ("eng", "fns", "deps", "signal", "sval", "dsem", "dtgt")


class Sched:
    ENGS = ("sp", "act", "dve", "pool", "pe")

    def __init__(self, nc):
        self.nc = nc
        self.ops = {e: [] for e in self.ENGS}
        self.dkeys = {}
        self.dlast = {}
        self.pending = {e: [] for e in self.ENGS}
        self.final = []

    def _dep(self, op, w):
        if w is None or w is op:
            return
        if w.eng == "pe" and op.eng == "pe":
            return
        op.deps.append(w)
        if w.dsem is None:
            w.signal = True

    def add(self, eng, fns, reads=(), writes=(), dkey=None, extra=()):
        op = Op()
        op.eng = eng
        op.fns = fns if isinstance(fns, (list, tuple)) else [fns]
        op.deps = []
        op.signal = False
        op.sval = None
        op.dsem = None
        op.dtgt = None
        if dkey is not None:
            ent = self.dkeys.setdefault(dkey, [0])
            ent[0] += 16 * len(op.fns)
            op.dsem = dkey
            op.dtgt = ent[0]
            self.dlast[dkey] = op
        for b in reads:
            self._dep(op, b.w)
        for b in writes:
            self._dep(op, b.w)
            for r in b.r:
                self._dep(op, r)
        for w in self.pending[eng]:
            self._dep(op, w)
        self.pending[eng] = []
        for w in extra:
            self._dep(op, w)
        for b in writes:
            b.w = op
            b.r = []
        for b in reads:
            if b.w is not op:
                b.r.append(op)
        self.ops[eng].append(op)
        return op

    def barrier(self):
        lasts = [self.ops[e][-1] for e in self.ENGS if self.ops[e] and self.ops[e][-1].dsem != "cv"]
        lasts += [op for k, op in self.dlast.items() if k != "cv"]
        for e in self.ENGS:
            self.pending[e] = self.pending[e] + lasts

    def emit(self, stack):
        nc = self.nc
        sems = {e: stack.enter_context(nc.semaphore("s_" + e)) for e in self.ENGS}
        dsems = {k: stack.enter_context(nc.semaphore("d_" + str(k))) for k in self.dkeys}
        for e in self.ENGS:
            c = 0
            for op in self.ops[e]:
                if op.dsem is None and op.signal:
                    c += 1
                    op.sval = c
        block = stack.enter_context(nc.Block())
        engmap = {"sp": block.sync, "act": block.scalar, "dve": block.vector, "pool": block.gpsimd,
                  "pe": block.tensor}

        def make(e):
            def body(eng):
                known = {}

                def waits(deps):
                    need = {}
                    for w in deps:
                        if w.dsem is not None:
                            key, val = ("d", w.dsem), w.dtgt
                        else:
                            key, val = ("e", w.eng), w.sval
                        if need.get(key, 0) < val:
                            need[key] = val
                    for key, val in need.items():
                        if known.get(key, 0) >= val:
                            continue
                        known[key] = val
                        eng.wait_ge(dsems[key[1]] if key[0] == "d" else sems[key[1]], val)

                for op in self.ops[e]:
                    waits(op.deps)
                    last = None
                    for fn in op.fns:
                        last = fn(eng)
                        if op.dsem is not None:
                            last.then_inc(dsems[op.dsem], 16)
                    if op.dsem is None and op.signal:
                        last.then_inc(sems[e], 1)
                if e == "sp":
                    waits(self.final)
            return body

        for e in self.ENGS:
            if self.ops[e] or e == "sp":
                engmap[e](make(e))


class Pool:
    def __init__(self, nc, nbytes):
        self.t = nc.alloc_sbuf_tensor("pool", [128, nbytes], U8)
        self.nbytes = nbytes

    def at(self, off, cols, dt, parts=128):
        sz = cols * (4 if dt == F32 else 2)
        assert off % 32 == 0 and off + sz <= self.nbytes, (off, sz, self.nbytes)
        return self.t[0:parts, off:off + sz].bitcast(dt)


def rel_bucket_np(dist):
    n = np.maximum(dist.astype(np.int64), 0)
    nf = np.maximum(n, 1).astype(np.float32)
    large = 16 + (np.log(nf / np.float32(16)) / np.float32(math.log(8.0)) * np.float32(16)).astype(np.int32)
    return np.where(n < 16, n, np.minimum(large, 31)).astype(np.int64)


_IN_OFF = dict(qa=0, kc=1024, vc=1280, ksl=1536, vsl=1792, kw=2048, vw=2304, gate=2560, qb=2584, kb=3608,
               vb=4632, gma=5656, gmb=7704)
KB = 1024
POOLB = 206 * KB


def build(stop=None):
    nc = bass.Bass("TRN2", target_bir_lowering=False)

    declared = set()

    def din(name, shape, dt=F32):
        declared.add(name)
        return nc.dram_tensor(name, list(shape), dt, kind="ExternalInput").ap()

    def dscr(name, shape, dt=BF16):
        return nc.dram_tensor(name, list(shape), dt).ap()

    xb = din("xb", [4096, 2048])
    xo = din("xo", [1024, 2048])
    w_in = din("w_in", [2048, 9752])
    norm_mix = din("norm_mix", [1, 2048])
    norm_ffn = din("norm_ffn", [1, 2048])
    final_norm = din("final_norm", [1, 2048])
    peT_k = din("peT_k", [128, 32])
    peT_v = din("peT_v", [128, 32])
    w1_k = din("w1_k", [4096, 256])
    w1_v = din("w1_v", [4096, 256])
    w2_k = din("w2_k", [256, 128])
    w2_v = din("w2_v", [256, 128])
    w_up_a = din("w_up_a", [1024, 2048])
    w_up_b = din("w_up_b", [1024, 2048])
    w_out = din("w_out", [2048, 2048])
    w_rt = din("w_rt", [2048, 72])
    b_rt = din("b_rt", [1, 72])
    if stop is None:
        w_eg = din("w_eg", [64, 2048, 512])
        w_eu = din("w_eu", [64, 2048, 512])
        w_ed = din("w_ed", [64, 512, 2048])
    biasC = din("biasC", [8, 256, 1024])
    bdiag = din("bdiag", [16, 128, 5 * 128])
    bwin = din("bwin", [8, 128, 8 * 128])
    crep = din("crep", [128, 16])
    fsel = din("fsel", [128, 8 * 64])
    pmneg = din("pmneg", [128, 8 * 16])
    past01 = din("past01", [128, 8 * 16])
    own01 = din("own01", [128, 8 * 16])
    c_ident = din("c_ident", [128, 128], BF16)
    c_ones = din("c_ones", [128, 128], BF16)
    c_ovl = din("c_ovl", [128, 2 * 65], BF16)
    c_e64 = din("c_e64", [64, 32 * 128], BF16)
    c_e16 = din("c_e16", [16, 32 * 128], BF16)
    c_sel24 = din("c_sel24", [24, 24 * 128], BF16)
    c_ustr = din("c_ustr", [128, 128], BF16)
    c_iota = din("c_iota", [128, 128])
    out = nc.dram_tensor("out", [1024, 2048], F32, kind="ExternalOutput").ap()

    FT = dscr("FT", [16, 128, 4096])
    TM = dscr("TM", [12, 128, 4096])
    QT = dscr("QT", [16, 128, 1024])
    GM = dscr("GM", [32, 128, 1024])
    dbg = {}
    dbg['_declared'] = declared

    def dout(name, shape):
        dbg[name] = nc.dram_tensor(name, list(shape), F32, kind="ExternalOutput").ap()
        return dbg[name]

    P = Pool(nc, POOLB)
    S = Sched(nc)
    banks = [nc.alloc_psum_tensor("pb%d" % i, [128, 512], F32) for i in range(8)]
    bbuf = [Buf("pb%d" % i) for i in range(8)]

    def bank(i):
        return banks[i][:, :]

    def bank16(i):
        return banks[i][:, :].bitcast(BF16)

    PB = 200 * KB
    ident = P.at(PB, 128, BF16)
    ones = P.at(PB + 256, 128, BF16)
    gT = P.at(PB + 512, 1024, BF16, parts=24)
    sstat = P.at(PB + 2560, 64, F32)
    b_const = Buf("const")
    b_gT = Buf("gT")
    S.add("sp", [lambda e: e.dma_start(out=ident, in_=c_ident), lambda e: e.dma_start(out=ones, in_=c_ones)],
          writes=[b_const], dkey="const")

    evac_rr = [0]

    def evac(out_ap, in_ap, reads, writes, scale=None):
        evac_rr[0] ^= 1
        if evac_rr[0]:
            if scale is None:
                return S.add("act", lambda e: e.activation(out=out_ap, in_=in_ap, func=AF.Copy), reads=reads, writes=writes)
            return S.add("act", lambda e: e.activation(out=out_ap, in_=in_ap, func=AF.Copy, scale=scale), reads=reads, writes=writes)
        if scale is None:
            return S.add("dve", lambda e: e.tensor_copy(out=out_ap, in_=in_ap), reads=reads, writes=writes)
        return S.add("dve", lambda e: e.tensor_scalar(out=out_ap, in0=in_ap, scalar1=scale, scalar2=None, op0=ALU.mult),
                     reads=reads, writes=writes)

    hTb = P.at(0, 16 * 4096, BF16).rearrange("p (k t) -> p k t", k=16)
    hTo = P.at(128 * KB, 16 * 1024, BF16).rearrange("p (k t) -> p k t", k=16)
    b_hTb = [Buf() for _ in range(32)]
    b_hTo = [Buf() for _ in range(8)]
    R0 = 160 * KB

    def norm_phase(srcs, gvec_dram, region, emit_tile):
        xs = [P.at(region + i * 8 * KB, 2048, F32) for i in range(2)]
        bxs = [Buf() for _ in range(2)]
        gt = P.at(region + 16 * KB, 2048, F32)
        b_gt = Buf()
        hn = [P.at(region + 24 * KB + i * 4 * KB, 2048, BF16) for i in range(2)]
        b_hn = [Buf() for _ in range(2)]
        junk = P.at(region + 32 * KB, 2048, BF16)
        b_junk = Buf()
        S.add("sp", lambda e: e.dma_start(out=gt, in_=gvec_dram.partition_broadcast(128)), writes=[b_gt], dkey="ld_g")
        for i, src in enumerate(srcs):
            s = i % 2
            st = sstat[:, 4 * s:4 * s + 4]
            S.add("sp", lambda e, src=src, s=s: e.dma_start(out=xs[s], in_=src), writes=[bxs[s]], dkey="ld_x%d" % s)
            S.add("act", lambda e, s=s, st=st: e.activation(out=junk, in_=xs[s], func=AF.Square, accum_out=st[:, 0:1]),
                  reads=[bxs[s]], writes=[b_junk, b_st[s]])
            S.add("dve", lambda e, st=st: e.tensor_scalar(out=st[:, 1:2], in0=st[:, 0:1], scalar1=1.0 / 2048, scalar2=1e-6,
                                                          op0=ALU.mult, op1=ALU.add), reads=[b_st[s]], writes=[b_st[s]])
            S.add("act", lambda e, st=st: e.activation(out=st[:, 2:3], in_=st[:, 1:2], func=AF.Sqrt), reads=[b_st[s]], writes=[b_st[s]])
            S.add("dve", lambda e, st=st: e.reciprocal(out=st[:, 3:4], in_=st[:, 2:3]), reads=[b_st[s]], writes=[b_st[s]])
            S.add("dve", lambda e, s=s, st=st: e.scalar_tensor_tensor(out=hn[s], in0=xs[s], scalar=st[:, 3:4], in1=gt,
                                                                     op0=ALU.mult, op1=ALU.mult),
                  reads=[bxs[s], b_st[s], b_gt], writes=[b_hn[s]])
            emit_tile(i, hn[s], b_hn[s], xs[s], bxs[s], st[:, 3:4], b_st[s])

    b_st = [Buf(), Buf()]

    def transpose_tile(hn_ap, b_hn, dst, b_dst, tcol):
        for half in range(2):
            bk = half
            for q in range(8):
                kc = half * 8 + q
                S.add("pe", lambda e, kc=kc, q=q, bk=bk: e.transpose(out=bank16(bk)[:, q * 128:(q + 1) * 128],
                                                                    in_=hn_ap[:, kc * 128:(kc + 1) * 128], identity=ident),
                      reads=[b_hn, b_const], writes=[bbuf[bk]])
            o = dst[:, half * 8:(half + 1) * 8, tcol:tcol + 128]
            i_ = bank16(bk).rearrange("p (k t) -> p k t", k=8)
            evac(o, i_, [bbuf[bk]], [b_dst])

    srcsA = [xb[t * 128:(t + 1) * 128, :] for t in range(32)] + [xo[t * 128:(t + 1) * 128, :] for t in range(8)]

    def emitA(i, hn_ap, b_hn, x_ap, b_x, rstd, bst):
        if i < 32:
            transpose_tile(hn_ap, b_hn, hTb, b_hTb[i], i * 128)
        else:
            transpose_tile(hn_ap, b_hn, hTo, b_hTo[i - 32], (i - 32) * 128)

    norm_phase(srcsA, norm_mix, R0, emitA)
    S.barrier()

    def dump_bf16(name, ap, n, parts=128):
        d = dout(name, [parts, n])
        tmp = P.at(160 * KB, n, F32, parts=parts)
        bt = Buf()
        S.add("dve", lambda e: e.tensor_copy(out=tmp, in_=ap), writes=[bt])
        S.final.append(S.add("sp", lambda e: e.dma_start(out=d, in_=tmp), reads=[bt], dkey="st_dbg"))

    if stop == "A":
        dump_bf16("d_hTb", hTb[:, 3, 0:2048], 2048)
        S.barrier()
        dump_bf16("d_hTo", hTo[:, 5, 0:1024], 1024)
        return nc, S, dbg

    wts = [P.at(R0 + i * 8 * KB, 16 * 256, BF16).rearrange("p (k c) -> p k c", k=16) for i in range(2)]
    b_wt = [Buf(), Buf()]
    stg = [P.at(R0 + 16 * KB + i * 8 * KB, 4096, BF16) for i in range(3)]
    b_stg = [Buf() for _ in range(3)]
    wslot = [0]
    sslot = [0]
    pbank = [0]

    def load_w(col0, ncols):
        s = wslot[0] % 2
        wslot[0] += 1
        src = w_in[:, col0:col0 + ncols].rearrange("(k p) c -> p k c", p=128)
        S.add("pool", lambda e: e.dma_start(out=wts[s][:, :, 0:ncols], in_=src), writes=[b_wt[s]], dkey="ld_w%d" % s)
        return wts[s], b_wt[s]

    def next_bank(lo=2, n=6):
        b = lo + pbank[0] % n
        pbank[0] += 1
        return b

    def next_stg():
        s = sslot[0] % 3
        sslot[0] += 1
        return s

    def proj_fm(wt, bw, c0, hT, b_hT, ntok, dst_dram, func=None, scale=None, tm=False):
        s = next_stg()
        for ch in range(ntok // 512):
            bk = next_bank()
            rb = [bw] + b_hT[ch * 4:(ch + 1) * 4]
            for kc in range(16):
                S.add("pe", lambda e, kc=kc, bk=bk, ch=ch: e.matmul(bank(bk), lhsT=wt[:, kc, c0:c0 + 128],
                                                                   rhs=hT[:, kc, ch * 512:(ch + 1) * 512],
                                                                   start=(kc == 0), stop=(kc == 15)),
                      reads=rb, writes=[bbuf[bk]])
            o = stg[s][:, ch * 512:(ch + 1) * 512]
            if func is None:
                evac(o, bank(bk), [bbuf[bk]], [b_stg[s]], scale=scale)
            else:
                S.add("act", lambda e, o=o, bk=bk: e.activation(out=o, in_=bank(bk), func=func), reads=[bbuf[bk]], writes=[b_stg[s]])
        if not tm:
            S.add("sp", lambda e: e.dma_start(out=dst_dram, in_=stg[s][:, 0:ntok]), reads=[b_stg[s]], dkey="st_stg%d" % s)
            return
        s2 = next_stg()
        for g8 in range(ntok // 1024):
            bk = next_bank()
            for q in range(8):
                t = g8 * 8 + q
                S.add("pe", lambda e, q=q, t=t, bk=bk: e.transpose(out=bank16(bk)[:, q * 128:(q + 1) * 128],
                                                                  in_=stg[s][:, t * 128:(t + 1) * 128], identity=ident),
                      reads=[b_stg[s], b_const], writes=[bbuf[bk]])
            evac(stg[s2][:, g8 * 1024:(g8 + 1) * 1024], bank16(bk), [bbuf[bk]], [b_stg[s2]])
        S.add("sp", lambda e: e.dma_start(out=dst_dram, in_=stg[s2][:, 0:ntok]), reads=[b_stg[s2]], dkey="st_stg%d" % s2)

    def pairs(base, n):
        return [(base + 256 * i) for i in range(n // 2)]

    ft_cols = [_IN_OFF["kc"], _IN_OFF["vc"], _IN_OFF["ksl"], _IN_OFF["kw"]] + pairs(_IN_OFF["kb"], 8)
    for pi, col0 in enumerate(ft_cols):
        if stop == "B3s":
            break
        wt, bw = load_w(col0, 256)
        for hh in range(2):
            proj_fm(wt, bw, hh * 128, hTb, b_hTb, 4096, FT[2 * pi + hh])
        if stop == "B1":
            break
    if stop == "B1":
        S.barrier()
        for i in range(2):
            a = P.at(176 * KB, 4096, BF16)
            bt = Buf()
            S.add("sp", lambda e, i=i, a=a: e.dma_start(out=a, in_=FT[i]), writes=[bt], dkey="ld_dbg")
            S.barrier()
            dump_bf16("d_FT%d" % i, a[:, 0:2048], 2048)
            S.barrier()
        return nc, S, dbg

    def dbg_exit(items):
        S.barrier()
        bt = Buf()
        for name, scr, i, w in items:
            a = P.at(32 * KB, w, BF16)
            S.add("sp", lambda e, a=a, scr=scr, i=i: e.dma_start(out=a, in_=scr[i]), writes=[bt], dkey="ld_dbg")
            S.barrier()
            dump_bf16("%s%d" % (name, i), a[:, 0:1024], 1024)
            S.barrier()
        return nc, S, dbg
    if stop == "B2":
        return dbg_exit([("d_FT", FT, 3, 4096), ("d_FT", FT, 15, 4096)])
    tm_cols = [_IN_OFF["vsl"], _IN_OFF["vw"]] + pairs(_IN_OFF["vb"], 8)
    def proj_tm(pi, wt, bw):
        s0, s1 = next_stg(), next_stg()
        for t2 in range(16):
            bk = next_bank()
            for tt in range(2):
                t = 2 * t2 + tt
                for kc in range(16):
                    S.add("pe", lambda e, kc=kc, bk=bk, t=t, tt=tt: e.matmul(bank(bk)[:, tt * 256:(tt + 1) * 256],
                                                                            lhsT=hTb[:, kc, t * 128:(t + 1) * 128],
                                                                            rhs=wt[:, kc, 0:256], start=(kc == 0), stop=(kc == 15)),
                          reads=[bw, b_hTb[t]], writes=[bbuf[bk]])
            for hh, s in ((0, s0), (1, s1)):
                o = stg[s][:, t2 * 256:(t2 + 1) * 256].rearrange("p (a d) -> p a d", a=2)
                i_ = bank(bk).rearrange("p (a h d) -> p a h d", a=2, h=2)[:, :, hh, :]
                evac(o, i_, [bbuf[bk]], [b_stg[s]])
        for hh, s in ((0, s0), (1, s1)):
            S.add("sp", lambda e, s=s, hh=hh, pi=pi: e.dma_start(out=TM[2 * pi + hh], in_=stg[s]), reads=[b_stg[s]],
                  dkey="st_stg%d" % s)

    for pi, col0 in enumerate(tm_cols):
        wt_, bw_ = load_w(col0, 256)
        for hh in range(2):
            proj_fm(wt_, bw_, hh * 128, hTb, b_hTb, 4096, TM[2 * pi + hh], tm=True)
        if stop in ("B3a", "B3s"):
            return dbg_exit([("d_TM", TM, 0, 4096), ("d_TM", TM, 1, 4096)])
    if stop == "B3":
        return dbg_exit([("d_TM", TM, 0, 4096), ("d_TM", TM, 11, 4096)])
    qs = 1.0 / math.sqrt(128.0)
    for pi, col0 in enumerate(pairs(_IN_OFF["qa"], 8) + pairs(_IN_OFF["qb"], 8)):
        wt, bw = load_w(col0, 256)
        for hh in range(2):
            proj_fm(wt, bw, hh * 128, hTo, b_hTo, 1024, QT[2 * pi + hh], scale=qs)
    if stop == "C1":
        return dbg_exit([("d_QT", QT, 0, 1024), ("d_QT", QT, 15, 1024)])
    for pi, col0 in enumerate(pairs(_IN_OFF["gma"], 16) + pairs(_IN_OFF["gmb"], 16)):
        wt, bw = load_w(col0, 256)
        for hh in range(2):
            proj_fm(wt, bw, hh * 128, hTo, b_hTo, 1024, GM[2 * pi + hh], func=AF.Sigmoid)
    def proj_gate(wt, bw):
      for ch in range(2):
        bk = next_bank()
        for kc in range(16):
            S.add("pe", lambda e, kc=kc, bk=bk, ch=ch: e.matmul(bank(bk)[0:24, :], lhsT=wt[:, kc, 0:24],
                                                               rhs=hTo[:, kc, ch * 512:(ch + 1) * 512],
                                                               start=(kc == 0), stop=(kc == 15)),
                  reads=[bw] + b_hTo[ch * 4:(ch + 1) * 4], writes=[bbuf[bk]])
        S.add("act", lambda e, bk=bk, ch=ch: e.activation(out=gT[:, ch * 512:(ch + 1) * 512], in_=bank(bk)[0:24, :],
                                                         func=AF.Sigmoid), reads=[bbuf[bk]], writes=[b_gT])

    if stop == "C2":
        return dbg_exit([("d_GM", GM, 0, 1024), ("d_GM", GM, 31, 1024)])
    wt_, bw_ = load_w(_IN_OFF["gate"], 24)
    proj_gate(wt_, bw_)
    S.barrier()

    if stop == "C":
        d1 = dout("d_gT", [24, 1024])
        tmp = P.at(0, 1024, F32, parts=24)
        bt = Buf()
        S.add("dve", lambda e: e.tensor_copy(out=tmp, in_=gT), reads=[b_gT], writes=[bt])
        S.final.append(S.add("sp", lambda e: e.dma_start(out=d1, in_=tmp), reads=[bt], dkey="st_dbg"))
        for name, scr, idxs, w in (("d_TM", TM, (0, 5, 11), 4096), ("d_QT", QT, (0, 9, 15), 1024), ("d_GM", GM, (0, 17, 31), 1024)):
            for i in idxs:
                a = P.at(32 * KB, w, BF16)
                S.add("sp", lambda e, a=a, scr=scr, i=i: e.dma_start(out=a, in_=scr[i]), writes=[bt], dkey="ld_dbg")
                S.barrier()
                dump_bf16("%s%d" % (name, i), a[:, 0:1024], 1024)
                S.barrier()
        S.final.append(S.add("sp", lambda e: e.dma_start(out=out[0:128, :], in_=P.at(0, 2048, F32)), reads=[bt], dkey="st_dbg"))
        return nc, S, dbg

    NCV = 0
    CV0 = 64 - NCV
    cv_chunks = []
    cv_last = [None]
    if NCV:
        WBg = dscr("WBg", [NCV, 512, 2048])
        WBu = dscr("WBu", [NCV, 512, 2048])
        WBd = dscr("WBd", [NCV, 512, 2048])
        for ex in range(CV0, 64):
            for src_t, dst_t in ((w_eg, WBg), (w_eu, WBu), (w_ed, WBd)):
                srcv = src_t[ex].rearrange("a b -> (a b)").rearrange("(r c) -> r c", c=2048)
                for c4 in range(4):
                    cv_chunks.append((srcv[c4 * 128:(c4 + 1) * 128, :], dst_t[ex - CV0][c4 * 128:(c4 + 1) * 128, :]))
    cv_pos = [0]

    def issue_cv(n):
        for _ in range(n):
            if cv_pos[0] >= len(cv_chunks):
                return
            src, dst = cv_chunks[cv_pos[0]]
            cv_pos[0] += 1
            cv_last[0] = S.add("pool", lambda e, src=src, dst=dst: e.dma_start(out=dst, in_=src), dkey="cv")

    def cast_load(dst, src, writes, dkey, ncv=0):
        op = S.add("pool", lambda e: e.dma_start(out=dst, in_=src), writes=writes, dkey=dkey)
        issue_cv(ncv)
        return op

    def sp_load(dst, src, writes, dkey):
        return S.add("sp", lambda e: e.dma_start(out=dst, in_=src), writes=writes, dkey=dkey)

    oaf = P.at(0, 8 * 1024, F32).rearrange("p (h t) -> p h t", h=8)
    obT = P.at(32 * KB, 8 * 1024, BF16).rearrange("p (h t) -> p h t", h=8)
    oaT = P.at(48 * KB, 8 * 1024, BF16).rearrange("p (h t) -> p h t", h=8)
    b_oaf = [Buf() for _ in range(8)]
    b_obT = [Buf() for _ in range(8)]
    b_oaT = Buf()
    selbT = P.at(64 * KB, 2 * 1024, BF16, parts=64).rearrange("p (g t) -> p g t", g=2)
    b_selbT = [Buf(), Buf()]
    selbTm = P.at(68 * KB, 1024, BF16, parts=16)
    b_selbTm = Buf()
    kcmpT = P.at(70 * KB, 2 * 256, BF16).rearrange("p (g c) -> p g c", g=2)
    vcmp = P.at(71 * KB, 2 * 256, BF16).rearrange("p (g c d) -> p g c d", g=2, c=2)
    b_kcmp = [Buf(), Buf()]
    b_vcmp = [Buf(), Buf()]
    psel = P.at(72 * KB, 512, F32).rearrange("p (t n) -> p t n", t=8)
    b_psel = Buf()
    e64 = P.at(74 * KB, 4096, BF16, parts=64).rearrange("p (k s) -> p k s", k=32)
    e16 = P.at(82 * KB, 4096, BF16, parts=16).rearrange("p (k s) -> p k s", k=32)
    sel24 = P.at(90 * KB, 3072, BF16, parts=24).rearrange("p (i s) -> p i s", i=24)
    ovl = P.at(96 * KB, 130, BF16).rearrange("p (c n) -> p c n", c=2)
    fselT = P.at(97 * KB, 512, F32).rearrange("p (t n) -> p t n", t=8)
    pmn = P.at(99 * KB, 128, F32).rearrange("p (t n) -> p t n", t=8)
    p01 = P.at(99 * KB + 512, 128, F32).rearrange("p (t n) -> p t n", t=8)
    o01 = P.at(100 * KB, 128, F32).rearrange("p (t n) -> p t n", t=8)
    crp = P.at(100 * KB + 512, 16, F32)
    b_c2 = Buf()
    S.add("sp", [lambda e: e.dma_start(out=P.at(74 * KB, 4096, BF16, parts=64), in_=c_e64),
                 lambda e: e.dma_start(out=P.at(82 * KB, 4096, BF16, parts=16), in_=c_e16),
                 lambda e: e.dma_start(out=P.at(90 * KB, 3072, BF16, parts=24), in_=c_sel24),
                 lambda e: e.dma_start(out=P.at(96 * KB, 130, BF16), in_=c_ovl),
                 lambda e: e.dma_start(out=P.at(97 * KB, 512, F32), in_=fsel),
                 lambda e: e.dma_start(out=P.at(99 * KB, 128, F32), in_=pmneg),
                 lambda e: e.dma_start(out=P.at(99 * KB + 512, 128, F32), in_=past01),
                 lambda e: e.dma_start(out=P.at(100 * KB, 128, F32), in_=own01),
                 lambda e: e.dma_start(out=crp, in_=crep)], writes=[b_c2], dkey="const2")

    KTs = [P.at(104 * KB + i * 8 * KB, 4096, BF16) for i in range(2)]
    Vs = [P.at(120 * KB + i * 8 * KB, 4096, BF16).rearrange("p (t d) -> p t d", t=32) for i in range(2)]
    Vs_flat = [P.at(120 * KB + i * 8 * KB, 4096, BF16) for i in range(2)]
    QTs = [P.at(136 * KB + i * 2 * KB, 1024, BF16) for i in range(2)]
    b_KT = [Buf(), Buf()]
    b_V = [Buf(), Buf()]
    b_QT = [Buf(), Buf()]
    biasCb = P.at(140 * KB, 2048, BF16).rearrange("p (c q) -> p c q", c=2)
    b_biasC = Buf()
    bdraw = P.at(144 * KB, 640, F32)
    bdb = P.at(147 * KB, 640, BF16).rearrange("p (v q) -> p v q", v=5)
    bdb_flat = P.at(147 * KB, 640, BF16)
    b_bdraw, b_bdb = Buf(), Buf()
    bwb = P.at(149 * KB, 1024, BF16).rearrange("p (v q) -> p v q", v=8)
    bwb_flat = P.at(149 * KB, 1024, BF16)
    b_bwb = Buf()
    PTs = [P.at(152 * KB + i * KB, 512, BF16) for i in range(3)]
    b_PT = [Buf() for _ in range(3)]
    rden = P.at(155 * KB, 512, F32)
    tmpf = P.at(157 * KB, 512, F32)
    b_rden, b_tmpf = Buf(), Buf()
    w1b = P.at(160 * KB, 32 * 256, BF16).rearrange("p (l h) -> p l h", l=32)
    w1b_flat = P.at(160 * KB, 32 * 256, BF16)
    w2b = P.at(176 * KB, 256, BF16).rearrange("p (c d) -> p c d", c=2)
    w2b_flat = P.at(176 * KB, 256, BF16)
    peb = P.at(176 * KB + 512, 32, BF16)
    hid = P.at(177 * KB, 512, BF16).rearrange("p (c n) -> p c n", c=2)
    zf = P.at(178 * KB, 256, F32)
    uf = P.at(179 * KB, 256, F32)
    sgf = P.at(180 * KB, 256, F32)
    bh = P.at(181 * KB, 8, F32)
    b_w1, b_w2, b_pe, b_hid, b_z, b_u, b_sg, b_bh = [Buf() for _ in range(8)]
    sc = P.at(182 * KB, 64, F32)
    sc2 = P.at(182 * KB + 256, 64, F32)
    m8a = P.at(182 * KB + 512, 8, F32)
    m8b = P.at(182 * KB + 576, 8, F32)
    selm = P.at(183 * KB, 64, F32)
    selb = P.at(183 * KB + 256, 64, BF16)
    km = P.at(184 * KB, 16, F32)
    kmT = P.at(184 * KB + 64, 16, BF16)
    rd1 = P.at(184 * KB + 128, 8, F32)
    b_sc, b_km, b_rd1 = Buf(), Buf(), Buf()
    lrot = [0]
    ptrot = [0]

    def Lbank():
        b = lrot[0] % 4
        lrot[0] += 1
        return b

    def PTslot():
        s = ptrot[0] % 3
        ptrot[0] += 1
        return s

    def mm(outap, lhsT, rhs, start, stop, reads, bk):
        S.add("pe", lambda e: e.matmul(outap, lhsT=lhsT, rhs=rhs, start=start, stop=stop), reads=reads, writes=[bbuf[bk]])

    def load_head(slot, ft_idx, tm_idx, q_idx):
        if ft_idx is not None:
            sp_load(KTs[slot], FT[ft_idx], [b_KT[slot]], "ld_KT%d" % slot)
        if tm_idx is not None:
            sp_load(Vs_flat[slot], TM[tm_idx], [b_V[slot]], "ld_V%d" % slot)
        if q_idx is not None:
            sp_load(QTs[slot], QT[q_idx], [b_QT[slot]], "ld_Q%d" % slot)

    def finalize_nsa(h, cq, gi, first, bo=4, bd_=5):
        ch = slice(cq * 512, (cq + 1) * 512)
        gb = Lbank()
        S.add("dve", lambda e: e.tensor_scalar(out=rden, in0=bank(bd_), scalar1=1e-30, scalar2=None, op0=ALU.max),
              reads=[bbuf[bd_]], writes=[b_rden])
        S.add("dve", lambda e: e.reciprocal(out=rden, in_=rden), reads=[b_rden], writes=[b_rden])
        mm(bank(gb), sel24[:, gi, :], gT[:, ch], True, True, [b_c2, b_gT], gb)
        S.add("dve", lambda e: e.tensor_tensor(out=tmpf, in0=bank(bo), in1=rden, op=ALU.mult), reads=[bbuf[bo], b_rden], writes=[b_tmpf])
        if first:
            S.add("dve", lambda e: e.tensor_tensor(out=oaf[:, h, ch], in0=bank(gb), in1=tmpf, op=ALU.mult),
                  reads=[bbuf[gb], b_tmpf], writes=[b_oaf[h]])
        else:
            S.add("dve", lambda e: e.tensor_tensor(out=tmpf, in0=bank(gb), in1=tmpf, op=ALU.mult), reads=[bbuf[gb], b_tmpf], writes=[b_tmpf])
            S.add("dve", lambda e: e.tensor_tensor(out=oaf[:, h, ch], in0=oaf[:, h, ch], in1=tmpf, op=ALU.add),
                  reads=[b_tmpf, b_oaf[h]], writes=[b_oaf[h]])

    def finalize_moba(h, cq, bo=4, bd_=5):
        ch = slice(cq * 512, (cq + 1) * 512)
        S.add("dve", lambda e: e.tensor_scalar(out=rden, in0=bank(bd_), scalar1=1e-30, scalar2=None, op0=ALU.max),
              reads=[bbuf[bd_]], writes=[b_rden])
        S.add("dve", lambda e: e.reciprocal(out=rden, in_=rden), reads=[b_rden], writes=[b_rden])
        S.add("dve", lambda e: e.tensor_tensor(out=obT[:, h, ch], in0=bank(bo), in1=rden, op=ALU.mult),
              reads=[bbuf[bo], b_rden], writes=[b_obT[h]])

    odrot = [0]

    def run_pipe(stages, lag=2, extra=None):
        n = len(stages)
        extra = list(extra or [])
        for i in range(min(lag, n)):
            stages[i][0]()
        for i in range(n):
            if i + lag < n:
                stages[i + lag][0]()
            stages[i][1]()
            if extra and i % 2 == 1:
                extra.pop(0)()
        for f in extra:
            f()

    def attn_causal(ks, vs, qs_, selT, b_sel, E, fin, extra=None):
        KTa, Va, QTa = KTs[ks], Vs[vs], QTs[qs_]
        stages = []
        for cq in range(2):
            nkt = 16 * cq + 16
            odrot[0] ^= 1
            bo, bd_ = (4, 5) if odrot[0] else (6, 7)
            for kt in range(nkt):
                st = {}

                def s1(cq=cq, kt=kt, st=st):
                    s0 = max(0, -((3 - kt) // 4) - 4 * cq)
                    c0 = 128 * s0
                    q0 = cq * 512 + c0
                    q1 = cq * 512 + 512
                    bl = Lbank()
                    diag = []
                    for s_ in range(s0, 4):
                        v = kt - (4 * (4 * cq + s_) - 1)
                        if 0 <= v <= 4:
                            diag.append((s_, v))
                    mm(bank(bl)[:, c0:512], KTa[:, kt * 128:(kt + 1) * 128], QTa[:, q0:q1], True, False, [b_KT[ks], b_QT[qs_]], bl)
                    mm(bank(bl)[:, c0:512], E[:, kt, :], selT[:, q0:q1], False, len(diag) == 0, [b_c2, b_sel], bl)
                    for di, (s_, v) in enumerate(diag):
                        mm(bank(bl)[:, s_ * 128:(s_ + 1) * 128], ident, bdb[:, v, :], False, di == len(diag) - 1, [b_bdb, b_const], bl)
                    ps_ = PTslot()
                    S.add("act", lambda e: e.activation(out=PTs[ps_][:, c0:512], in_=bank(bl)[:, c0:512], func=AF.Exp),
                          reads=[bbuf[bl]], writes=[b_PT[ps_]])
                    st["ps"] = ps_
                    st["c0"] = c0

                def s2(cq=cq, kt=kt, st=st, nkt=nkt, bo=bo, bd_=bd_):
                    ps_, c0 = st["ps"], st["c0"]
                    mm(bank(bo)[:, c0:512], Va[:, kt, :], PTs[ps_][:, c0:512], kt == 0, kt == nkt - 1, [b_V[vs], b_PT[ps_]], bo)
                    mm(bank(bd_)[:, c0:512], ones, PTs[ps_][:, c0:512], kt == 0, kt == nkt - 1, [b_const, b_PT[ps_]], bd_)
                    if kt == nkt - 1:
                        fin(cq, bo, bd_)

                stages.append((s1, s2))
        run_pipe(stages, extra=extra)

    def prep_bd(hidx):
        sp_load(bdraw, bdiag[hidx], [b_bdraw], "ld_bd")
        S.add("dve", lambda e: e.tensor_scalar(out=bdb_flat, in0=bdraw, scalar1=crp[:, hidx:hidx + 1], scalar2=None, op0=ALU.subtract),
              reads=[b_bdraw, b_c2], writes=[b_bdb])

    def compress(g, w1d, w2d, ped, src_ft, is_v):
        cast_load(w1b, w1d.rearrange("(l p) h -> p l h", p=128), [b_w1], "ld_w1")
        cast_load(w2b, w2d.rearrange("(c p) d -> p c d", p=128), [b_w2], "ld_w2")
        cast_load(peb, ped, [b_pe], "ld_pe")
        sp_load(KTs[0], FT[src_ft], [b_KT[0]], "ld_KT0")
        for hc in range(2):
            for l in range(32):
                mm(bank(7)[:, hc:hc + 1], w1b[:, l, hc * 128:(hc + 1) * 128], peb[:, l:l + 1], l == 0, l == 31, [b_w1, b_pe], 7)
        S.add("dve", lambda e: e.tensor_copy(out=bh[:, 0:2], in_=bank(7)[:, 0:2]), reads=[bbuf[7]], writes=[b_bh])
        for hc in range(2):
            bl = Lbank()
            for l in range(32):
                mm(bank(bl)[:, 0:255], w1b[:, l, hc * 128:(hc + 1) * 128], KTs[0][:, l:l + 16 * 254 + 1:16], l == 0, l == 31,
                   [b_w1, b_KT[0]], bl)
            S.add("dve", lambda e, bl=bl, hc=hc: e.tensor_scalar(out=zf[:, 0:255], in0=bank(bl)[:, 0:255], scalar1=bh[:, hc:hc + 1],
                                                                scalar2=None, op0=ALU.add), reads=[bbuf[bl], b_bh], writes=[b_z])
            S.add("dve", lambda e: e.tensor_tensor(out=uf[:, 0:255], in0=zf[:, 0:255], in1=zf[:, 0:255], op=ALU.mult), reads=[b_z], writes=[b_u])
            S.add("dve", lambda e: e.tensor_scalar(out=uf[:, 0:255], in0=uf[:, 0:255], scalar1=0.044715, scalar2=1.0, op0=ALU.mult, op1=ALU.add),
                  reads=[b_u], writes=[b_u])
            S.add("dve", lambda e: e.tensor_tensor(out=uf[:, 0:255], in0=uf[:, 0:255], in1=zf[:, 0:255], op=ALU.mult), reads=[b_u, b_z], writes=[b_u])
            S.add("act", lambda e: e.activation(out=sgf[:, 0:255], in_=uf[:, 0:255], func=AF.Sigmoid, scale=1.5957691216057308),
                  reads=[b_u], writes=[b_sg])
            S.add("dve", lambda e, hc=hc: e.tensor_tensor(out=hid[:, hc, 0:255], in0=zf[:, 0:255], in1=sgf[:, 0:255], op=ALU.mult),
                  reads=[b_z, b_sg], writes=[b_hid])
        if not is_v:
            bl = Lbank()
            for hc in range(2):
                mm(bank(bl)[:, 0:255], w2b[:, hc, :], hid[:, hc, 0:255], hc == 0, hc == 1, [b_w2, b_hid], bl)
            S.add("dve", lambda e: e.memset(kcmpT[:, g, :], 0.0), writes=[b_kcmp[g]])
            S.add("dve", lambda e, bl=bl: e.tensor_copy(out=kcmpT[:, g, 0:255], in_=bank(bl)[:, 0:255]), reads=[bbuf[bl]], writes=[b_kcmp[g]])
        else:
            S.add("dve", lambda e: e.memset(vcmp[:, g, :, :], 0.0), writes=[b_vcmp[g]])
            for ct in range(2):
                n = 128 if ct == 0 else 127
                bl = Lbank()
                for hc in range(2):
                    mm(bank(bl)[0:n, 0:128], hid[:, hc, ct * 128:ct * 128 + n], w2b[:, hc, :], hc == 0, hc == 1, [b_w2, b_hid], bl)
                S.add("dve", lambda e, bl=bl, ct=ct, n=n: e.tensor_copy(out=vcmp[0:n, g, ct, :], in_=bank(bl)[0:n, 0:128]),
                      reads=[bbuf[bl]], writes=[b_vcmp[g]])

    for g in range(2):
        compress(g, w1_k, w2_k, peT_k, 0 + g, False)
        compress(g, w1_v, w2_v, peT_v, 2 + g, True)

    for g in range(2):
        for jj in range(4):
            h = 4 * g + jj
            qs_ = h % 2
            load_head(qs_, None, None, h)
            cast_load(biasCb, biasC[h].rearrange("(c p) q -> p c q", p=128), [b_biasC], "ld_bc", ncv=7)
            for cq in range(2):
                ch = slice(cq * 512, (cq + 1) * 512)
                pts = []
                for ct in range(2):
                    bl = Lbank()
                    mm(bank(bl), kcmpT[:, g, ct * 128:(ct + 1) * 128], QTs[qs_][:, ch], True, False, [b_kcmp[g], b_QT[qs_]], bl)
                    mm(bank(bl), ident, biasCb[:, ct, ch], False, True, [b_biasC, b_const], bl)
                    ps_ = PTslot()
                    pts.append(ps_)
                    S.add("act", lambda e, bl=bl, ps_=ps_: e.activation(out=PTs[ps_], in_=bank(bl), func=AF.Exp),
                          reads=[bbuf[bl]], writes=[b_PT[ps_]])
                for ct in range(2):
                    mm(bank(4), vcmp[:, g, ct, :], PTs[pts[ct]], ct == 0, ct == 1, [b_vcmp[g], b_PT[pts[ct]]], 4)
                for ct in range(2):
                    mm(bank(5), ones, PTs[pts[ct]], ct == 0, ct == 1, [b_const, b_PT[pts[ct]]], 5)
                for s in range(4):
                    for ct in range(2):
                        mm(bank(7)[:, s * 65:(s + 1) * 65], PTs[pts[ct]][:, s * 128:(s + 1) * 128], ovl[:, ct, :], ct == 0, ct == 1,
                           [b_c2, b_PT[pts[ct]]], 7)
                for s in range(4):
                    t = 4 * cq + s
                    S.add("dve", lambda e, s=s: e.tensor_scalar(out=rd1[:, 0:1], in0=bank(7)[:, s * 65 + 64:s * 65 + 65], scalar1=1e-30,
                                                               scalar2=None, op0=ALU.max), reads=[bbuf[7]], writes=[b_rd1])
                    S.add("dve", lambda e: e.reciprocal(out=rd1[:, 0:1], in_=rd1[:, 0:1]), reads=[b_rd1], writes=[b_rd1])
                    if jj == 0:
                        S.add("dve", lambda e, s=s, t=t: e.tensor_scalar(out=psel[:, t, :], in0=bank(7)[:, s * 65:s * 65 + 64],
                                                                        scalar1=rd1[:, 0:1], scalar2=None, op0=ALU.mult),
                              reads=[bbuf[7], b_rd1], writes=[b_psel])
                    else:
                        S.add("dve", lambda e, s=s, t=t: e.scalar_tensor_tensor(out=psel[:, t, :], in0=bank(7)[:, s * 65:s * 65 + 64],
                                                                               scalar=rd1[:, 0:1], in1=psel[:, t, :], op0=ALU.mult, op1=ALU.add),
                              reads=[bbuf[7], b_rd1, b_psel], writes=[b_psel])
                finalize_nsa(h, cq, 3 * h + 0, True)
        for t in range(8):
            S.add("dve", lambda e, t=t: e.tensor_tensor(out=sc, in0=psel[:, t, :], in1=fselT[:, t, :], op=ALU.add), reads=[b_psel, b_c2], writes=[b_sc])
            S.add("dve", lambda e: e.max(out=m8a, in_=sc), reads=[b_sc], writes=[b_sc])
            S.add("dve", lambda e: e.match_replace(out=sc2, in_to_replace=m8a, in_values=sc, imm_value=-1e30), reads=[b_sc], writes=[b_sc])
            S.add("dve", lambda e: e.max(out=m8b, in_=sc2), reads=[b_sc], writes=[b_sc])
            S.add("dve", lambda e: e.tensor_scalar(out=selm, in0=sc, scalar1=m8b[:, 7:8], scalar2=None, op0=ALU.is_ge), reads=[b_sc], writes=[b_sc])
            S.add("dve", lambda e: e.tensor_scalar(out=selb, in0=selm, scalar1=-NEG, scalar2=NEG, op0=ALU.mult, op1=ALU.add), reads=[b_sc], writes=[b_sc])
            S.add("pe", lambda e: e.transpose(out=bank16(7)[0:64, 0:128], in_=selb, identity=ident), reads=[b_sc, b_const], writes=[bbuf[7]])
            S.add("dve", lambda e, t=t, g=g: e.tensor_copy(out=selbT[:, g, t * 128:(t + 1) * 128], in_=bank16(7)[0:64, 0:128]),
                  reads=[bbuf[7]], writes=[b_selbT[g]])
        load_head(0, 4 + g, 0 + g, None)
        for jj in range(4):
            h = 4 * g + jj
            qs_ = h % 2
            load_head(qs_, None, None, h)
            prep_bd(h)
            attn_causal(0, 0, qs_, selbT[:, g, :], b_selbT[g], e64, lambda cq, bo, bd_, h=h: finalize_nsa(h, cq, 3 * h + 1, False, bo, bd_))
        load_head(1, 6 + g, 2 + g, None)
        for jj in range(4):
            h = 4 * g + jj
            qs_ = h % 2
            load_head(qs_, None, None, h)
            cast_load(bwb_flat, bwin[h], [b_bwb], "ld_bw", ncv=7)
            stages = []
            for cq in range(2):
                odrot[0] ^= 1
                bo, bd_ = (4, 5) if odrot[0] else (6, 7)
                for s in range(4):
                    m = 4 * cq + s
                    kts = list(range(max(0, 4 * m - 4), 4 * m + 4))
                    for ki, kt in enumerate(kts):
                        st = {}

                        def s1(m=m, kt=kt, st=st, qs_=qs_):
                            v = kt - (4 * m - 4)
                            bl = Lbank()
                            mm(bank(bl)[:, 0:128], KTs[1][:, kt * 128:(kt + 1) * 128], QTs[qs_][:, m * 128:(m + 1) * 128], True, False,
                               [b_KT[1], b_QT[qs_]], bl)
                            mm(bank(bl)[:, 0:128], ident, bwb[:, v, :], False, True, [b_bwb, b_const], bl)
                            ps_ = PTslot()
                            S.add("act", lambda e: e.activation(out=PTs[ps_][:, 0:128], in_=bank(bl)[:, 0:128], func=AF.Exp),
                                  reads=[bbuf[bl]], writes=[b_PT[ps_]])
                            st["ps"] = ps_

                        def s2(s=s, kt=kt, ki=ki, nk=len(kts), st=st, bo=bo, bd_=bd_, cq=cq, h=h):
                            ps_ = st["ps"]
                            mm(bank(bo)[:, s * 128:(s + 1) * 128], Vs[1][:, kt, :], PTs[ps_][:, 0:128], ki == 0, ki == nk - 1,
                               [b_V[1], b_PT[ps_]], bo)
                            mm(bank(bd_)[:, s * 128:(s + 1) * 128], ones, PTs[ps_][:, 0:128], ki == 0, ki == nk - 1,
                               [b_const, b_PT[ps_]], bd_)
                            if s == 3 and ki == nk - 1:
                                finalize_nsa(h, cq, 3 * h + 2, False, bo, bd_)

                        stages.append((s1, s2))
            run_pipe(stages)

    selbTm2 = [selbTm, P.at(186 * KB, 1024, BF16, parts=16)]
    b_selbTm2 = [b_selbTm, Buf()]
    kmT2 = [kmT, P.at(184 * KB + 256, 16, BF16)]
    b_km2 = [b_km, Buf()]

    def moba_sel_steps(h):
        sl = h % 2
        steps = []

        def kstep():
            S.add("dve", lambda e: e.reduce_sum(out=km, in_=KTs[sl].rearrange("p (n s) -> p n s", n=16), axis=AX.X),
                  reads=[b_KT[sl]], writes=[b_sc])
            S.add("dve", lambda e: e.tensor_scalar(out=kmT2[sl], in0=km, scalar1=1.0 / 256, scalar2=None, op0=ALU.mult), reads=[b_sc], writes=[b_km2[sl]])
        steps.append(kstep)
        for t in range(8):
            def a_step(t=t):
                bl = Lbank()
                mm(bank(bl)[:, 0:16], QTs[sl][:, t * 128:(t + 1) * 128], kmT2[sl], True, True, [b_QT[sl], b_km2[sl]], bl)
                S.add("dve", lambda e: e.tensor_tensor(out=sc[:, 0:16], in0=bank(bl)[:, 0:16], in1=pmn[:, t, :], op=ALU.add),
                      reads=[bbuf[bl], b_c2], writes=[b_sc])
                S.add("dve", lambda e: e.max(out=m8a, in_=sc[:, 0:16]), reads=[b_sc], writes=[b_sc])
                S.add("dve", lambda e: e.tensor_scalar(out=selm[:, 0:16], in0=sc[:, 0:16], scalar1=m8a[:, 2:3], scalar2=None, op0=ALU.is_ge),
                      reads=[b_sc], writes=[b_sc])
                S.add("dve", lambda e: e.tensor_tensor(out=selm[:, 0:16], in0=selm[:, 0:16], in1=p01[:, t, :], op=ALU.mult),
                      reads=[b_sc, b_c2], writes=[b_sc])
                S.add("dve", lambda e: e.tensor_tensor(out=selm[:, 0:16], in0=selm[:, 0:16], in1=o01[:, t, :], op=ALU.add),
                      reads=[b_sc, b_c2], writes=[b_sc])
                S.add("dve", lambda e: e.tensor_scalar(out=selb[:, 0:16], in0=selm[:, 0:16], scalar1=-NEG, scalar2=NEG, op0=ALU.mult, op1=ALU.add),
                      reads=[b_sc], writes=[b_sc])

            def b_step(t=t):
                bl = Lbank()
                S.add("pe", lambda e: e.transpose(out=bank16(bl)[0:16, 0:128], in_=selb[:, 0:16], identity=ident), reads=[b_sc, b_const], writes=[bbuf[bl]])
                S.add("dve", lambda e: e.tensor_copy(out=selbTm2[sl][:, t * 128:(t + 1) * 128], in_=bank16(bl)[0:16, 0:128]),
                      reads=[bbuf[bl]], writes=[b_selbTm2[sl], b_sc])
            steps.append(a_step)
            steps.append(b_step)
        return steps

    load_head(0, 8, 4, 8)
    for f in moba_sel_steps(0):
        f()
    for h in range(8):
        sl = h % 2
        extra = None
        if h + 1 < 8:
            load_head((h + 1) % 2, 8 + h + 1, 4 + h + 1, 8 + h + 1)
            extra = moba_sel_steps(h + 1)
        prep_bd(8 + h)
        attn_causal(sl, sl, sl, selbTm2[sl], b_selbTm2[sl], e16, lambda cq, bo, bd_, h=h: finalize_moba(h, cq, bo, bd_), extra=extra)
    for h in range(8):
        S.add("dve", lambda e, h=h: e.tensor_copy(out=oaT[:, h, :], in_=oaf[:, h, :]), reads=[b_oaf[h]], writes=[b_oaT])
    S.barrier()

    def dump_many(items):
        for name, ap, n, parts in items:
            S.barrier()
            dump_bf16(name, ap, n, parts)
        S.barrier()

    if stop == "D":
        items = [("d_oa%d" % h, oaT[:, h, :], 1024, 128) for h in range(8)] + [("d_ob%d" % h, obT[:, h, :], 1024, 128) for h in range(8)]
        items += [("d_kcmp%d" % g, kcmpT[:, g, :], 256, 128) for g in range(2)]
        items += [("d_vcmp%d" % g, vcmp[:, g, :, :].rearrange("p c d -> p (c d)"), 256, 128) for g in range(2)]
        items += [("d_selbT%d" % g, selbT[:, g, :], 1024, 64) for g in range(2)]
        dump_many(items)
        return nc, S, dbg
    wupA = P.at(64 * KB, 8 * 2048, BF16).rearrange("p (h c) -> p h c", h=8)
    wupB = P.at(96 * KB, 8 * 2048, BF16).rearrange("p (h c) -> p h c", h=8)
    b_wup = [Buf(), Buf()]
    for hh in range(8):
        cast_load(wupA[:, hh, :], w_up_a[hh * 128:(hh + 1) * 128, :], [b_wup[0]], "ld_wupa", ncv=5)
        cast_load(wupB[:, hh, :], w_up_b[hh * 128:(hh + 1) * 128, :], [b_wup[1]], "ld_wupb")
    mT = P.at(128 * KB, 16 * 1024, BF16).rearrange("p (k t) -> p k t", k=16)
    b_mT = [Buf() for _ in range(8)]
    gms = [P.at(160 * KB + i * 2 * KB, 1024, BF16) for i in range(4)]
    b_gm = [Buf() for _ in range(4)]
    t1 = P.at(168 * KB, 512, F32)
    t2 = P.at(170 * KB, 512, F32)
    b_t1, b_t2 = Buf(), Buf()
    for ct in range(16):
        sa, sb_ = (ct % 2) * 2, (ct % 2) * 2 + 1
        sp_load(gms[sa], GM[ct], [b_gm[sa]], "ld_gm%d" % sa)
        sp_load(gms[sb_], GM[16 + ct], [b_gm[sb_]], "ld_gm%d" % sb_)
        for cq in range(2):
            ch = slice(cq * 512, (cq + 1) * 512)
            ba, bb2 = Lbank(), Lbank()
            for hh in range(8):
                mm(bank(ba), wupA[:, hh, ct * 128:(ct + 1) * 128], oaT[:, hh, ch], hh == 0, hh == 7, [b_wup[0], b_oaT], ba)
            for hh in range(8):
                mm(bank(bb2), wupB[:, hh, ct * 128:(ct + 1) * 128], obT[:, hh, ch], hh == 0, hh == 7, [b_wup[1]] + b_obT, bb2)
            S.add("dve", lambda e, ba=ba, sa=sa, ch=ch: e.tensor_tensor(out=t1, in0=bank(ba), in1=gms[sa][:, ch], op=ALU.mult),
                  reads=[bbuf[ba], b_gm[sa]], writes=[b_t1])
            S.add("dve", lambda e, bb2=bb2, sb_=sb_, ch=ch: e.tensor_tensor(out=t2, in0=bank(bb2), in1=gms[sb_][:, ch], op=ALU.mult),
                  reads=[bbuf[bb2], b_gm[sb_]], writes=[b_t2])
            S.add("dve", lambda e, ct=ct, ch=ch: e.tensor_tensor(out=mT[:, ct, ch], in0=t1, in1=t2, op=ALU.add),
                  reads=[b_t1, b_t2], writes=b_mT[cq * 4:(cq + 1) * 4])
    S.barrier()
    x2 = P.at(64 * KB, 8 * 2048, F32).rearrange("p (t c) -> p t c", t=8)
    b_x2 = [Buf() for _ in range(8)]
    for t in range(8):
        sp_load(x2[:, t, :], xo[t * 128:(t + 1) * 128, :], [b_x2[t]], "ld_x2")
    wos = [P.at(i * 16 * KB, 16 * 512, BF16).rearrange("p (k c) -> p k c", k=16) for i in range(2)]
    b_wo = [Buf(), Buf()]
    for dg in range(4):
        s = dg % 2
        cast_load(wos[s], w_out[:, dg * 512:(dg + 1) * 512].rearrange("(k p) c -> p k c", p=128), [b_wo[s]], "ld_wo%d" % s)
        for t in range(8):
            bl = Lbank()
            for kc in range(16):
                mm(bank(bl), mT[:, kc, t * 128:(t + 1) * 128], wos[s][:, kc, :], kc == 0, kc == 15, [b_mT[t], b_wo[s]], bl)
            S.add("dve", lambda e, bl=bl, t=t, dg=dg: e.tensor_tensor(out=x2[:, t, dg * 512:(dg + 1) * 512], in0=bank(bl),
                                                                     in1=x2[:, t, dg * 512:(dg + 1) * 512], op=ALU.add),
                  reads=[bbuf[bl], b_x2[t]], writes=[b_x2[t]])
    S.barrier()

    if stop == "F":
        d = dout("d_x2", [128, 8 * 2048])
        S.final.append(S.add("sp", lambda e: e.dma_start(out=d, in_=P.at(64 * KB, 8 * 2048, F32)), reads=b_x2, dkey="st_dbg"))
        return nc, S, dbg
    hn2 = P.at(128 * KB, 8 * 2048, BF16).rearrange("p (t c) -> p t c", t=8)
    hn2T = P.at(160 * KB, 16 * 1024, BF16).rearrange("p (k t) -> p k t", k=16)
    b_hn2 = [Buf() for _ in range(8)]
    b_hn2T = [Buf() for _ in range(8)]
    gff = P.at(0, 2048, F32)
    b_gff = Buf()
    junk2 = P.at(8 * KB, 2048, BF16)
    b_junk2 = Buf()
    S.add("sp", lambda e: e.dma_start(out=gff, in_=norm_ffn.partition_broadcast(128)), writes=[b_gff], dkey="ld_g")
    for t in range(8):
        st = sstat[:, 8 + 4 * (t % 2):12 + 4 * (t % 2)]
        bs = b_st[t % 2]
        S.add("act", lambda e, t=t, st=st: e.activation(out=junk2, in_=x2[:, t, :], func=AF.Square, accum_out=st[:, 0:1]),
              reads=[b_x2[t]], writes=[b_junk2, bs])
        S.add("dve", lambda e, st=st: e.tensor_scalar(out=st[:, 1:2], in0=st[:, 0:1], scalar1=1.0 / 2048, scalar2=1e-6, op0=ALU.mult, op1=ALU.add),
              reads=[bs], writes=[bs])
        S.add("act", lambda e, st=st: e.activation(out=st[:, 2:3], in_=st[:, 1:2], func=AF.Sqrt), reads=[bs], writes=[bs])
        S.add("dve", lambda e, st=st: e.reciprocal(out=st[:, 3:4], in_=st[:, 2:3]), reads=[bs], writes=[bs])
        S.add("dve", lambda e, t=t, st=st: e.scalar_tensor_tensor(out=hn2[:, t, :], in0=x2[:, t, :], scalar=st[:, 3:4], in1=gff,
                                                                 op0=ALU.mult, op1=ALU.mult), reads=[b_x2[t], bs, b_gff], writes=[b_hn2[t]])
        transpose_tile(hn2[:, t, :], b_hn2[t], hn2T, b_hn2T[t], t * 128)
    wr = P.at(12 * KB, 16 * 72, BF16).rearrange("p (k c) -> p k c", k=16)
    brt = P.at(16 * KB, 72, F32)
    b_wr, b_brt = Buf(), Buf()
    cast_load(wr, w_rt.rearrange("(k p) c -> p k c", p=128), [b_wr], "ld_wr")
    S.add("sp", lambda e: e.dma_start(out=brt, in_=b_rt.partition_broadcast(128)), writes=[b_brt], dkey="ld_brt")
    ustr = P.at(17 * KB, 128, BF16)
    iot = P.at(63 * KB, 128, F32)
    b_c3 = Buf()
    S.add("sp", [lambda e: e.dma_start(out=ustr, in_=c_ustr), lambda e: e.dma_start(out=iot, in_=c_iota)], writes=[b_c3], dkey="const3")
    A1h = P.at(194 * KB, 512, BF16).rearrange("p (t n) -> p t n", t=8)
    A2h = P.at(195 * KB, 512, BF16).rearrange("p (t n) -> p t n", t=8)
    W12 = P.at(199 * KB, 16, F32)
    YY = dscr("YY", [8192, 2048])
    Ind = P.at(196 * KB, 512, BF16).rearrange("p (t n) -> p t n", t=8)
    Rk = P.at(197 * KB, 512, F32).rearrange("p (t n) -> p t n", t=8)
    b_Wm, b_Ind, b_Rk = Buf(), Buf(), Buf()
    rl = P.at(20 * KB, 72, F32)
    Lm = P.at(21 * KB, 64, F32)
    mg = P.at(22 * KB, 8, F32)
    pen = P.at(22 * KB + 64, 8, F32)
    m8r = P.at(22 * KB + 128, 8, F32)
    sm = P.at(22 * KB + 192, 16, F32)
    a1 = P.at(23 * KB, 64, F32)
    a2 = P.at(23 * KB + 256, 64, F32)
    b_r = Buf()
    for t in range(8):
        for kc in range(16):
            mm(bank(7)[:, 0:72], hn2T[:, kc, t * 128:(t + 1) * 128], wr[:, kc, :], kc == 0, kc == 15, [b_hn2T[t], b_wr], 7)
        R = [b_r]
        S.add("dve", lambda e: e.tensor_tensor(out=rl, in0=bank(7)[:, 0:72], in1=brt, op=ALU.add), reads=[bbuf[7], b_brt], writes=R)
        S.add("dve", lambda e: e.reduce_max(out=sm[:, 0:1], in_=rl[:, 0:8], axis=AX.X), reads=R, writes=R)
        S.add("dve", lambda e: e.tensor_scalar(out=mg, in0=rl[:, 0:8], scalar1=sm[:, 0:1], scalar2=None, op0=ALU.is_ge), reads=R, writes=R)
        S.add("dve", lambda e: e.tensor_scalar(out=sm[:, 1:2], in0=sm[:, 0:1], scalar1=-1.0, scalar2=None, op0=ALU.mult), reads=R, writes=R)
        S.add("act", lambda e: e.activation(out=a1[:, 0:8], in_=rl[:, 0:8], func=AF.Exp, bias=sm[:, 1:2], accum_out=sm[:, 2:3]), reads=R, writes=R)
        S.add("dve", lambda e: e.reciprocal(out=sm[:, 3:4], in_=sm[:, 2:3]), reads=R, writes=R)
        S.add("dve", lambda e: e.tensor_scalar(out=pen, in0=mg, scalar1=1e30, scalar2=-1e30, op0=ALU.mult, op1=ALU.add), reads=R, writes=R)
        S.add("dve", lambda e: e.tensor_tensor(out=Lm.rearrange("p (g x) -> p g x", g=8), in0=rl[:, 8:72].rearrange("p (g x) -> p g x", g=8),
                                               in1=mg.unsqueeze(2).to_broadcast([128, 8, 8]), op=ALU.mult), reads=R, writes=R)
        S.add("dve", lambda e: e.tensor_tensor(out=Lm.rearrange("p (g x) -> p g x", g=8), in0=Lm.rearrange("p (g x) -> p g x", g=8),
                                               in1=pen.unsqueeze(2).to_broadcast([128, 8, 8]), op=ALU.add), reads=R, writes=R)
        S.add("dve", lambda e: e.max(out=m8r, in_=Lm), reads=R, writes=R)
        S.add("dve", lambda e: e.tensor_tensor(out=sm[:, 4:5], in0=m8r[:, 0:1], in1=m8r[:, 1:2], op=ALU.subtract), reads=R, writes=R)
        S.add("act", lambda e: e.activation(out=sm[:, 5:6], in_=sm[:, 4:5], func=AF.Sigmoid), reads=R, writes=R)
        S.add("dve", lambda e: e.tensor_tensor(out=sm[:, 6:7], in0=sm[:, 5:6], in1=sm[:, 3:4], op=ALU.mult), reads=R, writes=R)
        S.add("dve", lambda e: e.tensor_tensor(out=sm[:, 7:8], in0=sm[:, 3:4], in1=sm[:, 6:7], op=ALU.subtract), reads=R, writes=R)
        S.add("dve", lambda e: e.tensor_scalar(out=a1, in0=Lm, scalar1=m8r[:, 0:1], scalar2=sm[:, 6:7], op0=ALU.is_equal, op1=ALU.mult), reads=R, writes=R)
        S.add("dve", lambda e: e.tensor_scalar(out=a2, in0=Lm, scalar1=m8r[:, 1:2], scalar2=sm[:, 7:8], op0=ALU.is_equal, op1=ALU.mult), reads=R, writes=R)
        S.add("dve", lambda e, t=t: e.tensor_scalar(out=A1h[:, t, :], in0=Lm, scalar1=m8r[:, 0:1], scalar2=None, op0=ALU.is_equal), reads=R, writes=[b_Wm])
        S.add("dve", lambda e, t=t: e.tensor_scalar(out=A2h[:, t, :], in0=Lm, scalar1=m8r[:, 1:2], scalar2=None, op0=ALU.is_equal), reads=R, writes=[b_Wm])
        S.add("dve", lambda e, t=t: e.tensor_copy(out=W12[:, 2 * t:2 * t + 2], in_=sm[:, 6:8]), reads=R, writes=[b_Wm])
        S.add("dve", lambda e, t=t: e.tensor_scalar(out=Ind[:, t, :], in0=Lm, scalar1=m8r[:, 1:2], scalar2=None, op0=ALU.is_ge), reads=R, writes=[b_Ind])
    for t in range(8):
        for tp in range(t + 1):
            mm(bank(7)[:, 0:64], ustr if tp == t else ones, Ind[:, tp, :], tp == 0, tp == t, [b_Ind, b_c3, b_const], 7)
        S.add("dve", lambda e, t=t: e.tensor_scalar(out=Rk[:, t, :], in0=bank(7)[:, 0:64], scalar1=1.0, scalar2=None, op0=ALU.add),
              reads=[bbuf[7]], writes=[b_Rk])
        S.add("dve", lambda e, t=t: e.tensor_tensor(out=Rk[:, t, :], in0=Rk[:, t, :], in1=Ind[:, t, :], op=ALU.mult), reads=[b_Rk, b_Ind], writes=[b_Rk])
        S.add("dve", lambda e, t=t: e.tensor_scalar(out=Rk[:, t, :], in0=Rk[:, t, :], scalar1=-1.0, scalar2=None, op0=ALU.add),
              reads=[b_Rk], writes=[b_Rk])
    S.barrier()
    wgs = [P.at(0, 16 * 512, BF16).rearrange("p (k f) -> p k f", k=16), P.at(160 * KB, 16 * 512, BF16).rearrange("p (k f) -> p k f", k=16)]
    wus = [P.at(16 * KB, 16 * 512, BF16).rearrange("p (k f) -> p k f", k=16), P.at(176 * KB, 16 * 512, BF16).rearrange("p (k f) -> p k f", k=16)]
    wd = P.at(32 * KB, 4 * 2048, BF16).rearrange("p (f d) -> p f d", f=4)
    wgs_flat = [P.at(0, 16 * 512, BF16).rearrange("p (a b) -> p a b", b=2048), P.at(160 * KB, 16 * 512, BF16).rearrange("p (a b) -> p a b", b=2048)]
    wus_flat = [P.at(16 * KB, 16 * 512, BF16).rearrange("p (a b) -> p a b", b=2048), P.at(176 * KB, 16 * 512, BF16).rearrange("p (a b) -> p a b", b=2048)]
    b_wg, b_wu, b_wd = [Buf(), Buf()], [Buf(), Buf()], Buf()
    Sel = P.at(48 * KB, 1024, BF16).rearrange("p (t r) -> p t r", t=8)
    XeT = P.at(50 * KB, 2048, BF16).rearrange("p (k r) -> p k r", k=16)
    HT = P.at(54 * KB, 512, BF16).rearrange("p (f r) -> p f r", f=4)
    Ysbs = [P.at(55 * KB, 2048, BF16), P.at(59 * KB, 2048, BF16)]
    b_Ysbs = [Buf(), Buf()]
    sgt = P.at(192 * KB, 512, F32)
    b_Sel, b_XeT, b_HT, b_sgt = [Buf() for _ in range(4)]
    Sel2 = [Sel, P.at(203 * KB, 1024, BF16).rearrange("p (t r) -> p t r", t=8)]
    b_Sel2 = [b_Sel, Buf()]

    def gen_sel(ex):
        sl = ex % 2
        for t in range(8):
            S.add("dve", lambda e, t=t: e.tensor_scalar(out=Sel2[sl][:, t, :], in0=iot, scalar1=Rk[:, t, ex:ex + 1], scalar2=None,
                                                       op0=ALU.is_equal), reads=[b_c3, b_Rk], writes=[b_Sel2[sl]])

    def load_exp(ex):
        s = ex % 2
        if ex >= CV0:
            i = ex - CV0
            ex_dep = [cv_last[0]]
            S.add("sp", lambda e: e.dma_start(out=wgs_flat[s], in_=WBg[i].rearrange("a b -> (a b)").rearrange("(p a b) -> p a b", p=128, b=2048)),
                  writes=[b_wg[s]], dkey="ld_wg%d" % s, extra=ex_dep)
            S.add("sp", lambda e: e.dma_start(out=wus_flat[s], in_=WBu[i].rearrange("a b -> (a b)").rearrange("(p a b) -> p a b", p=128, b=2048)),
                  writes=[b_wu[s]], dkey="ld_wu%d" % s, extra=ex_dep)
            return
        S.add("pool", lambda e: e.dma_start(out=wgs_flat[s], in_=w_eg[ex].rearrange("(p k) f -> p (k f)", k=16).rearrange("p (a b) -> p a b", b=2048)),
              writes=[b_wg[s]], dkey="ld_wg%d" % s)
        S.add("pool", lambda e: e.dma_start(out=wus_flat[s], in_=w_eu[ex].rearrange("(p k) f -> p (k f)", k=16).rearrange("p (a b) -> p a b", b=2048)),
              writes=[b_wu[s]], dkey="ld_wu%d" % s)

    def load_wd(ex):
        if ex >= CV0:
            i = ex - CV0
            S.add("sp", lambda e: e.dma_start(out=wd, in_=WBd[i].rearrange("(f p) d -> p f d", p=128)), writes=[b_wd], dkey="ld_wd",
                  extra=[cv_last[0]])
            return
        S.add("pool", lambda e: e.dma_start(out=wd, in_=w_ed[ex].rearrange("(f p) d -> p f d", p=128)), writes=[b_wd], dkey="ld_wd")

    def gather(ex):
        sl = ex % 2
        for k4 in range(4):
            bl = k4 % 2
            for q in range(4):
                kc = 4 * k4 + q
                for t in range(8):
                    mm(bank(bl)[:, q * 128:(q + 1) * 128], hn2[:, t, kc:2048:16], Sel2[sl][:, t, :], t == 0, t == 7,
                       [b_hn2[t], b_Sel2[sl]], bl)
            evac(XeT[:, 4 * k4:4 * k4 + 4, :], bank(bl).rearrange("p (q r) -> p q r", q=4), [bbuf[bl]], [b_XeT])

    issue_cv(len(cv_chunks))
    load_exp(0)
    load_wd(0)
    gen_sel(0)
    gather(0)
    for ex in range(64):
        s = ex % 2
        if ex + 1 < 64:
            load_exp(ex + 1)
        for fc in range(4):
            for kc in range(16):
                mm(bank(2)[:, fc * 128:(fc + 1) * 128], wgs[s][:, kc, fc * 128:(fc + 1) * 128], XeT[:, kc, :], kc == 0, kc == 15, [b_wg[s], b_XeT], 2)
        for fc in range(4):
            for kc in range(16):
                mm(bank(3)[:, fc * 128:(fc + 1) * 128], wus[s][:, kc, fc * 128:(fc + 1) * 128], XeT[:, kc, :], kc == 0, kc == 15, [b_wu[s], b_XeT], 3)
        S.add("act", lambda e: e.activation(out=sgt, in_=bank(2), func=AF.Silu), reads=[bbuf[2]], writes=[b_sgt])
        S.add("dve", lambda e: e.tensor_tensor(out=HT.rearrange("p f r -> p (f r)"), in0=bank(3), in1=sgt, op=ALU.mult), reads=[bbuf[3], b_sgt], writes=[b_HT])
        if ex + 1 < 64:
            gen_sel(ex + 1)
            gather(ex + 1)
        ys = ex % 2
        for dg in range(4):
            bl = 4 + dg % 4
            for fc in range(4):
                mm(bank(bl), HT[:, fc, :], wd[:, fc, dg * 512:(dg + 1) * 512], fc == 0, fc == 3, [b_HT, b_wd], bl)
            evac(Ysbs[ys][:, dg * 512:(dg + 1) * 512], bank(bl), [bbuf[bl]], [b_Ysbs[ys]])
        if ex + 1 < 64:
            load_wd(ex + 1)
        S.add("sp", lambda e, ys=ys, ex=ex: e.dma_start(out=YY[ex * 128:(ex + 1) * 128, :], in_=Ysbs[ys]), reads=[b_Ysbs[ys]],
              dkey="st_ys%d" % ys)
    S.barrier()
    eb = P.at(0, 64, F32)
    vv = P.at(512, 64, F32)
    idf = P.at(1024, 16, F32)
    idu = P.at(2048, 16, F32).bitcast(mybir.dt.uint32)
    b_cmb = Buf()
    gbuf = [P.at(8 * KB + i * 4 * KB, 2048, BF16) for i in range(4)]
    b_gbuf = [Buf() for _ in range(4)]
    S.add("dve", lambda e: e.tensor_scalar(out=eb, in0=iot[:, 0:64], scalar1=128.0, scalar2=None, op0=ALU.mult), reads=[b_c3], writes=[b_cmb])
    for t in range(8):
        for k, Ah in enumerate((A1h, A2h)):
            col = 2 * t + k
            S.add("dve", lambda e, t=t: e.tensor_tensor(out=vv, in0=Rk[:, t, :], in1=eb, op=ALU.add), reads=[b_Rk, b_cmb], writes=[b_cmb])
            S.add("dve", lambda e, t=t, Ah=Ah: e.tensor_tensor(out=vv, in0=vv, in1=Ah[:, t, :], op=ALU.mult), reads=[b_cmb, b_Wm], writes=[b_cmb])
            S.add("dve", lambda e, col=col: e.reduce_sum(out=idf[:, col:col + 1], in_=vv, axis=AX.X), reads=[b_cmb], writes=[b_cmb])
    S.add("dve", lambda e: e.tensor_copy(out=idu, in_=idf), reads=[b_cmb], writes=[b_cmb])
    for t in range(8):
        for k in range(2):
            col = 2 * t + k
            gi_ = (2 * t + k) % 4
            S.add("pool", lambda e, col=col, gi_=gi_: e.indirect_dma_start(out=gbuf[gi_], out_offset=None, in_=YY,
                                                                          in_offset=bass.IndirectOffsetOnAxis(ap=idu[:, col:col + 1], axis=0)),
                  reads=[b_cmb], writes=[b_gbuf[gi_]], dkey="ld_gb%d" % gi_)
            S.add("dve", lambda e, t=t, col=col, gi_=gi_: e.scalar_tensor_tensor(out=x2[:, t, :], in0=gbuf[gi_], scalar=W12[:, col:col + 1],
                                                                                in1=x2[:, t, :], op0=ALU.mult, op1=ALU.add),
                  reads=[b_gbuf[gi_], b_Wm, b_x2[t]], writes=[b_x2[t]])
    S.barrier()

    gfn = P.at(0, 2048, F32)
    b_gfn = Buf()
    S.add("sp", lambda e: e.dma_start(out=gfn, in_=final_norm.partition_broadcast(128)), writes=[b_gfn], dkey="ld_g")
    ost = [P.at(8 * KB + i * 8 * KB, 2048, F32) for i in range(2)]
    b_ost = [Buf(), Buf()]
    junk3 = P.at(24 * KB, 2048, BF16)
    b_junk3 = Buf()
    for t in range(8):
        st = sstat[:, 16 + 4 * (t % 2):20 + 4 * (t % 2)]
        bs = b_st[t % 2]
        o_ = ost[t % 2]
        S.add("act", lambda e, t=t, st=st: e.activation(out=junk3, in_=x2[:, t, :], func=AF.Square, accum_out=st[:, 0:1]),
              reads=[b_x2[t]], writes=[b_junk3, bs])
        S.add("dve", lambda e, st=st: e.tensor_scalar(out=st[:, 1:2], in0=st[:, 0:1], scalar1=1.0 / 2048, scalar2=1e-6, op0=ALU.mult, op1=ALU.add),
              reads=[bs], writes=[bs])
        S.add("act", lambda e, st=st: e.activation(out=st[:, 2:3], in_=st[:, 1:2], func=AF.Sqrt), reads=[bs], writes=[bs])
        S.add("dve", lambda e, st=st: e.reciprocal(out=st[:, 3:4], in_=st[:, 2:3]), reads=[bs], writes=[bs])
        S.add("dve", lambda e, t=t, st=st, o_=o_: e.scalar_tensor_tensor(out=o_, in0=x2[:, t, :], scalar=st[:, 3:4], in1=gfn,
                                                                        op0=ALU.mult, op1=ALU.mult), reads=[b_x2[t], bs, b_gfn], writes=[b_ost[t % 2]])
        S.final.append(S.add("sp", lambda e, t=t, o_=o_: e.dma_start(out=out[t * 128:(t + 1) * 128, :], in_=o_), reads=[b_ost[t % 2]],
                             dkey="st_out%d" % (t % 2)))
    return nc, S, dbg


def host_inputs(inputs):
    x = np.ascontiguousarray(inputs["x"], dtype=np.float32)
    rel = np.asarray(inputs["rel_bias"], np.float32)
    bf = ml_dtypes.bfloat16
    shared = dict(
        w_in=np.ascontiguousarray(inputs["w_in"][0]),
        norm_mix=np.ascontiguousarray(inputs["norm_mix"]).reshape(1, 2048),
        norm_ffn=np.ascontiguousarray(inputs["norm_ffn"]).reshape(1, 2048),
        final_norm=np.ascontiguousarray(inputs["final_norm"]).reshape(1, 2048),
        peT_k=np.ascontiguousarray(inputs["cmp_pe_k"][0].T),
        peT_v=np.ascontiguousarray(inputs["cmp_pe_v"][0].T),
        w1_k=np.ascontiguousarray(inputs["cmp_w1_k"][0]),
        w1_v=np.ascontiguousarray(inputs["cmp_w1_v"][0]),
        w2_k=np.ascontiguousarray(inputs["cmp_w2_k"][0]),
        w2_v=np.ascontiguousarray(inputs["cmp_w2_v"][0]),
        w_up_a=np.ascontiguousarray(inputs["w_up_nsa"][0]),
        w_up_b=np.ascontiguousarray(inputs["w_up_moba"][0]),
        w_out=np.ascontiguousarray(inputs["w_out"][0]),
        w_rt=np.ascontiguousarray(np.concatenate(
            [inputs["w_group"][0], inputs["w_router"][0].transpose(1, 0, 2).reshape(2048, 64)], axis=1)),
        b_rt=np.ascontiguousarray(np.concatenate([inputs["b_group"][0].reshape(8), inputs["b_router"][0].reshape(64)]).reshape(1, 72)),
        w_eg=np.ascontiguousarray(inputs["w_exp_gate"][0]),
        w_eu=np.ascontiguousarray(inputs["w_exp_up"][0]),
        w_ed=np.ascontiguousarray(inputs["w_exp_down"][0]),
        c_ident=np.eye(128, dtype=np.float32).astype(bf),
        c_ones=np.ones((128, 128), np.float32).astype(bf),
        c_ustr=np.triu(np.ones((128, 128), np.float32), 1).astype(bf),
        c_iota=np.tile(np.arange(128, dtype=np.float32)[None, :], (128, 1)),
        crep=np.ascontiguousarray(np.tile(rel[31:32, :], (128, 1))),
    )
    c_start = np.arange(255) * 16
    sb = np.arange(64) * 64
    ovl = ((c_start[:, None] < sb[None, :] + 64) & (c_start[:, None] + 32 > sb[None, :])).astype(np.float32)
    ovl65 = np.zeros((256, 65), np.float32)
    ovl65[:255, :64] = ovl
    ovl65[:255, 64] = 1.0
    shared["c_ovl"] = np.ascontiguousarray(ovl65.reshape(2, 128, 65).transpose(1, 0, 2).reshape(128, 130)).astype(bf)
    e64 = np.zeros((64, 32, 128), np.float32)
    e16 = np.zeros((16, 32, 128), np.float32)
    for kt in range(32):
        e64[2 * kt, kt, 0:64] = 1
        e64[2 * kt + 1, kt, 64:128] = 1
        e16[kt // 2, kt, :] = 1
    shared["c_e64"] = e64.reshape(64, 4096).astype(bf)
    shared["c_e16"] = e16.reshape(16, 4096).astype(bf)
    s24 = np.zeros((24, 24, 128), np.float32)
    for i in range(24):
        s24[i, i, :] = 1
    shared["c_sel24"] = s24.reshape(24, 24 * 128).astype(bf)

    maps = []
    for c in range(8):
        b, j = divmod(c, 4)
        tiles = [4 * m + j for m in range(8)]
        tpos = np.concatenate([np.arange(128) + 128 * a for a in tiles])
        m = dict(shared)
        m["xb"] = x[b]
        m["xo"] = np.ascontiguousarray(x[b].reshape(32, 128, 2048)[tiles].reshape(1024, 2048))
        cend = c_start + 31
        dist_c = tpos[None, :] - cend[:, None]
        bc = np.full((8, 256, 1024), NEG, np.float32)
        val = rel[rel_bucket_np(dist_c)][:, :, :8]
        bc[:, :255, :] = np.where((dist_c >= 0)[None], val.transpose(2, 0, 1), NEG)
        m["biasC"] = bc
        r = np.arange(128)
        bd = np.zeros((16, 128, 5, 128), np.float32)
        for v in range(5):
            dist = 128 * (j + 1 - v) + r[None, :] - r[:, None]
            tb = rel[rel_bucket_np(dist)]
            bd[:, :, v, :] = np.where((dist >= 0)[None], tb.transpose(2, 0, 1), NEG)
        m["bdiag"] = bd.reshape(16, 128, 640)
        bw = np.zeros((8, 128, 8, 128), np.float32)
        for v in range(8):
            dist = 128 * (j + 4 - v) + r[None, :] - r[:, None]
            tb = rel[rel_bucket_np(dist)][:, :, :8]
            bw[:, :, v, :] = np.where(((dist >= 0) & (dist < 512))[None], tb.transpose(2, 0, 1), NEG)
        m["bwin"] = bw.reshape(8, 128, 1024)
        cur = tpos // 64
        n64 = np.arange(64)[None, :]
        forced = (n64 == 0) | (n64 == cur[:, None]) | (n64 == cur[:, None] - 1)
        m["fsel"] = np.ascontiguousarray(np.where(forced, 1e4, 0.0).astype(np.float32).reshape(8, 128, 64).transpose(1, 0, 2).reshape(128, 512))
        own = tpos // 256
        n16 = np.arange(16)[None, :]
        past = n16 < own[:, None]

        def lay16(a):
            return np.ascontiguousarray(a.astype(np.float32).reshape(8, 128, 16).transpose(1, 0, 2).reshape(128, 128))
        m["pmneg"] = lay16(np.where(past, 0.0, -1e30))
        m["past01"] = lay16(past)
        m["own01"] = lay16(n16 == own[:, None])
        maps.append(m)
    return maps


def kernel(**inputs):
    nc, S, dbg = build()
    with contextlib.ExitStack() as st:
        S.emit(st)
    maps = host_inputs(inputs)
    res = run_bass_kernel_spmd(nc, maps, core_ids=list(range(8)))
    outp = np.zeros((2, 32, 128, 2048), np.float32)
    for c in range(8):
        b, j = divmod(c, 4)
        o = np.asarray(res.results[c]["out"]).reshape(8, 128, 2048)
        for m in range(8):
            outp[b, 4 * m + j] = o[m]
    return outp.reshape(2, 4096, 2048)
```
